# Optimizing a Trainium2 kernel written in Bass

```python
import jax, jax.numpy as jnp
from jax import lax
import numpy as np

D_MODEL = 1024
BATCH = 8
SEQ = 4096
DEPTH = 4

D_MIX = D_MODEL
MLSTM_HEADS = 4
MLSTM_DQK = D_MODEL // 16
MLSTM_DV = D_MODEL // 8
GDN_HEADS = 4
GDN_DK = D_MODEL // 8
GDN_DV = D_MODEL // 8
CONV_K = 4
CHUNK = 64
GATE_SOFTCAP = 15.0
ML_QK = MLSTM_HEADS * MLSTM_DQK
ML_V = MLSTM_HEADS * MLSTM_DV
GD_QK = GDN_HEADS * GDN_DK
GD_V = GDN_HEADS * GDN_DV
SPLIT_SIZES = (ML_QK, ML_QK, ML_V, ML_V, MLSTM_HEADS, MLSTM_HEADS,
               GD_QK, GD_QK, GD_V, GD_V, GDN_HEADS, GDN_HEADS)
SPLIT_OFFSETS = tuple(int(o) for o in np.cumsum(SPLIT_SIZES)[:-1])
D_IN_PROJ = int(sum(SPLIT_SIZES))
N_MEM = 256
XA_HEADS = 4
XA_DH = D_MODEL // XA_HEADS
N_EXPERTS = 32
TOP_K = 4
D_EXPERT = D_MODEL
SWIGLU_LIMIT = 7.0
SWIGLU_ALPHA = 1.702
MOE_BLOCK = 256
DEEPNORM_ALPHA = (2.0 * DEPTH) ** 0.25
DEEPNORM_BETA = (8.0 * DEPTH) ** -0.25
LN_EPS = 1e-5
NORM_EPS = 1e-6

kernel_name = 'hybrid_mlstm_gdn_memxattn_moe_deepnorm'


def layer_norm(x, g, b):
    xf = x.astype(jnp.float32)
    mu = jnp.mean(xf, axis=-1, keepdims=True)
    var = jnp.mean(jnp.square(xf - mu), axis=-1, keepdims=True)
    return ((xf - mu) * lax.rsqrt(var + LN_EPS) * g + b).astype(x.dtype)


def rms_norm(x, g):
    return x * lax.rsqrt(jnp.mean(x * x, axis=-1, keepdims=True) + NORM_EPS) * g


def l2_normalize(x):
    return x * lax.rsqrt(jnp.sum(x * x, axis=-1, keepdims=True) + NORM_EPS)


def soft_cap(x):
    return GATE_SOFTCAP * jnp.tanh(x / GATE_SOFTCAP)


def to_head_chunks(t, n_heads):
    b, s, _ = t.shape
    return t.reshape(b, s // CHUNK, CHUNK, n_heads, -1).transpose(0, 3, 1, 2, 4)


def gate_chunks(t):
    b, s, h = t.shape
    return t.reshape(b, s // CHUNK, CHUNK, h).transpose(0, 3, 1, 2)


def from_head_chunks(t):
    b, h, n, l, d = t.shape
    return t.transpose(0, 2, 3, 1, 4).reshape(b, n * l, h * d)


def causal_depthwise_conv(x, w):
    s = x.shape[1]
    xp = jnp.pad(x, ((0, 0), (CONV_K - 1, 0), (0, 0)))
    y = w[0] * xp[:, 0:s]
    for j in range(1, CONV_K):
        y = y + w[j] * xp[:, j:j + s]
    return y


def mlstm_chunkwise(q, k, v, i_pre, f_pre):
    log_f = jax.nn.log_sigmoid(f_pre)
    b_cum = jnp.cumsum(log_f, axis=-1)
    causal = jnp.tril(jnp.ones((CHUNK, CHUNK), dtype=bool))
    d_intra = jnp.where(causal, b_cum[..., :, None] - b_cum[..., None, :] + i_pre[..., None, :], -jnp.inf)
    b_last = b_cum[..., -1]
    log_w_end = b_last[..., None] - b_cum + i_pre

    def step(carry, xs):
        c, n, m = carry
        k_j, v_j, bl_j, lw_j = xs
        m_new = jnp.maximum(bl_j + m, jnp.max(lw_j, axis=-1))
        decay = jnp.exp(bl_j + m - m_new)
        kw = k_j * jnp.exp(lw_j - m_new[..., None])[..., None]
        c_new = decay[..., None, None] * c + jnp.einsum('bhlk,bhlv->bhkv', kw, v_j)
        n_new = decay[..., None] * n + jnp.sum(kw, axis=-2)
        return (c_new, n_new, m_new), (c, n, m)

    bsz, nh = q.shape[0], q.shape[1]
    init = (jnp.zeros((bsz, nh, q.shape[-1], v.shape[-1]), jnp.float32),
            jnp.zeros((bsz, nh, q.shape[-1]), jnp.float32),
            jnp.zeros((bsz, nh), jnp.float32))
    xs = (jnp.moveaxis(k, 2, 0), jnp.moveaxis(v, 2, 0), jnp.moveaxis(b_last, 2, 0), jnp.moveaxis(log_w_end, 2, 0))
    _, (c_prev, n_prev, m_prev) = lax.scan(step, init, xs)
    c_prev = jnp.moveaxis(c_prev, 0, 2)
    n_prev = jnp.moveaxis(n_prev, 0, 2)
    m_prev = jnp.moveaxis(m_prev, 0, 2)

    m_inter = b_cum + m_prev[..., None]
    m_t = jnp.maximum(m_inter, jnp.max(d_intra, axis=-1))
    s_intra = jnp.einsum('bhntk,bhnsk->bhnts', q, k) * jnp.exp(d_intra - m_t[..., None])
    w_inter = jnp.exp(m_inter - m_t)
    num = (w_inter[..., None] * jnp.einsum('bhntk,bhnkv->bhntv', q, c_prev)
           + jnp.einsum('bhnts,bhnsv->bhntv', s_intra, v))
    den = w_inter * jnp.einsum('bhntk,bhnk->bhnt', q, n_prev) + jnp.sum(s_intra, axis=-1)
    return num / jnp.maximum(jnp.abs(den), jnp.exp(-m_t))[..., None]


def gated_delta_chunkwise(q, k, v, g, beta):
    gc = jnp.cumsum(g, axis=-1)
    causal = jnp.tril(jnp.ones((CHUNK, CHUNK), dtype=bool))
    strict = jnp.tril(jnp.ones((CHUNK, CHUNK), dtype=bool), -1)
    decay = jnp.exp(jnp.where(causal, gc[..., :, None] - gc[..., None, :], -jnp.inf))
    kk = jnp.einsum('bhntk,bhnsk->bhnts', k, k)
    a_mat = jnp.where(strict, beta[..., None] * kk * decay, 0.0) + jnp.eye(CHUNK, dtype=jnp.float32)
    rhs = jnp.concatenate([v * beta[..., None], k * (beta * jnp.exp(gc))[..., None]], axis=-1)
    sol = lax.linalg.triangular_solve(a_mat, rhs, left_side=True, lower=True, unit_diagonal=True)
    u, w = sol[..., :v.shape[-1]], sol[..., v.shape[-1]:]
    g_last = gc[..., -1]
    k_end = k * jnp.exp(g_last[..., None] - gc)[..., None]

    def step(s, xs):
        u_j, w_j, ke_j, gl_j = xs
        v_new = u_j - jnp.einsum('bhlk,bhkv->bhlv', w_j, s)
        s_next = jnp.exp(gl_j)[..., None, None] * s + jnp.einsum('bhlk,bhlv->bhkv', ke_j, v_new)
        return s_next, (s, v_new)

    s0 = jnp.zeros((q.shape[0], q.shape[1], k.shape[-1], v.shape[-1]), jnp.float32)
    xs = (jnp.moveaxis(u, 2, 0), jnp.moveaxis(w, 2, 0), jnp.moveaxis(k_end, 2, 0), jnp.moveaxis(g_last, 2, 0))
    _, (s_prev, v_new) = lax.scan(step, s0, xs)
    s_prev = jnp.moveaxis(s_prev, 0, 2)
    v_new = jnp.moveaxis(v_new, 0, 2)
    qk = jnp.einsum('bhntk,bhnsk->bhnts', q, k) * decay
    return (jnp.einsum('bhntk,bhnkv->bhntv', q * jnp.exp(gc)[..., None], s_prev)
            + jnp.einsum('bhnts,bhnsv->bhntv', qk, v_new))


def hybrid_mixer(x, w_in, ml_b_i, ml_b_f, ml_norm, gd_conv, gd_a_log, gd_dt_bias, gd_norm, w_out):
    proj = (x @ w_in).astype(jnp.float32)
    (ml_q, ml_k, ml_v, ml_o, ml_i, ml_f,
     gd_q, gd_k, gd_v, gd_z, gd_b, gd_a) = jnp.split(proj, SPLIT_OFFSETS, axis=-1)

    i_pre = gate_chunks(soft_cap(ml_i + ml_b_i))
    f_pre = gate_chunks(soft_cap(ml_f + ml_b_f))
    h_ml = mlstm_chunkwise(to_head_chunks(ml_q, MLSTM_HEADS) * MLSTM_DQK ** -0.5,
                           to_head_chunks(ml_k, MLSTM_HEADS),
                           to_head_chunks(ml_v, MLSTM_HEADS), i_pre, f_pre)
    h_ml = rms_norm(h_ml, ml_norm.reshape(MLSTM_HEADS, 1, 1, MLSTM_DV))
    h_ml = from_head_chunks(h_ml) * jax.nn.sigmoid(ml_o)

    qkv = jax.nn.silu(causal_depthwise_conv(jnp.concatenate([gd_q, gd_k, gd_v], axis=-1), gd_conv))
    cq, ck, cv = qkv[..., :GD_QK], qkv[..., GD_QK:2 * GD_QK], qkv[..., 2 * GD_QK:]
    beta = gate_chunks(jax.nn.sigmoid(gd_b))
    g = gate_chunks(-jnp.exp(gd_a_log) * jax.nn.softplus(gd_a + gd_dt_bias))
    h_gd = gated_delta_chunkwise(l2_normalize(to_head_chunks(cq, GDN_HEADS)) * GDN_DK ** -0.5,
                                 l2_normalize(to_head_chunks(ck, GDN_HEADS)),
                                 to_head_chunks(cv, GDN_HEADS), g, beta)
    h_gd = rms_norm(h_gd, gd_norm) * jax.nn.silu(to_head_chunks(gd_z, GDN_HEADS))
    h_gd = from_head_chunks(h_gd)

    mixed = jnp.concatenate([h_ml, h_gd], axis=-1).astype(x.dtype)
    return mixed @ w_out


def memory_cross_attention(x, mem, wq, wk, wv, wo):
    b, s, d = x.shape
    m = mem.shape[1]
    q = (x @ wq).reshape(b, s, XA_HEADS, XA_DH)
    k = (mem @ wk).reshape(b, m, XA_HEADS, XA_DH)
    v = (mem @ wv).reshape(b, m, XA_HEADS, XA_DH)
    scores = jnp.einsum('bshd,bmhd->bhsm', q, k).astype(jnp.float32) * XA_DH ** -0.5
    p = jax.nn.softmax(scores, axis=-1).astype(x.dtype)
    o = jnp.einsum('bhsm,bmhd->bshd', p, v).reshape(b, s, d)
    return o @ wo


def moe_ffn(h, router_w, router_b, w_gu, b_gu, w_down, b_down):
    bsz, seq, d = h.shape
    tokens = h.reshape(-1, d)
    n_tok = tokens.shape[0]
    logits = (tokens @ router_w + router_b).astype(jnp.float32)
    top_logit, top_idx = lax.top_k(logits, TOP_K)
    gates = jax.nn.softmax(top_logit, axis=-1)
    n_assign = n_tok * TOP_K
    flat_e = top_idx.reshape(-1).astype(jnp.int32)
    flat_tok = jnp.arange(n_assign, dtype=jnp.int32) // TOP_K
    order = jnp.argsort(flat_e)
    sorted_e = flat_e[order]
    counts = jnp.zeros((N_EXPERTS,), jnp.int32).at[flat_e].add(1)
    start = jnp.cumsum(counts) - counts
    padded = (counts + MOE_BLOCK - 1) // MOE_BLOCK * MOE_BLOCK
    pad_end = jnp.cumsum(padded)
    pad_start = pad_end - padded
    dest = pad_start[sorted_e] + jnp.arange(n_assign, dtype=jnp.int32) - start[sorted_e]
    n_blocks = n_assign // MOE_BLOCK + N_EXPERTS
    cap = n_blocks * MOE_BLOCK
    row_tok = jnp.full((cap,), n_tok, jnp.int32).at[dest].set(flat_tok[order])
    row_gate = jnp.zeros((cap,), jnp.float32).at[dest].set(gates.reshape(-1)[order])
    block_expert = jnp.minimum(
        jnp.searchsorted(pad_end, jnp.arange(n_blocks, dtype=jnp.int32) * MOE_BLOCK, side='right'),
        N_EXPERTS - 1)
    tokens_pad = jnp.concatenate([tokens, jnp.zeros((1, d), tokens.dtype)], axis=0)

    def expert_block(args):
        tok_ids, gate, e = args
        xb = tokens_pad[tok_ids]
        gu = xb @ w_gu[e] + b_gu[e]
        glu = jnp.minimum(gu[:, :D_EXPERT], SWIGLU_LIMIT)
        lin = jnp.clip(gu[:, D_EXPERT:], -SWIGLU_LIMIT, SWIGLU_LIMIT)
        act = (lin + 1.0) * glu * jax.nn.sigmoid(SWIGLU_ALPHA * glu)
        yb = act @ w_down[e] + b_down[e]
        return yb * gate[:, None].astype(yb.dtype)

    y_rows = lax.map(expert_block, (row_tok.reshape(n_blocks, MOE_BLOCK),
                                    row_gate.reshape(n_blocks, MOE_BLOCK), block_expert))
    out = jnp.zeros((n_tok + 1, d), y_rows.dtype).at[row_tok].add(y_rows.reshape(cap, d))
    return out[:n_tok].reshape(bsz, seq, d).astype(h.dtype)


def setup_inputs(seed: int = 0) -> dict:
    key = jax.random.key(seed)
    ks = jax.random.split(key, 27)
    L = DEPTH

    def nrm(k, shape, scale):
        return scale * jax.random.normal(k, shape, jnp.float32)

    return {
        'x': nrm(ks[0], (BATCH, SEQ, D_MODEL), 1.0),
        'mem': nrm(ks[1], (BATCH, N_MEM, D_MODEL), 1.0),
        'w_in': nrm(ks[2], (L, D_MODEL, D_IN_PROJ), D_MODEL ** -0.5),
        'mlstm_b_i': nrm(ks[3], (L, MLSTM_HEADS), 0.1),
        'mlstm_b_f': 3.0 + nrm(ks[4], (L, MLSTM_HEADS), 0.5),
        'mlstm_norm_w': 1.0 + nrm(ks[5], (L, ML_V), 0.02),
        'gdn_conv_w': nrm(ks[6], (L, CONV_K, 2 * GD_QK + GD_V), CONV_K ** -0.5),
        'gdn_a_log': jnp.log(jax.random.uniform(ks[7], (L, GDN_HEADS), jnp.float32, 1.0, 16.0)),
        'gdn_dt_bias': 1.0 + nrm(ks[8], (L, GDN_HEADS), 0.1),
        'gdn_norm_w': 1.0 + nrm(ks[9], (L, GDN_DV), 0.02),
        'w_out': nrm(ks[10], (L, D_MIX, D_MODEL), DEEPNORM_BETA * D_MIX ** -0.5),
        'ln1_g': 1.0 + nrm(ks[11], (L, D_MODEL), 0.02),
        'ln1_b': nrm(ks[12], (L, D_MODEL), 0.02),
        'xa_wq': nrm(ks[13], (L, D_MODEL, D_MODEL), D_MODEL ** -0.5),
        'xa_wk': nrm(ks[14], (L, D_MODEL, D_MODEL), D_MODEL ** -0.5),
        'xa_wv': nrm(ks[15], (L, D_MODEL, D_MODEL), D_MODEL ** -0.5),
        'xa_wo': nrm(ks[16], (L, D_MODEL, D_MODEL), DEEPNORM_BETA * D_MODEL ** -0.5),
        'ln2_g': 1.0 + nrm(ks[17], (L, D_MODEL), 0.02),
        'ln2_b': nrm(ks[18], (L, D_MODEL), 0.02),
        'router_w': nrm(ks[19], (L, D_MODEL, N_EXPERTS), D_MODEL ** -0.5),
        'router_b': nrm(ks[20], (L, N_EXPERTS), 0.01),
        'exp_w_gu': nrm(ks[21], (L, N_EXPERTS, D_MODEL, 2 * D_EXPERT), D_MODEL ** -0.5),
        'exp_b_gu': nrm(ks[22], (L, N_EXPERTS, 2 * D_EXPERT), 0.01),
        'exp_w_down': nrm(ks[23], (L, N_EXPERTS, D_EXPERT, D_MODEL), DEEPNORM_BETA * D_EXPERT ** -0.5),
        'exp_b_down': nrm(ks[24], (L, N_EXPERTS, D_MODEL), 0.01),
        'ln3_g': 1.0 + nrm(ks[25], (L, D_MODEL), 0.02),
        'ln3_b': nrm(ks[26], (L, D_MODEL), 0.02),
    }


def reference(x, mem, w_in, mlstm_b_i, mlstm_b_f, mlstm_norm_w, gdn_conv_w, gdn_a_log, gdn_dt_bias,
              gdn_norm_w, w_out, ln1_g, ln1_b, xa_wq, xa_wk, xa_wv, xa_wo, ln2_g, ln2_b,
              router_w, router_b, exp_w_gu, exp_b_gu, exp_w_down, exp_b_down, ln3_g, ln3_b):
    for l in range(DEPTH):
        h = hybrid_mixer(x, w_in[l], mlstm_b_i[l], mlstm_b_f[l], mlstm_norm_w[l], gdn_conv_w[l],
                         gdn_a_log[l], gdn_dt_bias[l], gdn_norm_w[l], w_out[l])
        x = layer_norm(DEEPNORM_ALPHA * x + h, ln1_g[l], ln1_b[l])
        h = memory_cross_attention(x, mem, xa_wq[l], xa_wk[l], xa_wv[l], xa_wo[l])
        x = layer_norm(DEEPNORM_ALPHA * x + h, ln2_g[l], ln2_b[l])
        h = moe_ffn(x, router_w[l], router_b[l], exp_w_gu[l], exp_b_gu[l], exp_w_down[l], exp_b_down[l])
        x = layer_norm(DEEPNORM_ALPHA * x + h, ln3_g[l], ln3_b[l])
    return x
```

```python
import numpy as np
from contextlib import ExitStack
import concourse.bass as bass
import concourse.mybir as mybir
from concourse.bass_utils import run_bass_kernel_spmd

F32 = mybir.dt.float32
BF16 = mybir.dt.bfloat16
I32 = mybir.dt.int32
AF = mybir.ActivationFunctionType
ALU = mybir.AluOpType
AX = mybir.AxisListType

S = 4096
D = 1024
L = 4
NE = 32
CAP = 1024
NCAP = CAP // 128
ALPHA = (2.0 * L) ** 0.25
NEG = -30000.0
GT = 256
NCG = GT // 64

SAME_ENGINE_SYNC = True
EPOCH = 16000
N_DMA_SEMS = 24

C_MLQ, C_MLK, C_MLV, C_MLO, C_MLI, C_MLF = 0, 256, 512, 1024, 1536, 1540
C_GDQ, C_GDK, C_GDV, C_GDZ, C_GDB, C_GDA = 1544, 2056, 2568, 3080, 3592, 3596


class Buf:
    __slots__ = ("name", "w", "r", "excl")

    def __init__(self, name, excl=False):
        self.name = name
        self.w = None
        self.r = []
        self.excl = excl


class Prog:
    ENGS = ("pe", "dve", "act", "pool", "sp")

    def __init__(self, nc, es):
        self.nc = nc
        self.es = es
        self.streams = {e: [] for e in self.ENGS}
        self.count = {e: 0 for e in self.ENGS}
        self.known = {e: {} for e in self.ENGS}
        self.sems = {}
        self.dma_next = {e: 0 for e in self.ENGS}
        self.dma_uses = {}
        self.n_ops = 0

    def sem(self, key):
        s = self.sems.get(key)
        if s is None:
            s = self.es.enter_context(self.nc.semaphore("s_%s_%s" % key))
            self.sems[key] = s
        return s

    def _wait(self, eng, tok):
        key, val = tok
        if key[0] == eng and not SAME_ENGINE_SYNC:
            return
        if self.known[eng].get(key, 0) >= val:
            return
        self.known[eng][key] = val
        self.streams[eng].append(("wait", key, val))

    def _deps(self, eng, reads, writes):
        for b in reads:
            if b.w is not None:
                self._wait(eng, b.w)
        for b in writes:
            if b.w is not None:
                self._wait(eng, b.w)
            for t in b.r:
                self._wait(eng, t)

    def _commit(self, tok, reads, writes):
        for b in reads:
            b.r.append(tok)
        for b in writes:
            b.w = tok
            b.r = []

    def op(self, eng, fn, reads=(), writes=()):
        ex = [b for b in reads if b.excl]
        if ex:
            reads = [b for b in reads if not b.excl]
            writes = list(writes) + ex
        self._deps(eng, reads, writes)
        n = self.count[eng]
        self.count[eng] = n + 1
        key = (eng, n // EPOCH)
        self.sem(key)
        tok = (key, n % EPOCH + 1)
        self.streams[eng].append(("op", fn, key, 1))
        self._commit(tok, reads, writes)
        self.n_ops += 1
        return tok

    def dma(self, eng, fn, reads=(), writes=()):
        self._deps(eng, reads, writes)
        i = self.dma_next[eng]
        self.dma_next[eng] = (i + 1) % N_DMA_SEMS
        key = ("d" + eng, i)
        uses = self.dma_uses.get(key, 0)
        self.sem(key)
        if uses > 0:
            self._wait(eng, (key, 16 * uses))
        self.dma_uses[key] = uses + 1
        tok = (key, 16 * (uses + 1))
        self.streams[eng].append(("op", fn, key, 16))
        self._commit(tok, reads, writes)
        self.n_ops += 1
        return tok

    def barrier(self):
        toks = []
        for e in self.ENGS:
            n = self.count[e]
            if n > 0:
                toks.append(((e, (n - 1) // EPOCH), (n - 1) % EPOCH + 1))
        for k, u in self.dma_uses.items():
            toks.append((k, 16 * u))
        for e in self.ENGS:
            for t in toks:
                if t[0][0] == e:
                    continue
                self._wait(e, t)

    def emit(self):
        nc = self.nc
        engmap = {"pe": "tensor", "dve": "vector", "act": "scalar", "pool": "gpsimd", "sp": "sync"}
        with nc.Block() as block:
            for e in self.ENGS:
                stream = self.streams[e]
                if not stream:
                    continue

                def body(engine, stream=stream):
                    for it in stream:
                        if it[0] == "wait":
                            engine.wait_ge(self.sems[it[1]], it[2])
                        else:
                            it[1](engine).then_inc(self.sems[it[2]], it[3])

                getattr(block, engmap[e])(body)


class Arena:
    def __init__(self, t, ncols):
        self.t = t
        self.n = ncols
        self.off = 0
        self.k = 0

    def reset(self):
        self.off = 0

    def _take(self, c32):
        assert self.off + c32 <= self.n, ("arena overflow", self.off, c32, self.n)
        a = self.t[:, self.off:self.off + c32]
        self.off += c32
        self.k += 1
        return a, Buf("ar%d" % self.k)

    def f32(self, cols):
        return self._take(cols)

    def bf16(self, cols):
        a, b = self._take((cols + 1) // 2)
        return a.bitcast(BF16), b

    def i32(self, cols):
        a, b = self._take(cols)
        return a.bitcast(I32), b


def bc3(ap, shape, axis):
    return ap.unsqueeze(axis).to_broadcast(list(shape))


def build(n_layers=L, stop_after=None, dbg=False, max_groups=None, small=False, skip=()):
    nc = bass.Bass("TRN2", target_bir_lowering=False)
    es = ExitStack()

    BIG = ("xa_wq", "xa_wk", "xa_wv", "xa_wo", "router_w", "exp_w_gu", "exp_b_gu", "exp_w_down", "exp_b_down")

    def din(name, shape, dt=F32):
        if small and name in (small if isinstance(small, (set, tuple, list)) else BIG):
            shape = [1] * len(shape)
        return nc.dram_tensor(name, list(shape), dt, kind="ExternalInput").ap()

    x_in = din("x", [S, D])
    mem_in = din("mem", [256, D])
    w_in = din("w_in", [L, D, 3600])
    b_i = din("mlstm_b_i", [L, 4])
    b_f = din("mlstm_b_f", [L, 4])
    ml_nw = din("mlstm_norm_w", [L, 512])
    conv_w = din("gdn_conv_w", [L, 4, 1536])
    a_log = din("gdn_a_log", [L, 4])
    dt_b = din("gdn_dt_bias", [L, 4])
    gd_nw = din("gdn_norm_w", [L, 128])
    w_out = din("w_out", [L, D, D])
    lng = [din("ln%d_g" % i, [L, D]) for i in (1, 2, 3)]
    lnb = [din("ln%d_b" % i, [L, D]) for i in (1, 2, 3)]
    xa_w = [din("xa_w" + n, [L, D, D]) for n in "qkvo"]
    r_w = din("router_w", [L, D, NE])
    r_b = din("router_b", [L, NE])
    w_gu = din("exp_w_gu", [L, NE, D, 2 * D])
    b_gu = din("exp_b_gu", [L, NE, 2 * D])
    w_dn = din("exp_w_down", [L, NE, D, D])
    b_dn = din("exp_b_down", [L, NE, D])
    c_ident = din("c_ident", [128, 128])
    c_t1 = din("c_t1", [64, 64])
    c_negc = din("c_negc", [64, 64])
    c_negs = din("c_negs", [64, 64])
    c_tri = din("c_tri", [128, 128])
    c_eoff = din("c_eoff", [128, NE])
    c_ones = din("c_ones", [128, 128])
    c_cp = din("c_cp", [128, 16])
    out_d = nc.dram_tensor("out", [S, D], F32, kind="ExternalOutput").ap()
    xres = nc.dram_tensor("xres", [S, D], F32, kind="Internal").ap()
    xs_d = nc.dram_tensor("xs_d", [NE * CAP * 4, 256], BF16, kind="Internal").ap()
    ys_d = nc.dram_tensor("ys_d", [NE * CAP * 4, 256], F32, kind="Internal").ap()
    b_xres = [Buf("xres%d" % i) for i in range(64)]
    b_out = Buf("out")
    b_xs = [Buf("xs%d" % e) for e in range(NE)]
    b_ys = [Buf("ys%d" % e) for e in range(NE)]

    P = Prog(nc, es)

    def sb(name, shape, dt):
        return es.enter_context(nc.sbuf_tensor(name, list(shape), dt))

    with es:
        ident_f = sb("ident_f", [128, 128], F32)
        ident_b = sb("ident_b", [128, 128], BF16)
        ones_f = sb("ones_f", [128, 128], F32)
        ones_b = sb("ones_b", [128, 128], BF16)
        tri_b = sb("tri_b", [128, 128], BF16)
        t1_f = sb("t1_f", [64, 64], F32)
        negc = sb("negc", [64, 64], F32)
        negs = sb("negs", [64, 64], F32)
        eoff = sb("eoff", [128, NE], F32)
        dest_t = sb("dest_t", [128, 512], I32)
        cpoff = sb("cpoff", [128, 16], F32)
        idx_t = [sb("idx%d" % i, [128, 1], I32) for i in range(8)]
        b_idx = [Buf("idx%d" % i) for i in range(8)]
        idx_ctr = [0]
        xbf_t = [[sb("xbf%d_%d" % (i, c), [128, 256], BF16) for c in range(4)] for i in range(2)]
        gth_t = [[sb("gth%d_%d" % (i, c), [128, 256], F32) for c in range(4)] for i in range(4)]
        b_const = Buf("const")
        ARENA_COLS = 44000
        arena_t = sb("arena", [128, ARENA_COLS], F32)
        AR = Arena(arena_t, ARENA_COLS)
        banks = [es.enter_context(nc.psum_tensor("ps%d" % i, [128, 512], F32)) for i in range(7)]
        b_banks = [Buf("bank%d" % i, True) for i in range(7)]
        psb = es.enter_context(nc.psum_tensor("psb", [128, 1024], BF16))
        b_psb = Buf("psb", True)
        bank_ctr = [0]

        def bank():
            i = bank_ctr[0] % 7
            bank_ctr[0] += 1
            return banks[i], b_banks[i]

        def MM(out, lhsT, rhs, R, W, start=True, stop=True):
            P.op("pe", lambda e: e.matmul(out, lhsT, rhs, start=start, stop=stop), R, W)

        def TR(out, in_, idn, R, W):
            P.op("pe", lambda e: e.transpose(out, in_, idn), R, W)

        def TT(eng, out, a, b, op, R, W):
            P.op(eng, lambda e: e.tensor_tensor(out, a, b, op), R, W)

        def TS(eng, out, a, s1, op0, R, W, s2=None, op1=None):
            if op1 is None:
                P.op(eng, lambda e: e.tensor_scalar(out, a, s1, None, op0), R, W)
            else:
                P.op(eng, lambda e: e.tensor_scalar(out, a, s1, s2, op0, op1), R, W)

        def STT(out, a, s, b, op0, op1, R, W):
            P.op("dve", lambda e: e.scalar_tensor_tensor(out, a, s, b, op0, op1), R, W)

        def ACT(out, in_, func, R, W, bias=0.0, scale=1.0):
            P.op("act", lambda e: e.activation(out, in_, func, bias=bias, scale=scale), R, W)

        def CP(eng, out, in_, R, W):
            if eng == "act":
                P.op("act", lambda e: e.copy(out, in_), R, W)
            else:
                P.op(eng, lambda e: e.tensor_copy(out, in_), R, W)

        def DMA(eng, out, in_, R, W):
            return P.dma(eng, lambda e: e.dma_start(out=out, in_=in_), R, W)

        def rsqrt_(out, in_, scale, eps, R, W, tmp):
            ACT(tmp[0], in_, AF.Sqrt, R, [tmp[1]], bias=eps, scale=scale)
            P.op("dve", lambda e: e.reciprocal(out, tmp[0]), [tmp[1]], W)

        DMA("sp", ident_f[:], c_ident, [], [b_const])
        DMA("sp", ones_f[:], c_ones, [], [b_const])
        DMA("sp", t1_f[:], c_t1, [], [b_const])
        DMA("sp", negc[:], c_negc, [], [b_const])
        DMA("sp", negs[:], c_negs, [], [b_const])
        DMA("sp", eoff[:], c_eoff, [], [b_const])
        DMA("sp", cpoff[:], c_cp, [], [b_const])
        DMA("pool", ident_b[:], c_ident, [], [b_const])
        DMA("pool", ones_b[:], c_ones, [], [b_const])
        DMA("pool", tri_b[:], c_tri, [], [b_const])
        P.barrier()

        bc_state = {}

        def bc_reg(e):
            if "r" not in bc_state:
                bc_state["r"] = e.alloc_register("bc")
                e.reg_mov(bc_state["r"], NE * CAP * 4 - 1)
            return bc_state["r"]

        def ln_epilogue(Pn, row0, hparts, hbufs, xsrc, xsrc_bufs, G, Bt, b_gb, dst, dst_bufs, work, want_xT):
            xo, b_xo, stt_, b_st, mv, b_mv, sc, b_sc = work
            DMA("sp", xo[0:Pn, :], xsrc[row0:row0 + Pn, :], xsrc_bufs, [b_xo])
            for n in range(2):
                STT(xo[0:Pn, n * 512:(n + 1) * 512], xo[0:Pn, n * 512:(n + 1) * 512], ALPHA, hparts[n],
                    ALU.mult, ALU.add, [b_xo, hbufs[n]], [b_xo])
            for n in range(2):
                P.op("dve", lambda e, n=n: e.bn_stats(stt_[0:Pn, n, :], xo[0:Pn, n * 512:(n + 1) * 512]), [b_xo], [b_st])
            P.op("dve", lambda e: e.bn_aggr(mv[0:Pn, :], stt_[0:Pn, :, :]), [b_st], [b_mv])
            rsqrt_(sc[0:Pn, 1:2], mv[0:Pn, 1:2], 1.0, 1e-5, [b_mv], [b_sc], (sc[0:Pn, 0:1], b_sc))
            TS("dve", xo[0:Pn, :], xo[0:Pn, :], mv[0:Pn, 0:1], ALU.subtract, [b_xo, b_mv, b_sc], [b_xo],
               s2=sc[0:Pn, 1:2], op1=ALU.mult)
            TT("pool", xo[0:Pn, :], xo[0:Pn, :], G[0:Pn, :], ALU.mult, [b_xo, b_gb], [b_xo])
            TT("pool", xo[0:Pn, :], xo[0:Pn, :], Bt[0:Pn, :], ALU.add, [b_xo, b_gb], [b_xo])
            DMA("sp", dst[row0:row0 + Pn, :], xo[0:Pn, :], [b_xo], dst_bufs)

        def load_bcast(dst, src_row, Pn, W):
            DMA("sp", dst[0:Pn, :], src_row.partition_broadcast(Pn), [], W)

        def load_xT(src, src_bufs, row0, ntok, xTg3, b_xTg, stg):
            for t in range(ntok // 128):
                st_, b_st_ = stg
                DMA("sp", st_[:, :], src[row0 + t * 128:row0 + (t + 1) * 128, :], src_bufs, [b_st_])
                for half in range(2):
                    bk, b_bk = bank()
                    for k in range(8):
                        TR(bk[:, k * 64:(k + 1) * 64], st_[half * 64:(half + 1) * 64, k * 128:(k + 1) * 128],
                           ident_f[half * 64:(half + 1) * 64, half * 64:(half + 1) * 64], [b_st_, b_const], [b_bk])
                    o = t * 128 + half * 64
                    CP("act", xTg3[:, :, o:o + 64], bk[:, 0:512].rearrange("p (k n) -> p k n", k=8), [b_bk], [b_xTg])

        def mixer(l, src, src_bufs_fn, dst, dst_bufs_fn):
            AR.reset()
            win, b_win = AR.bf16(8 * 3600)
            win3 = win.rearrange("p (k n) -> p k n", k=8)
            wout, b_wout = AR.bf16(8 * 1024)
            wout3 = wout.rearrange("p (k n) -> p k n", k=8)
            for k in range(8):
                DMA("pool", win3[:, k, :], w_in[l, k * 128:(k + 1) * 128, :], [], [b_win])
            DMA("pool", wout3, w_out[l].rearrange("(k p) n -> p k n", p=128), [], [b_wout])
            G, b_gb = AR.f32(1024)
            Bt, _ = AR.f32(1024)
            load_bcast(G, lng[0][l:l + 1, :], 64, [b_gb])
            load_bcast(Bt, lnb[0][l:l + 1, :], 64, [b_gb])
            mlnw, b_small = AR.f32(512)
            load_bcast(mlnw, ml_nw[l:l + 1, :], 64, [b_small])
            bif, _ = AR.f32(8)
            load_bcast(bif[:, 0:4], b_i[l:l + 1, :], 64, [b_small])
            load_bcast(bif[:, 4:8], b_f[l:l + 1, :], 64, [b_small])
            negA, _ = AR.f32(4)
            dtb, _ = AR.f32(4)
            load_bcast(negA, a_log[l:l + 1, :], 64, [b_small])
            load_bcast(dtb, dt_b[l:l + 1, :], 64, [b_small])
            ACT(negA[0:64, :], negA[0:64, :], AF.Exp, [b_small], [b_small])
            TS("dve", negA[0:64, :], negA[0:64, :], -1.0, ALU.mult, [b_small], [b_small])
            gdnw, _ = AR.f32(1)
            DMA("sp", gdnw[:, 0:1], gd_nw[l].rearrange("(p o) -> p o", o=1), [], [b_small])
            cwr, b_cwr = AR.f32(128)
            cw, _ = AR.f32(48)
            DMA("sp", cwr[0:48, :], conv_w[l].rearrange("j (c p) -> (j c) p", p=128), [], [b_cwr])
            bk, b_bk = bank()
            TR(bk[:, 0:48], cwr[0:48, :], ident_f[0:48, 0:48], [b_cwr, b_const], [b_bk])
            CP("dve", cw[:, :], bk[:, 0:48], [b_bk], [b_small])
            Cn, b_Cn = AR.f32(4 * 129)
            Cn3 = Cn[0:64, :].rearrange("p (h n) -> p h n", h=4)
            Cnb, b_Cnb = AR.bf16(4 * 130)
            Cnb3 = Cnb[0:64, 0:4 * 130].rearrange("p (h n) -> p h n", h=4)
            Sst, b_S = AR.f32(512)
            Sst3 = Sst.rearrange("p (h n) -> p h n", h=4)
            Sb, b_Sb = AR.bf16(512)
            Sb3 = Sb.rearrange("p (h n) -> p h n", h=4)
            carry, b_carry = AR.f32(36)
            carry3 = carry.rearrange("p (c n) -> p c n", c=12)
            P.op("pool", lambda e: e.memset(Cn[:, :], 0.0), [], [b_Cn])
            P.op("pool", lambda e: e.memset(Cnb[:, :], 0.0), [], [b_Cnb])
            P.op("pool", lambda e: e.memset(Sst[:, :], 0.0), [], [b_S])
            P.op("pool", lambda e: e.memset(Sb[:, :], 0.0), [], [b_Sb])
            P.op("pool", lambda e: e.memset(carry[:, :], 0.0), [], [b_carry])
            mlqT, b_mlqT = AR.bf16(4 * GT)
            mlqT3 = mlqT[0:64, :].rearrange("p (h n) -> p h n", h=4)
            mlkT, b_mlkT = AR.bf16(4 * GT)
            mlkT3 = mlkT[0:64, :].rearrange("p (h n) -> p h n", h=4)
            siluz, b_siluz = AR.f32(4 * GT)
            siluz3 = siluz.rearrange("p (h n) -> p h n", h=4)
            gdq, b_gdq = AR.bf16(4 * GT)
            gdq3 = gdq.rearrange("p (h n) -> p h n", h=4)
            gdk, b_gdk = AR.bf16(4 * GT)
            gdk3 = gdk.rearrange("p (h n) -> p h n", h=4)
            gdkf, b_gdkf = AR.f32(4 * GT)
            gdkf3 = gdkf.rearrange("p (h n) -> p h n", h=4)
            gdvf, b_gdvf = AR.f32(4 * GT)
            gdvf3 = gdvf.rearrange("p (h n) -> p h n", h=4)
            stage, b_stage = AR.f32(GT + 3)
            acc, b_acc = AR.f32(GT)
            post, b_post = AR.f32(GT)
            sq, b_sq = AR.f32(GT)
            rn, b_rn = AR.f32(GT)
            gates, b_gates = AR.f32(NCG * 16)
            gates3 = gates[0:64, :].rearrange("p (c n) -> p c n", c=NCG)
            gt, b_gt = AR.f32(NCG * 16)
            gt3 = gt[0:64, :].rearrange("p (c n) -> p c n", c=NCG)
            LG, b_LG = AR.f32(NCG * 8)
            LG3 = LG[0:64, :].rearrange("p (c n) -> p c n", c=NCG)
            ipre, b_ipre = AR.f32(NCG * 4)
            ipre3 = ipre[0:64, :].rearrange("p (c n) -> p c n", c=NCG)
            beta, b_beta = AR.f32(NCG * 4)
            beta3 = beta[0:64, :].rearrange("p (c n) -> p c n", c=NCG)
            CS, b_CS = AR.f32(NCG * 8)
            CS3 = CS[0:64, :].rearrange("p (c n) -> p c n", c=NCG)
            LA, b_LA = AR.f32(NCG * 8)
            LA3 = LA[0:64, :].rearrange("p (c n) -> p c n", c=NCG)
            EG128, b_EG = AR.f32(NCG * 4)
            EG3 = EG128.rearrange("p (c n) -> p c n", c=NCG)
            der, b_der = AR.f32(NCG * 24)
            der3 = der[0:64, :].rearrange("p (c n) -> p c n", c=NCG)
            mlk_tm, b_mlk_tm = AR.f32(256)
            mlv1, b_mlv1 = AR.bf16(4 * 130)
            mlv13 = mlv1[0:64, 0:4 * 130].rearrange("p (h n) -> p h n", h=4)
            P.op("pool", lambda e: e.memset(mlv1[:, :], 1.0), [], [b_mlv1])
            sigo, b_sigo = AR.f32(512)
            LcolF, b_Lcol = AR.f32(4 * 64)
            LcolF3 = LcolF[0:64, :].rearrange("p (h n) -> p h n", h=4)
            LcolG, _ = AR.f32(4 * 128)
            LcolG3 = LcolG[0:64, :].rearrange("p (h n) -> p h n", h=4)
            t44, b_t44 = AR.f32(256)
            t443 = t44[0:64, :].rearrange("p (h n) -> p h n", h=4)
            DTm, b_DT = AR.f32(256)
            DTm3 = DTm[0:64, :].rearrange("p (h n) -> p h n", h=4)
            Ef, b_Ef = AR.f32(256)
            Ef3 = Ef[0:64, :].rearrange("p (h n) -> p h n", h=4)
            DecS, b_DecS = AR.f32(256)
            DecS3 = DecS[0:64, :].rearrange("p (h n) -> p h n", h=4)
            DecT, b_DecT = AR.f32(256)
            DecT3 = DecT[0:64, :].rearrange("p (h n) -> p h n", h=4)
            E128, b_E128 = AR.f32(256)
            E1283 = E128.rearrange("p (h n) -> p h n", h=4)
            qtil, b_qtil = AR.bf16(256)
            qtil3 = qtil[0:64, :].rearrange("p (h n) -> p h n", h=4)
            gqt, b_gqt = AR.bf16(256)
            gqt3 = gqt.rearrange("p (h n) -> p h n", h=4)
            sint, b_sint = AR.bf16(256)
            sint3 = sint[0:64, :].rearrange("p (h n) -> p h n", h=4)
            rden, b_rden = AR.f32(8)
            hml, b_hml = AR.f32(512)
            hml3 = hml[0:64, :].rearrange("p (h n) -> p h n", h=4)
            hsq, b_hsq = AR.f32(512)
            hsq3 = hsq[0:64, :].rearrange("p (h n) -> p h n", h=4)
            ssum, b_ssum = AR.f32(8)
            kw, b_kw = AR.bf16(256)
            kw3 = kw[0:64, :].rearrange("p (h n) -> p h n", h=4)
            mixT, b_mixT = AR.bf16(8 * 64)
            mixT3 = mixT.rearrange("p (f n) -> p f n", f=8)
            bv, b_bv = AR.bf16(512)
            bv3 = bv[0:64, :].rearrange("p (h n) -> p h n", h=4)
            kbg, b_kbg = AR.bf16(512)
            kbg3 = kbg[0:64, :].rearrange("p (h n) -> p h n", h=4)
            kend, b_kend = AR.bf16(512)
            kend3 = kend[0:64, :].rearrange("p (h n) -> p h n", h=4)
            P0f, b_P0f = AR.f32(256)
            P0f3 = P0f[0:64, :].rearrange("p (h n) -> p h n", h=4)
            Pm = [AR.bf16(256) for _ in range(2)]
            PT = [AR.bf16(256) for _ in range(2)]
            Yb = [AR.bf16(256) for _ in range(2)]
            nwT, b_nwT = AR.bf16(256)
            nwT3 = nwT.rearrange("p (h n) -> p h n", h=4)
            vnew, b_vnew = AR.bf16(512)
            vnew3 = vnew[0:64, :].rearrange("p (h n) -> p h n", h=4)
            qkd, b_qkd = AR.bf16(256)
            qkd3 = qkd[0:64, :].rearrange("p (h n) -> p h n", h=4)
            osq, b_osq = AR.f32(256)
            orstd, b_orstd = AR.f32(256)
            ot1, b_ot1 = AR.f32(256)
            lnw = []
            for _ in range(2):
                a1, bb1 = AR.f32(1024)
                a2, bb2 = AR.f32(12)
                a3, bb3 = AR.f32(2)
                a4, bb4 = AR.f32(2)
                lnw.append((a1, bb1, a2[:, :].rearrange("p (a b) -> p a b", a=2), bb2, a3, bb3, a4, bb4))

            xTg, b_xTg = AR.bf16(8 * GT)
            xT3 = xTg.rearrange("p (k n) -> p k n", k=8)
            xstg = AR.f32(1024)
            for g in range(max_groups or (S // GT)):
                tok0 = 0
                xbufs = [b_xTg]
                load_xT(src, [bb for c in range(NCG) for bb in src_bufs_fn(g * NCG + c)], g * GT, GT, xT3, b_xTg, xstg)
                for qk in range(2):
                    for h in range(4):
                        col0 = (C_MLQ if qk == 0 else C_MLK) + h * 64
                        bk, b_bk = bank()
                        for k in range(8):
                            MM(bk[0:64, 0:GT], win3[:, k, col0:col0 + 64], xT3[:, k, 0:GT],
                               [b_win] + xbufs, [b_bk], start=(k == 0), stop=(k == 7))
                        if qk == 0:
                            ACT(mlqT3[:, h, :], bk[0:64, 0:GT], AF.Copy, [b_bk], [b_mlqT], scale=0.125)
                        else:
                            CP("dve", mlkT3[:, h, :], bk[0:64, 0:GT], [b_bk], [b_mlkT])
                for h in range(4):
                    bk, b_bk = bank()
                    for k in range(8):
                        MM(bk[:, 0:GT], win3[:, k, C_GDZ + h * 128:C_GDZ + (h + 1) * 128], xT3[:, k, 0:GT],
                           [b_win] + xbufs, [b_bk], start=(k == 0), stop=(k == 7))
                    ACT(siluz3[:, h, :], bk[:, 0:GT], AF.Silu, [b_bk], [b_siluz])
                for c in range(12):
                    bk, b_bk = bank()
                    for k in range(8):
                        MM(bk[:, 0:GT], win3[:, k, C_GDQ + c * 128:C_GDQ + (c + 1) * 128], xT3[:, k, 0:GT],
                           [b_win] + xbufs, [b_bk], start=(k == 0), stop=(k == 7))
                    CP("pool", stage[:, 0:3], carry3[:, c, :], [b_carry], [b_stage])
                    CP("act", stage[:, 3:GT + 3], bk[:, 0:GT], [b_bk], [b_stage])
                    CP("pool", carry3[:, c, :], stage[:, GT:GT + 3], [b_stage], [b_carry])
                    TS("dve", acc[:, :], stage[:, 0:GT], cw[:, c:c + 1], ALU.mult, [b_stage, b_small], [b_acc])
                    for j in range(1, 4):
                        STT(acc[:, :], stage[:, j:j + GT], cw[:, j * 12 + c:j * 12 + c + 1], acc[:, :], ALU.mult, ALU.add,
                            [b_stage, b_small, b_acc], [b_acc])
                    if c >= 8:
                        ACT(gdvf3[:, c - 8, :], acc[:, :], AF.Silu, [b_acc], [b_gdvf])
                    else:
                        ACT(post[:, :], acc[:, :], AF.Silu, [b_acc], [b_post])
                        TT("pool", sq[:, :], post[:, :], post[:, :], ALU.mult, [b_post], [b_sq])
                        bk2, b_bk2 = bank()
                        MM(bk2[:, 0:GT], ones_f[:, :], sq[:, :], [b_const, b_sq], [b_bk2])
                        rsqrt_(rn[:, :], bk2[:, 0:GT], 1.0, 1e-6, [b_bk2], [b_rn], (sq[:, :], b_sq))
                        if c < 4:
                            STT(gdq3[:, c, :], post[:, :], 128.0 ** -0.5, rn[:, :], ALU.mult, ALU.mult, [b_post, b_rn], [b_gdq])
                        else:
                            TT("dve", gdkf3[:, c - 4, :], post[:, :], rn[:, :], ALU.mult, [b_post, b_rn], [b_gdkf])
                            CP("pool", gdk3[:, c - 4, :], gdkf3[:, c - 4, :], [b_gdkf], [b_gdk])
                bkg, b_bkg = bank()
                for c in range(NCG):
                    t0 = c * 64
                    for part, col in ((0, C_MLI), (1, C_GDB)):
                        for k in range(8):
                            MM(bkg[0:64, c * 16 + part * 8:c * 16 + part * 8 + 8], xT3[:, k, t0:t0 + 64], win3[:, k, col:col + 8],
                               [b_win, b_xTg], [b_bkg], start=(k == 0), stop=(k == 7))
                CP("dve", gates[0:64, :], bkg[0:64, 0:NCG * 16], [b_bkg], [b_gates])
                TT("dve", gt3[:, :, 0:8], gates3[:, :, 0:8], bc3(bif[0:64, 0:8], [64, NCG, 8], 1), ALU.add, [b_gates, b_small], [b_gt])
                ACT(gt3[:, :, 0:8], gt3[:, :, 0:8], AF.Tanh, [b_gt], [b_gt], scale=1.0 / 15.0)
                TS("dve", ipre3[:, :, :], gt3[:, :, 0:4], 15.0, ALU.mult, [b_gt], [b_ipre])
                ACT(gt3[:, :, 4:8], gt3[:, :, 4:8], AF.Exp, [b_gt], [b_gt], scale=-15.0)
                ACT(gt3[:, :, 4:8], gt3[:, :, 4:8], AF.Ln, [b_gt], [b_gt], bias=1.0)
                TS("dve", LG3[:, :, 0:4], gt3[:, :, 4:8], -1.0, ALU.mult, [b_gt], [b_LG])
                ACT(beta3[:, :, :], gates3[:, :, 8:12], AF.Sigmoid, [b_gates], [b_beta])
                TT("dve", gt3[:, :, 8:12], gates3[:, :, 12:16], bc3(dtb[0:64, 0:4], [64, NCG, 4], 1), ALU.add, [b_gates, b_small], [b_gt])
                ACT(gt3[:, :, 12:16], gt3[:, :, 8:12], AF.Abs, [b_gt], [b_gt])
                ACT(gt3[:, :, 12:16], gt3[:, :, 12:16], AF.Exp, [b_gt], [b_gt], scale=-1.0)
                ACT(gt3[:, :, 12:16], gt3[:, :, 12:16], AF.Ln, [b_gt], [b_gt], bias=1.0)
                TS("dve", gt3[:, :, 8:12], gt3[:, :, 8:12], 0.0, ALU.max, [b_gt], [b_gt])
                TT("dve", gt3[:, :, 8:12], gt3[:, :, 8:12], gt3[:, :, 12:16], ALU.add, [b_gt], [b_gt])
                TT("dve", LG3[:, :, 4:8], gt3[:, :, 8:12], bc3(negA[0:64, 0:4], [64, NCG, 4], 1), ALU.mult, [b_gt, b_small], [b_LG])
                bkc, b_bkc = bank()
                MM(bkc[0:64, 0:NCG * 8], t1_f[:, :], LG[0:64, 0:NCG * 8], [b_const, b_LG], [b_bkc])
                MM(bkc[0:64, 64:64 + NCG * 8], ones_f[0:64, 0:64], LG[0:64, 0:NCG * 8], [b_const, b_LG], [b_bkc])
                MM(bkc[:, 128:128 + NCG * 8], ones_f[0:64, :], LG[0:64, 0:NCG * 8], [b_const, b_LG], [b_bkc])
                CP("dve", CS[0:64, :], bkc[0:64, 0:NCG * 8], [b_bkc], [b_CS])
                CP("dve", LA[0:64, :], bkc[0:64, 64:64 + NCG * 8], [b_bkc], [b_LA])
                ACT(EG3[:, :, :], bkc[:, 128:128 + NCG * 8].rearrange("p (c n) -> p c n", c=NCG)[:, :, 4:8], AF.Exp, [b_bkc], [b_EG])
                TT("dve", der3[:, :, 0:4], ipre3[:, :, :], CS3[:, :, 0:4], ALU.subtract, [b_ipre, b_CS], [b_der])
                TT("dve", der3[:, :, 4:8], der3[:, :, 0:4], LA3[:, :, 0:4], ALU.add, [b_der, b_LA], [b_der])
                ACT(der3[:, :, 4:8], der3[:, :, 4:8], AF.Exp, [b_der], [b_der])
                ACT(der3[:, :, 8:12], LA3[:, :, 0:4], AF.Exp, [b_LA], [b_der])
                ACT(der3[:, :, 12:16], CS3[:, :, 4:8], AF.Exp, [b_CS], [b_der])
                TT("dve", der3[:, :, 12:16], der3[:, :, 12:16], beta3[:, :, :], ALU.mult, [b_der, b_beta], [b_der])
                TT("dve", der3[:, :, 16:20], LA3[:, :, 4:8], CS3[:, :, 4:8], ALU.subtract, [b_LA, b_CS], [b_der])
                ACT(der3[:, :, 16:20], der3[:, :, 16:20], AF.Exp, [b_der], [b_der])
                TS("dve", der3[:, :, 20:24], beta3[:, :, :], -1.0, ALU.mult, [b_beta], [b_der])

                for c in range(NCG):
                    t0 = c * 64
                    lo = c * 64
                    ci = g * NCG + c
                    xb_ = [b_xTg]
                    bk, b_bk = bank()
                    for k in range(8):
                        MM(bk[0:64, 0:256], xT3[:, k, t0:t0 + 64], win3[:, k, C_MLK:C_MLK + 256], [b_win] + xb_, [b_bk],
                           start=(k == 0), stop=(k == 7))
                    CP("act", mlk_tm[0:64, :], bk[0:64, 0:256], [b_bk], [b_mlk_tm])
                    bk, b_bk = bank()
                    for k in range(8):
                        MM(bk[0:64, 0:512], xT3[:, k, t0:t0 + 64], win3[:, k, C_MLV:C_MLV + 512], [b_win] + xb_, [b_bk],
                           start=(k == 0), stop=(k == 7))
                    CP("dve", mlv13[:, :, 0:128], bk[0:64, 0:512].rearrange("p (h n) -> p h n", h=4), [b_bk], [b_mlv1])
                    bk, b_bk = bank()
                    for k in range(8):
                        MM(bk[0:64, 0:512], xT3[:, k, t0:t0 + 64], win3[:, k, C_MLO:C_MLO + 512], [b_win] + xb_, [b_bk],
                           start=(k == 0), stop=(k == 7))
                    ACT(sigo[0:64, :], bk[0:64, 0:512], AF.Sigmoid, [b_bk], [b_sigo])
                    CP("pool", LcolF3[:, :, :], bc3(LG3[:, c, 0:4], [64, 4, 64], 2), [b_LG], [b_Lcol])
                    CP("pool", LcolG3[:, :, :], bc3(LG3[:, c, 4:8], [64, 4, 128], 2), [b_LG], [b_Lcol])
                    bkM, b_bkM = bank()
                    for h in range(4):
                        MM(bkM[0:64, h * 64:(h + 1) * 64], LcolF3[:, h, :], t1_f[:, :], [b_Lcol, b_const], [b_bkM])
                    for h in range(4):
                        MM(bkM[0:64, 256 + h * 64:256 + (h + 1) * 64], LcolG3[:, h, 0:64], t1_f[:, :], [b_Lcol, b_const], [b_bkM])
                    bkE, b_bkE = bank()
                    for h in range(4):
                        MM(bkE[:, h * 64:(h + 1) * 64], LcolG3[:, h, :], t1_f[:, :], [b_Lcol, b_const], [b_bkE])
                    Mf = bkM[0:64, 0:256].rearrange("p (h n) -> p h n", h=4)
                    Mg = bkM[0:64, 256:512].rearrange("p (h n) -> p h n", h=4)
                    TT("dve", t443, Mf, bc3(der3[:, c, 0:4], [64, 4, 64], 2), ALU.add, [b_bkM, b_der], [b_t44])
                    TT("dve", t443, t443, bc3(negc[:, :], [64, 4, 64], 1), ALU.add, [b_t44, b_const], [b_t44])
                    ACT(DTm3, t443, AF.Exp, [b_t44], [b_DT])
                    ACT(Ef3, Mf, AF.Exp, [b_bkM], [b_Ef])
                    TT("dve", qtil3, mlqT3[:, :, lo:lo + 64], Ef3, ALU.mult, [b_mlqT, b_Ef], [b_qtil])
                    STT(t443, Mg, -1.0, bc3(CS3[:, c, 4:8], [64, 4, 64], 2), ALU.mult, ALU.add, [b_bkM, b_CS, b_t44], [b_t44])
                    TT("dve", t443, t443, bc3(negs[:, :], [64, 4, 64], 1), ALU.add, [b_t44, b_const], [b_t44])
                    ACT(DecS3, t443, AF.Exp, [b_t44], [b_DecS])
                    TT("dve", t443, Mg, bc3(CS3[:, c, 4:8], [64, 4, 64], 2), ALU.subtract, [b_bkM, b_CS, b_t44], [b_t44])
                    TT("dve", t443, t443, bc3(negc[:, :], [64, 4, 64], 1), ALU.add, [b_t44, b_const], [b_t44])
                    ACT(DecT3, t443, AF.Exp, [b_t44], [b_DecT])
                    ACT(E128[:, :], bkE[:, 0:256], AF.Exp, [b_bkE], [b_E128])
                    TT("dve", gqt3, gdq3[:, :, lo:lo + 64], E1283, ALU.mult, [b_gdq, b_E128], [b_gqt])

                    bk, b_bk = bank()
                    for h in range(4):
                        MM(bk[0:64, h * 64:(h + 1) * 64], mlkT3[:, h, lo:lo + 64], mlqT3[:, h, lo:lo + 64], [b_mlkT, b_mlqT], [b_bk])
                    TT("dve", sint3, bk[0:64, 0:256].rearrange("p (h n) -> p h n", h=4), DTm3, ALU.mult, [b_bk, b_DT], [b_sint])
                    for hp in range(2):
                        bk, b_bk = bank()
                        for j in range(2):
                            h = hp * 2 + j
                            MM(bk[0:64, j * 129:(j + 1) * 129], qtil3[:, h, :], Cnb3[:, h, 0:129], [b_qtil, b_Cnb], [b_bk], start=True, stop=False)
                            MM(bk[0:64, j * 129:(j + 1) * 129], sint3[:, h, :], mlv13[:, h, 0:129], [b_sint, b_mlv1], [b_bk], start=False, stop=True)
                        bv_ = bk[0:64, 0:258].rearrange("p (j n) -> p j n", j=2)
                        ACT(rden[0:64, hp * 2:hp * 2 + 2], bv_[:, :, 128], AF.Abs, [b_bk], [b_rden])
                        TS("dve", rden[0:64, hp * 2:hp * 2 + 2], rden[0:64, hp * 2:hp * 2 + 2], 1.0, ALU.max, [b_rden], [b_rden])
                        P.op("dve", lambda e, hp=hp: e.reciprocal(rden[0:64, 4 + hp * 2:4 + hp * 2 + 2], rden[0:64, hp * 2:hp * 2 + 2]), [b_rden], [b_rden])
                        TT("dve", hml3[:, hp * 2:hp * 2 + 2, :], bv_[:, :, 0:128], bc3(rden[0:64, 4 + hp * 2:4 + hp * 2 + 2], [64, 2, 128], 2),
                           ALU.mult, [b_bk, b_rden], [b_hml])
                    TT("pool", hsq3, hml3, hml3, ALU.mult, [b_hml], [b_hsq])
                    P.op("dve", lambda e: e.tensor_reduce(ssum[0:64, 0:4], hsq3, AX.X, ALU.add), [b_hsq], [b_ssum])
                    rsqrt_(ssum[0:64, 4:8], ssum[0:64, 0:4], 1.0 / 128.0, 1e-6, [b_ssum], [b_ssum], (ssum[0:64, 0:4], b_ssum))
                    TT("dve", hml3, hml3, bc3(ssum[0:64, 4:8], [64, 4, 128], 2), ALU.mult, [b_hml, b_ssum], [b_hml])
                    TT("pool", hml[0:64, :], hml[0:64, :], mlnw[0:64, :], ALU.mult, [b_hml, b_small], [b_hml])
                    TT("pool", hml[0:64, :], hml[0:64, :], sigo[0:64, :], ALU.mult, [b_hml, b_sigo], [b_hml])
                    bk, b_bk = bank()
                    for h in range(4):
                        TR(bk[:, h * 64:(h + 1) * 64], hml[0:64, h * 128:(h + 1) * 128], ident_f[0:64, 0:64], [b_hml, b_const], [b_bk])
                    CP("act", mixT3[:, 0:4, :], bk[:, 0:256].rearrange("p (h n) -> p h n", h=4), [b_bk], [b_mixT])
                    TT("dve", kw3, mlk_tm[0:64, :].rearrange("p (h n) -> p h n", h=4), bc3(der3[:, c, 4:8], [64, 4, 64], 2), ALU.mult,
                       [b_mlk_tm, b_der], [b_kw])
                    TT("dve", Cn3, Cn3, bc3(der3[:, c, 8:12], [64, 4, 129], 2), ALU.mult, [b_Cn, b_der], [b_Cn])
                    for hp in range(2):
                        bk, b_bk = bank()
                        for j in range(2):
                            h = hp * 2 + j
                            MM(bk[0:64, j * 129:(j + 1) * 129], kw3[:, h, :], mlv13[:, h, 0:129], [b_kw, b_mlv1], [b_bk])
                        TT("dve", Cn3[:, hp * 2:hp * 2 + 2, :], Cn3[:, hp * 2:hp * 2 + 2, :], bk[0:64, 0:258].rearrange("p (j n) -> p j n", j=2),
                           ALU.add, [b_Cn, b_bk], [b_Cn])
                    CP("pool", Cnb3[:, :, 0:129], Cn3, [b_Cn], [b_Cnb])

                    bkk, b_bkk = bank()
                    bkv, b_bkv = bank()
                    for h in range(4):
                        TR(bkk[0:64, h * 128:(h + 1) * 128], gdkf3[:, h, lo:lo + 64], ident_f[:, :], [b_gdkf, b_const], [b_bkk])
                    for h in range(4):
                        TR(bkv[0:64, h * 128:(h + 1) * 128], gdvf3[:, h, lo:lo + 64], ident_f[:, :], [b_gdvf, b_const], [b_bkv])
                    ktm = bkk[0:64, 0:512].rearrange("p (h n) -> p h n", h=4)
                    vtm = bkv[0:64, 0:512].rearrange("p (h n) -> p h n", h=4)
                    TT("dve", bv3, vtm, bc3(beta3[:, c, :], [64, 4, 128], 2), ALU.mult, [b_bkv, b_beta], [b_bv])
                    TT("dve", kbg3, ktm, bc3(der3[:, c, 12:16], [64, 4, 128], 2), ALU.mult, [b_bkk, b_der], [b_kbg])
                    TT("dve", kend3, ktm, bc3(der3[:, c, 16:20], [64, 4, 128], 2), ALU.mult, [b_bkk, b_der], [b_kend])
                    bk, b_bk = bank()
                    for h in range(4):
                        MM(bk[0:64, h * 64:(h + 1) * 64], gdk3[:, h, lo:lo + 64], gdk3[:, h, lo:lo + 64], [b_gdk], [b_bk])
                    TT("dve", t443, bk[0:64, 0:256].rearrange("p (h n) -> p h n", h=4), bc3(der3[:, c, 20:24], [64, 4, 64], 2), ALU.mult,
                       [b_bk, b_der, b_t44], [b_t44])
                    TT("dve", P0f3, t443, DecS3, ALU.mult, [b_t44, b_DecS], [b_P0f])
                    pm, b_pm = Pm[0]
                    pt, b_pt = PT[0]
                    yb, b_yb = Yb[0]
                    pm3 = pm[0:64, :].rearrange("p (h n) -> p h n", h=4)
                    CP("pool", pm[0:64, :], P0f[0:64, :], [b_P0f], [b_pm])
                    bk, b_bk = bank()
                    for h in range(4):
                        TR(bk[0:64, h * 64:(h + 1) * 64], P0f3[:, h, :], ident_f[0:64, 0:64], [b_P0f, b_const], [b_bk])
                    CP("act", pt[0:64, :], bk[0:64, 0:256], [b_bk], [b_pt])
                    TT("dve", yb[0:64, :].rearrange("p (h n) -> p h n", h=4), bk[0:64, 0:256].rearrange("p (h n) -> p h n", h=4),
                       bc3(ident_f[0:64, 0:64], [64, 4, 64], 1), ALU.add, [b_bk, b_const], [b_yb])
                    cur = 0
                    for lev in range(1, 6):
                        pm, b_pm = Pm[cur]
                        pt, b_pt = PT[cur]
                        yb, b_yb = Yb[cur]
                        pmn, b_pmn = Pm[1 - cur]
                        ptn, b_ptn = PT[1 - cur]
                        ybn, b_ybn = Yb[1 - cur]
                        v3 = lambda a: a[0:64, :].rearrange("p (h n) -> p h n", h=4)
                        bkA, b_bkA = bank()
                        for h in range(4):
                            MM(bkA[0:64, h * 64:(h + 1) * 64], v3(pt)[:, h, :], v3(pm)[:, h, :], [b_pt, b_pm], [b_bkA])
                        CP("act", pmn[0:64, :], bkA[0:64, 0:256], [b_bkA], [b_pmn])
                        if lev < 5:
                            bkB, b_bkB = bank()
                            for h in range(4):
                                MM(bkB[0:64, h * 64:(h + 1) * 64], v3(pm)[:, h, :], v3(pt)[:, h, :], [b_pt, b_pm], [b_bkB])
                            CP("dve", ptn[0:64, :], bkB[0:64, 0:256], [b_bkB], [b_ptn])
                        bkC, b_bkC = bank()
                        for h in range(4):
                            MM(bkC[0:64, h * 64:(h + 1) * 64], v3(pmn)[:, h, :], v3(yb)[:, h, :], [b_pmn, b_yb], [b_bkC])
                        TT("dve", ybn[0:64, :], yb[0:64, :], bkC[0:64, 0:256], ALU.add, [b_yb, b_bkC], [b_ybn])
                        cur = 1 - cur
                    RT, b_RT = Yb[cur]
                    RT3 = RT[0:64, :].rearrange("p (h n) -> p h n", h=4)
                    bk, b_bk = bank()
                    for h in range(4):
                        MM(bk[:, h * 64:(h + 1) * 64], kbg3[:, h, :], RT3[:, h, :], [b_kbg, b_RT], [b_bk])
                    ACT(nwT[:, :], bk[:, 0:256], AF.Copy, [b_bk], [b_nwT], scale=-1.0)
                    bk, b_bk = bank()
                    for h in range(4):
                        MM(bk[0:64, h * 128:(h + 1) * 128], RT3[:, h, :], bv3[:, h, :], [b_RT, b_bv], [b_bk], start=True, stop=False)
                        MM(bk[0:64, h * 128:(h + 1) * 128], nwT3[:, h, :], Sb3[:, h, :], [b_nwT, b_Sb], [b_bk], start=False, stop=True)
                    CP("act", vnew[0:64, :], bk[0:64, 0:512], [b_bk], [b_vnew])
                    bk, b_bk = bank()
                    for h in range(4):
                        MM(bk[0:64, h * 64:(h + 1) * 64], gdk3[:, h, lo:lo + 64], gdq3[:, h, lo:lo + 64], [b_gdk, b_gdq], [b_bk])
                    TT("dve", qkd3, bk[0:64, 0:256].rearrange("p (h n) -> p h n", h=4), DecT3, ALU.mult, [b_bk, b_DecT], [b_qkd])
                    bko, b_bko = bank()
                    for h in range(4):
                        MM(bko[:, h * 64:(h + 1) * 64], Sb3[:, h, :], gqt3[:, h, :], [b_Sb, b_gqt], [b_bko], start=True, stop=False)
                        MM(bko[:, h * 64:(h + 1) * 64], vnew3[:, h, :], qkd3[:, h, :], [b_vnew, b_qkd], [b_bko], start=False, stop=True)
                    bk, b_bk = bank()
                    for h in range(4):
                        MM(bk[:, h * 128:(h + 1) * 128], kend3[:, h, :], vnew3[:, h, :], [b_kend, b_vnew], [b_bk])
                    TT("dve", Sst3, Sst3, bc3(EG3[:, c, :], [128, 4, 128], 2), ALU.mult, [b_S, b_EG], [b_S])
                    TT("dve", Sst[:, :], Sst[:, :], bk[:, 0:512], ALU.add, [b_S, b_bk], [b_S])
                    CP("pool", Sb[:, :], Sst[:, :], [b_S], [b_Sb])
                    ACT(osq[:, :], bko[:, 0:256], AF.Square, [b_bko], [b_osq])
                    bk, b_bk = bank()
                    MM(bk[:, 0:256], ones_f[:, :], osq[:, :], [b_const, b_osq], [b_bk])
                    rsqrt_(orstd[:, :], bk[:, 0:256], 1.0 / 128.0, 1e-6, [b_bk], [b_orstd], (osq[:, :], b_osq))
                    TT("dve", ot1[:, :], bko[:, 0:256], orstd[:, :], ALU.mult, [b_bko, b_orstd], [b_ot1])
                    STT(mixT3[:, 4:8, :], ot1[:, :].rearrange("p (h n) -> p h n", h=4), gdnw[:, 0:1], siluz3[:, :, lo:lo + 64],
                        ALU.mult, ALU.mult, [b_ot1, b_small, b_siluz], [b_mixT])
                    hp_ = []
                    hb_ = []
                    for n in range(2):
                        bk, b_bk = bank()
                        for f in range(8):
                            MM(bk[0:64, 0:512], mixT3[:, f, :], wout3[:, f, n * 512:(n + 1) * 512], [b_mixT, b_wout], [b_bk],
                               start=(f == 0), stop=(f == 7))
                        hp_.append(bk[0:64, 0:512])
                        hb_.append(b_bk)
                    ln_epilogue(64, g * GT + c * 64, hp_, hb_, src, src_bufs_fn(ci), G, Bt, b_gb, dst, dst_bufs_fn(ci), lnw[ci % 2], False)
            P.barrier()


        def xattn(l):
            AR.reset()
            W = []
            for i in range(4):
                w, bw = AR.bf16(8 * 1024)
                w3 = w.rearrange("p (k n) -> p k n", k=8)
                DMA("pool", w3, xa_w[i][l].rearrange("(k p) n -> p k n", p=128), [], [bw])
                W.append((w3, bw))
            (wq3, b_wq), (wk3, b_wk), (wv3, b_wv), (wo3, b_wo) = W
            G, b_gb = AR.f32(1024)
            Bt, _ = AR.f32(1024)
            load_bcast(G, lng[1][l:l + 1, :], 128, [b_gb])
            load_bcast(Bt, lnb[1][l:l + 1, :], 128, [b_gb])
            memT, b_memT = AR.bf16(8 * 256)
            memT3 = memT.rearrange("p (k n) -> p k n", k=8)
            xstg = AR.f32(1024)
            load_xT(mem_in, [], 0, 256, memT3, b_memT, xstg)
            kT, b_kT = AR.bf16(8 * 256)
            kT3 = kT.rearrange("p (k n) -> p k n", k=8)
            for f in range(8):
                bk, b_bk = bank()
                for k in range(8):
                    MM(bk[:, 0:256], wk3[:, k, f * 128:(f + 1) * 128], memT3[:, k, :], [b_wk, b_memT], [b_bk], start=(k == 0), stop=(k == 7))
                CP("dve", kT3[:, f, :], bk[:, 0:256], [b_bk], [b_kT])
            vv, b_vv = AR.bf16(2 * 1024)
            vv3 = vv.rearrange("p (m n) -> p m n", m=2)
            for mc in range(2):
                for n in range(2):
                    bk, b_bk = bank()
                    for k in range(8):
                        MM(bk[:, 0:512], memT3[:, k, mc * 128:(mc + 1) * 128], wv3[:, k, n * 512:(n + 1) * 512], [b_wv, b_memT], [b_bk],
                           start=(k == 0), stop=(k == 7))
                    CP("act", vv3[:, mc, n * 512:(n + 1) * 512], bk[:, 0:512], [b_bk], [b_vv])
            XG = 512
            xTg, b_xTg = AR.bf16(8 * XG)
            xT3 = xTg.rearrange("p (k n) -> p k n", k=8)
            qT, b_qT = AR.bf16(8 * XG)
            qT3 = qT.rearrange("p (k n) -> p k n", k=8)
            expT = [AR.bf16(XG) for _ in range(2)]
            rdn, b_rdn = AR.f32(XG)
            oT, b_oT = AR.bf16(8 * XG)
            oT3 = oT.rearrange("p (k n) -> p k n", k=8)
            lnw = []
            for _ in range(2):
                a1, bb1 = AR.f32(1024)
                a2, bb2 = AR.f32(12)
                a3, bb3 = AR.f32(2)
                a4, bb4 = AR.f32(2)
                lnw.append((a1, bb1, a2[:, :].rearrange("p (a b) -> p a b", a=2), bb2, a3, bb3, a4, bb4))
            for g in range(S // XG):
                rb_ = [b_xres[g * 8 + c] for c in range(8)]
                load_xT(xres, rb_, g * XG, XG, xT3, b_xTg, xstg)
                for f in range(8):
                    bk, b_bk = bank()
                    for k in range(8):
                        MM(bk[:, 0:XG], wq3[:, k, f * 128:(f + 1) * 128], xT3[:, k, :], [b_wq, b_xTg], [b_bk], start=(k == 0), stop=(k == 7))
                    ACT(qT3[:, f, :], bk[:, 0:XG], AF.Copy, [b_bk], [b_qT], scale=1.0 / 16.0)
                for h in range(4):
                    for mc in range(2):
                        bk, b_bk = bank()
                        for j in range(2):
                            MM(bk[:, 0:XG], kT3[:, 2 * h + j, mc * 128:(mc + 1) * 128], qT3[:, 2 * h + j, :], [b_kT, b_qT], [b_bk],
                               start=(j == 0), stop=(j == 1))
                        ACT(expT[mc][0][:, :], bk[:, 0:XG], AF.Exp, [b_bk], [expT[mc][1]])
                    bd, b_bd = bank()
                    for mc in range(2):
                        MM(bd[:, 0:XG], ones_b[:, :], expT[mc][0][:, :], [b_const, expT[mc][1]], [b_bd], start=(mc == 0), stop=(mc == 1))
                    P.op("dve", lambda e, bd=bd: e.reciprocal(rdn[:, :], bd[:, 0:XG]), [b_bd], [b_rdn])
                    for j in range(2):
                        bk, b_bk = bank()
                        for mc in range(2):
                            MM(bk[:, 0:XG], vv3[:, mc, (2 * h + j) * 128:(2 * h + j + 1) * 128], expT[mc][0][:, :], [b_vv, expT[mc][1]], [b_bk],
                               start=(mc == 0), stop=(mc == 1))
                        TT("dve", oT3[:, 2 * h + j, :], bk[:, 0:XG], rdn[:, :], ALU.mult, [b_bk, b_rdn], [b_oT])
                for t in range(XG // 128):
                    hp_, hb_ = [], []
                    for n in range(2):
                        bk, b_bk = bank()
                        for f in range(8):
                            MM(bk[:, 0:512], oT3[:, f, t * 128:(t + 1) * 128], wo3[:, f, n * 512:(n + 1) * 512], [b_oT, b_wo], [b_bk],
                               start=(f == 0), stop=(f == 7))
                        hp_.append(bk[:, 0:512])
                        hb_.append(b_bk)
                    r0 = g * XG + t * 128
                    rb2 = [b_xres[r0 // 64], b_xres[r0 // 64 + 1]]
                    ln_epilogue(128, r0, hp_, hb_, xres, rb2, G, Bt, b_gb, xres, rb2, lnw[t % 2], False)
            P.barrier()

        def moe(l, dst, dst_bufs_fn):
            AR.reset()
            G, b_gb = AR.f32(1024)
            Bt, _ = AR.f32(1024)
            load_bcast(G, lng[2][l:l + 1, :], 128, [b_gb])
            load_bcast(Bt, lnb[2][l:l + 1, :], 128, [b_gb])
            dest = dest_t
            b_dest = Buf("dest")
            gate, b_gate = AR.f32(32 * 4)
            gate3 = gate.rearrange("p (t k) -> p t k", t=32)
            mark = AR.off
            rw, b_rw = AR.f32(8 * NE)
            rw3 = rw.rearrange("p (k e) -> p k e", k=8)
            DMA("sp", rw3, r_w[l].rearrange("(k p) e -> p k e", p=128), [], [b_rw])
            rb, _ = AR.f32(NE)
            load_bcast(rb, r_b[l:l + 1, :], 128, [b_rw])
            cnt, b_cnt = AR.f32(NE)
            P.op("pool", lambda e: e.memset(cnt[:, :], 0.0), [], [b_cnt])
            xs2 = [AR.f32(1024) for _ in range(2)]
            xT32, b_xT32 = AR.f32(8 * 128)
            xT323 = xT32.rearrange("p (k n) -> p k n", k=8)
            xbf = [(xbf_t[i], Buf("xbf%d" % i)) for i in range(2)]
            lg, b_lg = AR.f32(NE)
            mx8, b_mx8 = AR.f32(8)
            sm, b_sm = AR.f32(8)
            ex, b_ex = AR.f32(NE)
            msk, b_msk = AR.f32(NE)
            mskb, b_mskb = AR.bf16(NE)
            pos, b_pos = AR.f32(NE)
            ov, b_ov = AR.f32(NE)
            oh, b_oh = AR.f32(NE)
            tm, b_tm = AR.f32(NE)
            dsel, b_dsel = AR.f32(4)
            dsel16, b_dsel16 = AR.f32(16)
            b_xsall = Buf("xs_all")
            b_ysall = Buf("ys_all")
            for t in range(32):
                xs_, b_xs_ = xs2[t % 2]
                xb_, b_xb_ = xbf[t % 2]
                rbf = [b_xres[2 * t], b_xres[2 * t + 1]]
                DMA("sp", xs_[:, :], xres[t * 128:(t + 1) * 128, :], rbf, [b_xs_])
                for half in range(2):
                    bk, b_bk = bank()
                    for k in range(8):
                        TR(bk[:, k * 64:(k + 1) * 64], xs_[half * 64:(half + 1) * 64, k * 128:(k + 1) * 128],
                           ident_f[half * 64:(half + 1) * 64, half * 64:(half + 1) * 64], [b_xs_, b_const], [b_bk])
                    CP("dve", xT323[:, :, half * 64:(half + 1) * 64], bk[:, 0:512].rearrange("p (k n) -> p k n", k=8), [b_bk], [b_xT32])
                for cp in range(4):
                    CP("act", xb_[cp][:, :], xs_[:, cp * 256:(cp + 1) * 256], [b_xs_], [b_xb_])
                bl, b_bl = bank()
                for k in range(8):
                    MM(bl[:, 0:NE], xT323[:, k, :], rw3[:, k, :], [b_xT32, b_rw], [b_bl], start=(k == 0), stop=(k == 7))
                TT("dve", lg[:, :], bl[:, 0:NE], rb[:, :], ALU.add, [b_bl, b_rw], [b_lg])
                P.op("dve", lambda e: e.max(mx8[:, :], lg[:, :]), [b_lg], [b_mx8])
                TS("dve", sm[:, 0:1], mx8[:, 0:1], -1.0, ALU.mult, [b_mx8], [b_sm])
                ACT(ex[:, :], lg[:, :], AF.Exp, [b_lg, b_sm], [b_ex], bias=sm[:, 0:1])
                TS("dve", msk[:, :], lg[:, :], mx8[:, 3:4], ALU.is_ge, [b_lg, b_mx8], [b_msk])
                TT("dve", ex[:, :], ex[:, :], msk[:, :], ALU.mult, [b_ex, b_msk], [b_ex])
                P.op("dve", lambda e: e.tensor_reduce(sm[:, 1:2], ex[:, :], AX.X, ALU.add), [b_ex], [b_sm])
                P.op("dve", lambda e: e.reciprocal(sm[:, 2:3], sm[:, 1:2]), [b_sm], [b_sm])
                TS("dve", ex[:, :], ex[:, :], sm[:, 2:3], ALU.mult, [b_ex, b_sm], [b_ex])
                CP("pool", mskb[:, :], msk[:, :], [b_msk], [b_mskb])
                bp, b_bp = bank()
                MM(bp[:, 0:NE], tri_b[:, :], mskb[:, :], [b_const, b_mskb], [b_bp])
                MM(bp[:, NE:2 * NE], ones_b[:, :], mskb[:, :], [b_const, b_mskb], [b_bp])
                TT("dve", pos[:, :], bp[:, 0:NE], cnt[:, :], ALU.add, [b_bp, b_cnt], [b_pos])
                TT("dve", cnt[:, :], cnt[:, :], bp[:, NE:2 * NE], ALU.add, [b_bp, b_cnt], [b_cnt])
                TS("dve", ov[:, :], pos[:, :], float(CAP), ALU.is_ge, [b_pos], [b_ov])
                STT(pos[:, :], ov[:, :], 1.0e7, pos[:, :], ALU.mult, ALU.add, [b_ov, b_pos], [b_pos])
                TT("dve", pos[:, :], pos[:, :], eoff[:, :], ALU.add, [b_pos, b_const], [b_pos])
                TS("dve", ov[:, :], ov[:, :], -1.0, ALU.mult, [b_ov], [b_ov], s2=1.0, op1=ALU.add)
                TT("dve", ex[:, :], ex[:, :], ov[:, :], ALU.mult, [b_ex, b_ov], [b_ex])
                for k in range(4):
                    TS("dve", oh[:, :], lg[:, :], mx8[:, k:k + 1], ALU.is_equal, [b_lg, b_mx8], [b_oh])
                    TT("dve", tm[:, :], oh[:, :], pos[:, :], ALU.mult, [b_oh, b_pos], [b_tm])
                    P.op("dve", lambda e, k=k: e.tensor_reduce(dsel[:, k:k + 1], tm[:, :], AX.X, ALU.add), [b_tm], [b_dsel])
                    TT("dve", tm[:, :], oh[:, :], ex[:, :], ALU.mult, [b_oh, b_ex], [b_tm])
                    P.op("dve", lambda e, k=k, t=t: e.tensor_reduce(gate3[:, t, k:k + 1], tm[:, :], AX.X, ALU.add), [b_tm], [b_gate])
                TS("dve", dsel16[:, :].rearrange("p (k c) -> p k c", k=4), bc3(dsel[:, 0:4], [128, 4, 4], 2), 4.0, ALU.mult, [b_dsel], [b_dsel16])
                TT("dve", dsel16[:, :], dsel16[:, :], cpoff[:, :], ALU.add, [b_dsel16, b_const], [b_dsel16])
                CP("dve", dest[:, t * 16:t * 16 + 16], dsel16[:, :], [b_dsel16], [b_dest])
                for k in range(4):
                    for cp in range(4):
                        q = idx_ctr[0] % 8
                        idx_ctr[0] += 1
                        CP("dve", idx_t[q][:, :], dest[:, t * 16 + k * 4 + cp:t * 16 + k * 4 + cp + 1], [b_dest], [b_idx[q]])
                        P.dma("pool", lambda e, q=q, xb_=xb_, cp=cp: e.indirect_dma_start(
                            out=xs_d, out_offset=bass.IndirectOffsetOnAxis(ap=idx_t[q][:, :], axis=0),
                            in_=xb_[cp][:, :], in_offset=None, bounds_check=bc_reg(e), oob_is_err=False), [b_xb_, b_idx[q]], [b_xsall])
            P.barrier()
            AR.off = mark
            wgu = [AR.bf16(8 * 2048) for _ in range(2)]
            wdn = [AR.bf16(8 * 1024) for _ in range(2)]
            bdn = [AR.f32(1024) for _ in range(1)]
            bgr, b_bgr = AR.f32(128)
            bguT, b_bguT = AR.f32(NE * 16)
            for q in range(4):
                DMA("sp", bgr[:, :], b_gu[l].rearrange("e (c p) -> (e c) p", p=128)[q * 128:(q + 1) * 128, :], [], [b_bgr])
                bk, b_bk = bank()
                TR(bk[:, 0:128], bgr[:, :], ident_f[:, :], [b_bgr, b_const], [b_bk])
                CP("dve", bguT[:, q * 128:(q + 1) * 128], bk[:, 0:128], [b_bk], [b_bguT])
            xsel, b_xsel = AR.bf16(NCAP * 1024)
            xsel3 = xsel.rearrange("p (j d) -> p j d", j=NCAP)
            xselT, b_xselT = AR.bf16(8 * CAP)
            xselT3 = xselT.rearrange("p (k n) -> p k n", k=8)
            actT, b_actT = AR.bf16(8 * CAP)
            actT3 = actT.rearrange("p (k n) -> p k n", k=8)
            HC = CAP // 2
            gtmp, b_gtmp = AR.f32(HC)
            stmp, b_stmp = AR.f32(HC)
            ltmp, b_ltmp = AR.f32(HC)
            ysb = [AR.f32(1024) for _ in range(1)]
            for e_ in range(NE):
                wg, b_wg = wgu[e_ % 2]
                wg3 = wg.rearrange("p (k n) -> p k n", k=8)
                wd, b_wd = wdn[e_ % 2]
                wd3 = wd.rearrange("p (k n) -> p k n", k=8)
                bd_, b_bd_ = bdn[0]
                for k in range(8):
                    DMA("pool", wg3[:, k, :], w_gu[l, e_, k * 128:(k + 1) * 128, :], [], [b_wg])
                DMA("pool", wd3, w_dn[l, e_].rearrange("(k p) n -> p k n", p=128), [], [b_wd])
                DMA("sp", bd_[:, :], b_dn[l, e_:e_ + 1, :].partition_broadcast(128), [], [b_bd_])
                DMA("sp", xsel3, xs_d[e_ * CAP * 4:(e_ + 1) * CAP * 4, :].rearrange("(j p c) d -> p j (c d)", p=128, c=4), [b_xsall], [b_xsel])
                for j in range(NCAP):
                    for k in range(8):
                        TR(psb[:, k * 128:(k + 1) * 128], xsel3[:, j, k * 128:(k + 1) * 128], ident_b[:, :], [b_xsel, b_const], [b_psb])
                    CP("act", xselT3[:, :, j * 128:(j + 1) * 128], psb[:, :].rearrange("p (k n) -> p k n", k=8), [b_psb], [b_xselT])
                for f in range(8):
                    for hh in range(2):
                        bg, b_bg = bank()
                        for k in range(8):
                            MM(bg[:, 0:HC], wg3[:, k, f * 128:(f + 1) * 128], xselT3[:, k, hh * HC:(hh + 1) * HC], [b_wg, b_xselT], [b_bg],
                               start=(k == 0), stop=(k == 7))
                        bl, b_bl = bank()
                        for k in range(8):
                            MM(bl[:, 0:HC], wg3[:, k, 1024 + f * 128:1024 + (f + 1) * 128], xselT3[:, k, hh * HC:(hh + 1) * HC], [b_wg, b_xselT], [b_bl],
                               start=(k == 0), stop=(k == 7))
                        c0 = e_ * 16 + f
                        TS("dve", gtmp[:, :], bg[:, 0:HC], bguT[:, c0:c0 + 1], ALU.add, [b_bg, b_bguT], [b_gtmp], s2=7.0, op1=ALU.min)
                        ACT(stmp[:, :], gtmp[:, :], AF.Sigmoid, [b_gtmp], [b_stmp], scale=1.702)
                        TS("dve", ltmp[:, :], bl[:, 0:HC], bguT[:, c0 + 8:c0 + 9], ALU.add, [b_bl, b_bguT], [b_ltmp], s2=7.0, op1=ALU.min)
                        TS("dve", ltmp[:, :], ltmp[:, :], -7.0, ALU.max, [b_ltmp], [b_ltmp], s2=1.0, op1=ALU.add)
                        TT("pool", gtmp[:, :], gtmp[:, :], stmp[:, :], ALU.mult, [b_gtmp, b_stmp], [b_gtmp])
                        TT("pool", actT3[:, f, hh * HC:(hh + 1) * HC], gtmp[:, :], ltmp[:, :], ALU.mult, [b_gtmp, b_ltmp], [b_actT])
                for j in range(NCAP):
                    ys_, b_ys_ = ysb[0]
                    for n in range(2):
                        bk, b_bk = bank()
                        for f in range(8):
                            MM(bk[:, 0:512], actT3[:, f, j * 128:(j + 1) * 128], wd3[:, f, n * 512:(n + 1) * 512], [b_actT, b_wd], [b_bk],
                               start=(f == 0), stop=(f == 7))
                        TT("dve", ys_[:, n * 512:(n + 1) * 512], bk[:, 0:512], bd_[:, n * 512:(n + 1) * 512], ALU.add, [b_bk, b_bd_], [b_ys_])
                    DMA("sp", ys_d[(e_ * CAP + j * 128) * 4:(e_ * CAP + (j + 1) * 128) * 4, :].rearrange("(p c) d -> p (c d)", c=4), ys_[:, :], [b_ys_], [b_ysall])
            P.barrier()
            AR.off = mark
            gth = [(gth_t[i], Buf("gth%d" % i)) for i in range(4)]
            for k in range(4):
                for cp in range(4):
                    P.op("pool", lambda e, k=k, cp=cp: e.memset(gth[k][0][cp][:, :], 0.0), [], [gth[k][1]])
            accs = [AR.f32(1024) for _ in range(2)]
            lnw = []
            for _ in range(2):
                a1, bb1 = AR.f32(1024)
                a2, bb2 = AR.f32(12)
                a3, bb3 = AR.f32(2)
                a4, bb4 = AR.f32(2)
                lnw.append((a1, bb1, a2[:, :].rearrange("p (a b) -> p a b", a=2), bb2, a3, bb3, a4, bb4))
            for t in range(32):
                for k in range(4):
                    for cp in range(4):
                        q = idx_ctr[0] % 8
                        idx_ctr[0] += 1
                        CP("dve", idx_t[q][:, :], dest[:, t * 16 + k * 4 + cp:t * 16 + k * 4 + cp + 1], [b_dest], [b_idx[q]])
                        P.dma("pool", lambda e, k=k, q=q, cp=cp: e.indirect_dma_start(
                            out=gth[k][0][cp][:, :], out_offset=None, in_=ys_d,
                            in_offset=bass.IndirectOffsetOnAxis(ap=idx_t[q][:, :], axis=0),
                            bounds_check=bc_reg(e), oob_is_err=False), [b_ysall, b_idx[q]], [gth[k][1]])
                ac, b_ac = accs[t % 2]
                for cp in range(4):
                    cs_ = slice(cp * 256, (cp + 1) * 256)
                    TS("dve", ac[:, cs_], gth[0][0][cp][:, :], gate3[:, t, 0:1], ALU.mult, [gth[0][1], b_gate], [b_ac])
                    for k in range(1, 4):
                        STT(ac[:, cs_], gth[k][0][cp][:, :], gate3[:, t, k:k + 1], ac[:, cs_], ALU.mult, ALU.add, [gth[k][1], b_gate, b_ac], [b_ac])
                rb2 = [b_xres[2 * t], b_xres[2 * t + 1]]
                ln_epilogue(128, t * 128, [ac[:, 0:512], ac[:, 512:1024]], [b_ac, b_ac], xres, rb2, G, Bt, b_gb, dst, dst_bufs_fn(t), lnw[t % 2], False)
            P.barrier()

        for l in range(n_layers):
            if "mixer" in skip:
                AR.reset()
                cpb = [AR.f32(1024) for _ in range(2)]
                for t in range(32):
                    a, bb = cpb[t % 2]
                    DMA("sp", a[:, :], x_in[t * 128:(t + 1) * 128, :], [], [bb])
                    DMA("sp", xres[t * 128:(t + 1) * 128, :], a[:, :], [bb], [b_xres[2 * t], b_xres[2 * t + 1]])
                P.barrier()
            elif l == 0:
                mixer(l, x_in, lambda ci: [], xres, lambda ci: [b_xres[ci]])
            else:
                mixer(l, xres, lambda ci: [b_xres[ci]], xres, lambda ci: [b_xres[ci]])
            if stop_after == (l, "mixer"):
                break
            if "xattn" not in skip:
                xattn(l)
            if stop_after == (l, "xattn"):
                break
            last = (l == n_layers - 1) and stop_after is None
            if last:
                moe(l, out_d, lambda t: [b_out])
            else:
                moe(l, xres, lambda t: [b_xres[2 * t], b_xres[2 * t + 1]])
            if stop_after == (l, "moe"):
                break
        if stop_after is not None:
            AR.reset()
            fin = [AR.f32(1024) for _ in range(2)]
            for t in range(32):
                a, bb = fin[t % 2]
                DMA("sp", a[:, :], xres[t * 128:(t + 1) * 128, :], [b_xres[2 * t], b_xres[2 * t + 1]], [bb])
                DMA("sp", out_d[t * 128:(t + 1) * 128, :], a[:, :], [bb], [b_out])
        P.barrier()
        print("ops:", P.n_ops, {e: len(s) for e, s in P.streams.items()})
        P.emit()
    return nc


def make_consts():
    i = np.arange(64)
    c = {
        "c_ident": np.eye(128, dtype=np.float32),
        "c_t1": (i[:, None] <= i[None, :]).astype(np.float32),
        "c_negc": np.where(i[None, :] >= i[:, None], 0.0, NEG).astype(np.float32),
        "c_negs": np.where(i[None, :] < i[:, None], 0.0, NEG).astype(np.float32),
        "c_tri": (np.arange(128)[:, None] < np.arange(128)[None, :]).astype(np.float32),
        "c_eoff": np.tile((np.arange(NE) * CAP).astype(np.float32)[None, :], (128, 1)),
        "c_ones": np.ones((128, 128), np.float32),
        "c_cp": np.tile(np.arange(4, dtype=np.float32)[None, :], (128, 4)),
    }
    return c


_NAMES = ["w_in", "mlstm_b_i", "mlstm_b_f", "mlstm_norm_w", "gdn_conv_w", "gdn_a_log", "gdn_dt_bias", "gdn_norm_w", "w_out",
          "ln1_g", "ln1_b", "xa_wq", "xa_wk", "xa_wv", "xa_wo", "ln2_g", "ln2_b", "router_w", "router_b",
          "exp_w_gu", "exp_b_gu", "exp_w_down", "exp_b_down", "ln3_g", "ln3_b"]


def run(inputs, n_layers=L, stop_after=None, cores=8, max_groups=None, small=False, skip=()):
    nc = build(n_layers, stop_after, max_groups=max_groups, small=small, skip=skip)
    consts = make_consts()
    shared = {k: np.ascontiguousarray(np.asarray(inputs[k], dtype=np.float32)) for k in _NAMES}
    if small:
        names = small if isinstance(small, (set, tuple, list)) else ("xa_wq", "xa_wk", "xa_wv", "xa_wo", "router_w", "exp_w_gu", "exp_b_gu", "exp_w_down", "exp_b_down")
        for k in names:
            shared[k] = np.zeros([1] * shared[k].ndim, np.float32)
    in_maps = []
    for c in range(cores):
        m = dict(shared)
        m.update(consts)
        m["x"] = np.ascontiguousarray(np.asarray(inputs["x"][c], dtype=np.float32))
        m["mem"] = np.ascontiguousarray(np.asarray(inputs["mem"][c], dtype=np.float32))
        in_maps.append(m)
    res = run_bass_kernel_spmd(nc, in_maps, core_ids=list(range(cores)))
    return np.stack([np.asarray(r["out"]) for r in res.results], axis=0)


def kernel(**inputs):
    return run(inputs).astype(np.float32)
```

```python
import numpy as np
from contextlib import ExitStack
import concourse.bass as bass
import concourse.mybir as mybir
from concourse.bass_utils import run_bass_kernel_spmd

F32 = mybir.dt.float32
BF16 = mybir.dt.bfloat16
I32 = mybir.dt.int32
AF = mybir.ActivationFunctionType
ALU = mybir.AluOpType
AX = mybir.AxisListType

S = 4096
D = 1024
L = 4
NE = 32
CAP = 1024
NCAP = CAP // 128
ALPHA = (2.0 * L) ** 0.25
NEG = -30000.0
GT = 256
NCG = GT // 64

import os as _os
SAME_ENGINE_SYNC = _os.environ.get("SES", "1") == "1"
EPOCH = 16000
N_DMA_SEMS = 24

C_MLQ, C_MLK, C_MLV, C_MLO, C_MLI, C_MLF = 0, 256, 512, 1024, 1536, 1540
C_GDQ, C_GDK, C_GDV, C_GDZ, C_GDB, C_GDA = 1544, 2056, 2568, 3080, 3592, 3596


class Buf:
    __slots__ = ("name", "w", "r", "excl")

    def __init__(self, name, excl=False):
        self.name = name
        self.w = None
        self.r = []
        self.excl = excl


class Prog:
    ENGS = ("pe", "dve", "act", "pool", "sp")

    def __init__(self, nc, es):
        self.nc = nc
        self.es = es
        self.streams = {e: [] for e in self.ENGS}
        self.count = {e: 0 for e in self.ENGS}
        self.known = {e: {} for e in self.ENGS}
        self.sems = {}
        self.dma_next = {e: 0 for e in self.ENGS}
        self.dma_uses = {}
        self.n_ops = 0

    def sem(self, key):
        s = self.sems.get(key)
        if s is None:
            s = self.es.enter_context(self.nc.semaphore("s_%s_%s" % key))
            self.sems[key] = s
        return s

    def _wait(self, eng, tok):
        key, val = tok
        if key[0] == eng and (eng == "pe" or not SAME_ENGINE_SYNC):
            return
        if self.known[eng].get(key, 0) >= val:
            return
        self.known[eng][key] = val
        self.streams[eng].append(("wait", key, val))

    def _deps(self, eng, reads, writes):
        for b in reads:
            if b.w is not None:
                self._wait(eng, b.w)
        for b in writes:
            if b.w is not None:
                self._wait(eng, b.w)
            for t in b.r:
                self._wait(eng, t)

    def _commit(self, tok, reads, writes):
        for b in reads:
            b.r.append(tok)
        for b in writes:
            b.w = tok
            b.r = []

    def op(self, eng, fn, reads=(), writes=()):
        ex = [b for b in reads if b.excl]
        if ex:
            reads = [b for b in reads if not b.excl]
            writes = list(writes) + ex
        self._deps(eng, reads, writes)
        n = self.count[eng]
        self.count[eng] = n + 1
        key = (eng, n // EPOCH)
        self.sem(key)
        tok = (key, n % EPOCH + 1)
        self.streams[eng].append(("op", fn, key, 1))
        self._commit(tok, reads, writes)
        self.n_ops += 1
        return tok

    def dma(self, eng, fn, reads=(), writes=()):
        self._deps(eng, reads, writes)
        i = self.dma_next[eng]
        self.dma_next[eng] = (i + 1) % N_DMA_SEMS
        key = ("d" + eng, i)
        uses = self.dma_uses.get(key, 0)
        self.sem(key)
        if uses > 0:
            self._wait(eng, (key, 16 * uses))
        self.dma_uses[key] = uses + 1
        tok = (key, 16 * (uses + 1))
        self.streams[eng].append(("op", fn, key, 16))
        self._commit(tok, reads, writes)
        self.n_ops += 1
        return tok

    def barrier(self):
        toks = []
        for e in self.ENGS:
            n = self.count[e]
            if n > 0:
                toks.append(((e, (n - 1) // EPOCH), (n - 1) % EPOCH + 1))
        for k, u in self.dma_uses.items():
            toks.append((k, 16 * u))
        for e in self.ENGS:
            for t in toks:
                if t[0][0] == e:
                    continue
                self._wait(e, t)

    def emit(self):
        nc = self.nc
        engmap = {"pe": "tensor", "dve": "vector", "act": "scalar", "pool": "gpsimd", "sp": "sync"}
        with nc.Block() as block:
            for e in self.ENGS:
                stream = self.streams[e]
                if not stream:
                    continue

                def body(engine, stream=stream):
                    for it in stream:
                        if it[0] == "wait":
                            engine.wait_ge(self.sems[it[1]], it[2])
                        else:
                            it[1](engine).then_inc(self.sems[it[2]], it[3])

                getattr(block, engmap[e])(body)


class Arena:
    def __init__(self, t, ncols):
        self.t = t
        self.n = ncols
        self.off = 0
        self.k = 0

    def reset(self):
        self.off = 0

    def _take(self, c32):
        assert self.off + c32 <= self.n, ("arena overflow", self.off, c32, self.n)
        a = self.t[:, self.off:self.off + c32]
        self.off += c32
        self.k += 1
        return a, Buf("ar%d" % self.k)

    def f32(self, cols):
        return self._take(cols)

    def bf16(self, cols):
        a, b = self._take((cols + 1) // 2)
        return a.bitcast(BF16), b

    def i32(self, cols):
        a, b = self._take(cols)
        return a.bitcast(I32), b


def bc3(ap, shape, axis):
    return ap.unsqueeze(axis).to_broadcast(list(shape))


def build(n_layers=L, stop_after=None, dbg=False, max_groups=None, small=False, skip=()):
    nc = bass.Bass("TRN2", target_bir_lowering=False)
    es = ExitStack()

    BIG = ("xa_wq", "xa_wk", "xa_wv", "xa_wo", "router_w", "exp_w_gu", "exp_b_gu", "exp_w_down", "exp_b_down")

    def din(name, shape, dt=F32):
        if small and name in (small if isinstance(small, (set, tuple, list)) else BIG):
            shape = [1] * len(shape)
        return nc.dram_tensor(name, list(shape), dt, kind="ExternalInput").ap()

    x_in = din("x", [S, D])
    mem_in = din("mem", [256, D])
    w_in = din("w_in", [L, D, 3600])
    b_i = din("mlstm_b_i", [L, 4])
    b_f = din("mlstm_b_f", [L, 4])
    ml_nw = din("mlstm_norm_w", [L, 512])
    conv_w = din("gdn_conv_w", [L, 4, 1536])
    a_log = din("gdn_a_log", [L, 4])
    dt_b = din("gdn_dt_bias", [L, 4])
    gd_nw = din("gdn_norm_w", [L, 128])
    w_out = din("w_out", [L, D, D])
    lng = [din("ln%d_g" % i, [L, D]) for i in (1, 2, 3)]
    lnb = [din("ln%d_b" % i, [L, D]) for i in (1, 2, 3)]
    xa_w = [din("xa_w" + n, [L, D, D]) for n in "qkvo"]
    r_w = din("router_w", [L, D, NE])
    r_b = din("router_b", [L, NE])
    w_gu = din("exp_w_gu", [L, NE, D, 2 * D])
    b_gu = din("exp_b_gu", [L, NE, 2 * D])
    w_dn = din("exp_w_down", [L, NE, D, D])
    b_dn = din("exp_b_down", [L, NE, D])
    c_ident = din("c_ident", [128, 128])
    c_t1 = din("c_t1", [64, 64])
    c_negc = din("c_negc", [64, 64])
    c_negs = din("c_negs", [64, 64])
    c_tri = din("c_tri", [128, 128])
    c_eoff = din("c_eoff", [128, NE])
    c_ones = din("c_ones", [128, 128])
    c_cp = din("c_cp", [128, 16])
    out_d = nc.dram_tensor("out", [S, D], F32, kind="ExternalOutput").ap()
    xres = nc.dram_tensor("xres", [S, D], F32, kind="Internal").ap()
    xs_d = nc.dram_tensor("xs_d", [NE * CAP * 4, 256], BF16, kind="Internal").ap()
    ys_d = nc.dram_tensor("ys_d", [NE * CAP * 4, 256], F32, kind="Internal").ap()
    b_xres = [Buf("xres%d" % i) for i in range(64)]
    b_out = Buf("out")
    b_xs = [Buf("xs%d" % e) for e in range(NE)]
    b_ys = [Buf("ys%d" % e) for e in range(NE)]

    P = Prog(nc, es)

    def sb(name, shape, dt):
        return es.enter_context(nc.sbuf_tensor(name, list(shape), dt))

    with es:
        ident_f = sb("ident_f", [128, 128], F32)
        ident_b = sb("ident_b", [128, 128], BF16)
        ones_f = sb("ones_f", [128, 128], F32)
        ones_b = sb("ones_b", [128, 128], BF16)
        tri_b = sb("tri_b", [128, 128], BF16)
        t1_f = sb("t1_f", [64, 64], F32)
        negc = sb("negc", [64, 64], F32)
        negs = sb("negs", [64, 64], F32)
        eoff = sb("eoff", [128, NE], F32)
        dest_t = sb("dest_t", [128, 512], I32)
        cpoff = sb("cpoff", [128, 16], F32)
        idx_t = [sb("idx%d" % i, [128, 1], I32) for i in range(8)]
        b_idx = [Buf("idx%d" % i) for i in range(8)]
        idx_ctr = [0]
        xbf_t = [[sb("xbf%d_%d" % (i, c), [128, 256], BF16) for c in range(4)] for i in range(2)]
        gth_t = [[sb("gth%d_%d" % (i, c), [128, 256], F32) for c in range(4)] for i in range(4)]
        b_const = Buf("const")
        ARENA_COLS = 44000
        arena_t = sb("arena", [128, ARENA_COLS], F32)
        AR = Arena(arena_t, ARENA_COLS)
        banks = [es.enter_context(nc.psum_tensor("ps%d" % i, [128, 512], F32)) for i in range(7)]
        b_banks = [Buf("bank%d" % i, True) for i in range(7)]
        psb = es.enter_context(nc.psum_tensor("psb", [128, 1024], BF16))
        b_psb = Buf("psb", True)
        bank_ctr = [0]

        def bank():
            i = bank_ctr[0] % 7
            bank_ctr[0] += 1
            return banks[i], b_banks[i]

        def MM(out, lhsT, rhs, R, W, start=True, stop=True):
            P.op("pe", lambda e: e.matmul(out, lhsT, rhs, start=start, stop=stop), R, W)

        def TR(out, in_, idn, R, W):
            P.op("pe", lambda e: e.transpose(out, in_, idn), R, W)

        def TT(eng, out, a, b, op, R, W):
            P.op(eng, lambda e: e.tensor_tensor(out, a, b, op), R, W)

        def TS(eng, out, a, s1, op0, R, W, s2=None, op1=None):
            if op1 is None:
                P.op(eng, lambda e: e.tensor_scalar(out, a, s1, None, op0), R, W)
            else:
                P.op(eng, lambda e: e.tensor_scalar(out, a, s1, s2, op0, op1), R, W)

        def STT(out, a, s, b, op0, op1, R, W):
            P.op("dve", lambda e: e.scalar_tensor_tensor(out, a, s, b, op0, op1), R, W)

        def ACT(out, in_, func, R, W, bias=0.0, scale=1.0):
            P.op("act", lambda e: e.activation(out, in_, func, bias=bias, scale=scale), R, W)

        def CP(eng, out, in_, R, W):
            if eng == "act":
                P.op("act", lambda e: e.copy(out, in_), R, W)
            else:
                P.op(eng, lambda e: e.tensor_copy(out, in_), R, W)

        def DMA(eng, out, in_, R, W):
            return P.dma(eng, lambda e: e.dma_start(out=out, in_=in_), R, W)

        def rsqrt_(out, in_, scale, eps, R, W, tmp):
            ACT(tmp[0], in_, AF.Sqrt, R, [tmp[1]], bias=eps, scale=scale)
            P.op("dve", lambda e: e.reciprocal(out, tmp[0]), [tmp[1]], W)

        DMA("sp", ident_f[:], c_ident, [], [b_const])
        DMA("sp", ones_f[:], c_ones, [], [b_const])
        DMA("sp", t1_f[:], c_t1, [], [b_const])
        DMA("sp", negc[:], c_negc, [], [b_const])
        DMA("sp", negs[:], c_negs, [], [b_const])
        DMA("sp", eoff[:], c_eoff, [], [b_const])
        DMA("sp", cpoff[:], c_cp, [], [b_const])
        DMA("pool", ident_b[:], c_ident, [], [b_const])
        DMA("pool", ones_b[:], c_ones, [], [b_const])
        DMA("pool", tri_b[:], c_tri, [], [b_const])
        P.barrier()

        bc_state = {}

        def bc_reg(e):
            if "r" not in bc_state:
                bc_state["r"] = e.alloc_register("bc")
                e.reg_mov(bc_state["r"], NE * CAP * 4 - 1)
            return bc_state["r"]

        def ln_epilogue(Pn, row0, hparts, hbufs, xsrc, xsrc_bufs, G, Bt, b_gb, dst, dst_bufs, work, want_xT):
            xo, b_xo, stt_, b_st, mv, b_mv, sc, b_sc = work
            DMA("sp", xo[0:Pn, :], xsrc[row0:row0 + Pn, :], xsrc_bufs, [b_xo])
            for n in range(2):
                STT(xo[0:Pn, n * 512:(n + 1) * 512], xo[0:Pn, n * 512:(n + 1) * 512], ALPHA, hparts[n],
                    ALU.mult, ALU.add, [b_xo, hbufs[n]], [b_xo])
            for n in range(2):
                P.op("dve", lambda e, n=n: e.bn_stats(stt_[0:Pn, n, :], xo[0:Pn, n * 512:(n + 1) * 512]), [b_xo], [b_st])
            P.op("dve", lambda e: e.bn_aggr(mv[0:Pn, :], stt_[0:Pn, :, :]), [b_st], [b_mv])
            rsqrt_(sc[0:Pn, 1:2], mv[0:Pn, 1:2], 1.0, 1e-5, [b_mv], [b_sc], (sc[0:Pn, 0:1], b_sc))
            TS("dve", xo[0:Pn, :], xo[0:Pn, :], mv[0:Pn, 0:1], ALU.subtract, [b_xo, b_mv, b_sc], [b_xo],
               s2=sc[0:Pn, 1:2], op1=ALU.mult)
            TT("pool", xo[0:Pn, :], xo[0:Pn, :], G[0:Pn, :], ALU.mult, [b_xo, b_gb], [b_xo])
            TT("pool", xo[0:Pn, :], xo[0:Pn, :], Bt[0:Pn, :], ALU.add, [b_xo, b_gb], [b_xo])
            DMA("sp", dst[row0:row0 + Pn, :], xo[0:Pn, :], [b_xo], dst_bufs)

        def load_bcast(dst, src_row, Pn, W):
            DMA("sp", dst[0:Pn, :], src_row.partition_broadcast(Pn), [], W)

        def load_xT(src, src_bufs, row0, ntok, xTg3, b_xTg, stg):
            for t in range(ntok // 128):
                st_, b_st_ = stg
                DMA("sp", st_[:, :], src[row0 + t * 128:row0 + (t + 1) * 128, :], src_bufs, [b_st_])
                for half in range(2):
                    bk, b_bk = bank()
                    for k in range(8):
                        TR(bk[:, k * 64:(k + 1) * 64], st_[half * 64:(half + 1) * 64, k * 128:(k + 1) * 128],
                           ident_f[half * 64:(half + 1) * 64, half * 64:(half + 1) * 64], [b_st_, b_const], [b_bk])
                    o = t * 128 + half * 64
                    CP("act", xTg3[:, :, o:o + 64], bk[:, 0:512].rearrange("p (k n) -> p k n", k=8), [b_bk], [b_xTg])

        def mixer(l, src, src_bufs_fn, dst, dst_bufs_fn):
            AR.reset()
            win, b_win = AR.bf16(8 * 3600)
            win3 = win.rearrange("p (k n) -> p k n", k=8)
            wout, b_wout = AR.bf16(8 * 1024)
            wout3 = wout.rearrange("p (k n) -> p k n", k=8)
            for k in range(8):
                DMA("pool", win3[:, k, :], w_in[l, k * 128:(k + 1) * 128, :], [], [b_win])
            DMA("pool", wout3, w_out[l].rearrange("(k p) n -> p k n", p=128), [], [b_wout])
            G, b_gb = AR.f32(1024)
            Bt, _ = AR.f32(1024)
            load_bcast(G, lng[0][l:l + 1, :], 64, [b_gb])
            load_bcast(Bt, lnb[0][l:l + 1, :], 64, [b_gb])
            mlnw, b_small = AR.f32(512)
            load_bcast(mlnw, ml_nw[l:l + 1, :], 64, [b_small])
            bif, _ = AR.f32(8)
            load_bcast(bif[:, 0:4], b_i[l:l + 1, :], 64, [b_small])
            load_bcast(bif[:, 4:8], b_f[l:l + 1, :], 64, [b_small])
            negA, _ = AR.f32(4)
            dtb, _ = AR.f32(4)
            load_bcast(negA, a_log[l:l + 1, :], 64, [b_small])
            load_bcast(dtb, dt_b[l:l + 1, :], 64, [b_small])
            ACT(negA[0:64, :], negA[0:64, :], AF.Exp, [b_small], [b_small])
            TS("dve", negA[0:64, :], negA[0:64, :], -1.0, ALU.mult, [b_small], [b_small])
            gdnw, _ = AR.f32(1)
            DMA("sp", gdnw[:, 0:1], gd_nw[l].rearrange("(p o) -> p o", o=1), [], [b_small])
            cwr, b_cwr = AR.f32(128)
            cw, _ = AR.f32(48)
            DMA("sp", cwr[0:48, :], conv_w[l].rearrange("j (c p) -> (j c) p", p=128), [], [b_cwr])
            bk, b_bk = bank()
            TR(bk[:, 0:48], cwr[0:48, :], ident_f[0:48, 0:48], [b_cwr, b_const], [b_bk])
            CP("dve", cw[:, :], bk[:, 0:48], [b_bk], [b_small])
            Cn, b_Cn = AR.f32(4 * 129)
            Cn3 = Cn[0:64, :].rearrange("p (h n) -> p h n", h=4)
            Cnb, b_Cnb = AR.bf16(4 * 130)
            Cnb3 = Cnb[0:64, 0:4 * 130].rearrange("p (h n) -> p h n", h=4)
            Sst, b_S = AR.f32(512)
            Sst3 = Sst.rearrange("p (h n) -> p h n", h=4)
            Sb, b_Sb = AR.bf16(512)
            Sb3 = Sb.rearrange("p (h n) -> p h n", h=4)
            carry, b_carry = AR.f32(36)
            carry3 = carry.rearrange("p (c n) -> p c n", c=12)
            P.op("pool", lambda e: e.memset(Cn[:, :], 0.0), [], [b_Cn])
            P.op("pool", lambda e: e.memset(Cnb[:, :], 0.0), [], [b_Cnb])
            P.op("pool", lambda e: e.memset(Sst[:, :], 0.0), [], [b_S])
            P.op("pool", lambda e: e.memset(Sb[:, :], 0.0), [], [b_Sb])
            P.op("pool", lambda e: e.memset(carry[:, :], 0.0), [], [b_carry])
            mlqT, b_mlqT = AR.bf16(4 * GT)
            mlqT3 = mlqT[0:64, :].rearrange("p (h n) -> p h n", h=4)
            mlkT, b_mlkT = AR.bf16(4 * GT)
            mlkT3 = mlkT[0:64, :].rearrange("p (h n) -> p h n", h=4)
            siluz, b_siluz = AR.f32(4 * GT)
            siluz3 = siluz.rearrange("p (h n) -> p h n", h=4)
            gdq, b_gdq = AR.bf16(4 * GT)
            gdq3 = gdq.rearrange("p (h n) -> p h n", h=4)
            gdk, b_gdk = AR.bf16(4 * GT)
            gdk3 = gdk.rearrange("p (h n) -> p h n", h=4)
            gdkf, b_gdkf = AR.f32(4 * GT)
            gdkf3 = gdkf.rearrange("p (h n) -> p h n", h=4)
            gdvf, b_gdvf = AR.f32(4 * GT)
            gdvf3 = gdvf.rearrange("p (h n) -> p h n", h=4)
            stage, b_stage = AR.f32(GT + 3)
            acc, b_acc = AR.f32(GT)
            post, b_post = AR.f32(GT)
            sq, b_sq = AR.f32(GT)
            rn, b_rn = AR.f32(GT)
            gates, b_gates = AR.f32(NCG * 16)
            gates3 = gates[0:64, :].rearrange("p (c n) -> p c n", c=NCG)
            gt, b_gt = AR.f32(NCG * 16)
            gt3 = gt[0:64, :].rearrange("p (c n) -> p c n", c=NCG)
            LG, b_LG = AR.f32(NCG * 8)
            LG3 = LG[0:64, :].rearrange("p (c n) -> p c n", c=NCG)
            ipre, b_ipre = AR.f32(NCG * 4)
            ipre3 = ipre[0:64, :].rearrange("p (c n) -> p c n", c=NCG)
            beta, b_beta = AR.f32(NCG * 4)
            beta3 = beta[0:64, :].rearrange("p (c n) -> p c n", c=NCG)
            CS, b_CS = AR.f32(NCG * 8)
            CS3 = CS[0:64, :].rearrange("p (c n) -> p c n", c=NCG)
            LA, b_LA = AR.f32(NCG * 8)
            LA3 = LA[0:64, :].rearrange("p (c n) -> p c n", c=NCG)
            EG128, b_EG = AR.f32(NCG * 4)
            EG3 = EG128.rearrange("p (c n) -> p c n", c=NCG)
            der, b_der = AR.f32(NCG * 24)
            der3 = der[0:64, :].rearrange("p (c n) -> p c n", c=NCG)
            mlk_tm, b_mlk_tm = AR.f32(256)
            mlv1, b_mlv1 = AR.bf16(4 * 130)
            mlv13 = mlv1[0:64, 0:4 * 130].rearrange("p (h n) -> p h n", h=4)
            P.op("pool", lambda e: e.memset(mlv1[:, :], 1.0), [], [b_mlv1])
            sigo, b_sigo = AR.f32(512)
            LcolF, b_Lcol = AR.f32(4 * 64)
            LcolF3 = LcolF[0:64, :].rearrange("p (h n) -> p h n", h=4)
            LcolG, _ = AR.f32(4 * 128)
            LcolG3 = LcolG[0:64, :].rearrange("p (h n) -> p h n", h=4)
            t44, b_t44 = AR.f32(256)
            t443 = t44[0:64, :].rearrange("p (h n) -> p h n", h=4)
            DTm, b_DT = AR.f32(256)
            DTm3 = DTm[0:64, :].rearrange("p (h n) -> p h n", h=4)
            Ef, b_Ef = AR.f32(256)
            Ef3 = Ef[0:64, :].rearrange("p (h n) -> p h n", h=4)
            DecS, b_DecS = AR.f32(256)
            DecS3 = DecS[0:64, :].rearrange("p (h n) -> p h n", h=4)
            DecT, b_DecT = AR.f32(256)
            DecT3 = DecT[0:64, :].rearrange("p (h n) -> p h n", h=4)
            E128, b_E128 = AR.f32(256)
            E1283 = E128.rearrange("p (h n) -> p h n", h=4)
            qtil, b_qtil = AR.bf16(256)
            qtil3 = qtil[0:64, :].rearrange("p (h n) -> p h n", h=4)
            gqt, b_gqt = AR.bf16(256)
            gqt3 = gqt.rearrange("p (h n) -> p h n", h=4)
            sint, b_sint = AR.bf16(256)
            sint3 = sint[0:64, :].rearrange("p (h n) -> p h n", h=4)
            rden, b_rden = AR.f32(8)
            hml, b_hml = AR.f32(512)
            hml3 = hml[0:64, :].rearrange("p (h n) -> p h n", h=4)
            hsq, b_hsq = AR.f32(512)
            hsq3 = hsq[0:64, :].rearrange("p (h n) -> p h n", h=4)
            ssum, b_ssum = AR.f32(8)
            kw, b_kw = AR.bf16(256)
            kw3 = kw[0:64, :].rearrange("p (h n) -> p h n", h=4)
            mixT, b_mixT = AR.bf16(8 * 64)
            mixT3 = mixT.rearrange("p (f n) -> p f n", f=8)
            bv, b_bv = AR.bf16(512)
            bv3 = bv[0:64, :].rearrange("p (h n) -> p h n", h=4)
            kbg, b_kbg = AR.bf16(512)
            kbg3 = kbg[0:64, :].rearrange("p (h n) -> p h n", h=4)
            kend, b_kend = AR.bf16(512)
            kend3 = kend[0:64, :].rearrange("p (h n) -> p h n", h=4)
            P0f, b_P0f = AR.f32(256)
            P0f3 = P0f[0:64, :].rearrange("p (h n) -> p h n", h=4)
            Pm = [AR.bf16(256) for _ in range(2)]
            PT = [AR.bf16(256) for _ in range(2)]
            Yb = [AR.bf16(256) for _ in range(2)]
            nwT, b_nwT = AR.bf16(256)
            nwT3 = nwT.rearrange("p (h n) -> p h n", h=4)
            vnew, b_vnew = AR.bf16(512)
            vnew3 = vnew[0:64, :].rearrange("p (h n) -> p h n", h=4)
            qkd, b_qkd = AR.bf16(256)
            qkd3 = qkd[0:64, :].rearrange("p (h n) -> p h n", h=4)
            osq, b_osq = AR.f32(256)
            orstd, b_orstd = AR.f32(256)
            ot1, b_ot1 = AR.f32(256)
            lnw = []
            for _ in range(2):
                a1, bb1 = AR.f32(1024)
                a2, bb2 = AR.f32(12)
                a3, bb3 = AR.f32(2)
                a4, bb4 = AR.f32(2)
                lnw.append((a1, bb1, a2[:, :].rearrange("p (a b) -> p a b", a=2), bb2, a3, bb3, a4, bb4))

            xTg, b_xTg = AR.bf16(8 * GT)
            xT3 = xTg.rearrange("p (k n) -> p k n", k=8)
            xstg = AR.f32(1024)
            for g in range(max_groups or (S // GT)):
                tok0 = 0
                xbufs = [b_xTg]
                load_xT(src, [bb for c in range(NCG) for bb in src_bufs_fn(g * NCG + c)], g * GT, GT, xT3, b_xTg, xstg)
                for qk in range(2):
                    for h in range(4):
                        col0 = (C_MLQ if qk == 0 else C_MLK) + h * 64
                        bk, b_bk = bank()
                        for k in range(8):
                            MM(bk[0:64, 0:GT], win3[:, k, col0:col0 + 64], xT3[:, k, 0:GT],
                               [b_win] + xbufs, [b_bk], start=(k == 0), stop=(k == 7))
                        if qk == 0:
                            ACT(mlqT3[:, h, :], bk[0:64, 0:GT], AF.Copy, [b_bk], [b_mlqT], scale=0.125)
                        else:
                            CP("dve", mlkT3[:, h, :], bk[0:64, 0:GT], [b_bk], [b_mlkT])
                for h in range(4):
                    bk, b_bk = bank()
                    for k in range(8):
                        MM(bk[:, 0:GT], win3[:, k, C_GDZ + h * 128:C_GDZ + (h + 1) * 128], xT3[:, k, 0:GT],
                           [b_win] + xbufs, [b_bk], start=(k == 0), stop=(k == 7))
                    ACT(siluz3[:, h, :], bk[:, 0:GT], AF.Silu, [b_bk], [b_siluz])
                for c in range(12):
                    bk, b_bk = bank()
                    for k in range(8):
                        MM(bk[:, 0:GT], win3[:, k, C_GDQ + c * 128:C_GDQ + (c + 1) * 128], xT3[:, k, 0:GT],
                           [b_win] + xbufs, [b_bk], start=(k == 0), stop=(k == 7))
                    CP("pool", stage[:, 0:3], carry3[:, c, :], [b_carry], [b_stage])
                    CP("act", stage[:, 3:GT + 3], bk[:, 0:GT], [b_bk], [b_stage])
                    CP("pool", carry3[:, c, :], stage[:, GT:GT + 3], [b_stage], [b_carry])
                    TS("dve", acc[:, :], stage[:, 0:GT], cw[:, c:c + 1], ALU.mult, [b_stage, b_small], [b_acc])
                    for j in range(1, 4):
                        STT(acc[:, :], stage[:, j:j + GT], cw[:, j * 12 + c:j * 12 + c + 1], acc[:, :], ALU.mult, ALU.add,
                            [b_stage, b_small, b_acc], [b_acc])
                    if c >= 8:
                        ACT(gdvf3[:, c - 8, :], acc[:, :], AF.Silu, [b_acc], [b_gdvf])
                    else:
                        ACT(post[:, :], acc[:, :], AF.Silu, [b_acc], [b_post])
                        TT("pool", sq[:, :], post[:, :], post[:, :], ALU.mult, [b_post], [b_sq])
                        bk2, b_bk2 = bank()
                        MM(bk2[:, 0:GT], ones_f[:, :], sq[:, :], [b_const, b_sq], [b_bk2])
                        rsqrt_(rn[:, :], bk2[:, 0:GT], 1.0, 1e-6, [b_bk2], [b_rn], (sq[:, :], b_sq))
                        if c < 4:
                            STT(gdq3[:, c, :], post[:, :], 128.0 ** -0.5, rn[:, :], ALU.mult, ALU.mult, [b_post, b_rn], [b_gdq])
                        else:
                            TT("dve", gdkf3[:, c - 4, :], post[:, :], rn[:, :], ALU.mult, [b_post, b_rn], [b_gdkf])
                            CP("pool", gdk3[:, c - 4, :], gdkf3[:, c - 4, :], [b_gdkf], [b_gdk])
                bkg, b_bkg = bank()
                for c in range(NCG):
                    t0 = c * 64
                    for part, col in ((0, C_MLI), (1, C_GDB)):
                        for k in range(8):
                            MM(bkg[0:64, c * 16 + part * 8:c * 16 + part * 8 + 8], xT3[:, k, t0:t0 + 64], win3[:, k, col:col + 8],
                               [b_win, b_xTg], [b_bkg], start=(k == 0), stop=(k == 7))
                CP("dve", gates[0:64, :], bkg[0:64, 0:NCG * 16], [b_bkg], [b_gates])
                TT("dve", gt3[:, :, 0:8], gates3[:, :, 0:8], bc3(bif[0:64, 0:8], [64, NCG, 8], 1), ALU.add, [b_gates, b_small], [b_gt])
                ACT(gt3[:, :, 0:8], gt3[:, :, 0:8], AF.Tanh, [b_gt], [b_gt], scale=1.0 / 15.0)
                TS("dve", ipre3[:, :, :], gt3[:, :, 0:4], 15.0, ALU.mult, [b_gt], [b_ipre])
                ACT(gt3[:, :, 4:8], gt3[:, :, 4:8], AF.Exp, [b_gt], [b_gt], scale=-15.0)
                ACT(gt3[:, :, 4:8], gt3[:, :, 4:8], AF.Ln, [b_gt], [b_gt], bias=1.0)
                TS("dve", LG3[:, :, 0:4], gt3[:, :, 4:8], -1.0, ALU.mult, [b_gt], [b_LG])
                ACT(beta3[:, :, :], gates3[:, :, 8:12], AF.Sigmoid, [b_gates], [b_beta])
                TT("dve", gt3[:, :, 8:12], gates3[:, :, 12:16], bc3(dtb[0:64, 0:4], [64, NCG, 4], 1), ALU.add, [b_gates, b_small], [b_gt])
                ACT(gt3[:, :, 12:16], gt3[:, :, 8:12], AF.Abs, [b_gt], [b_gt])
                ACT(gt3[:, :, 12:16], gt3[:, :, 12:16], AF.Exp, [b_gt], [b_gt], scale=-1.0)
                ACT(gt3[:, :, 12:16], gt3[:, :, 12:16], AF.Ln, [b_gt], [b_gt], bias=1.0)
                TS("dve", gt3[:, :, 8:12], gt3[:, :, 8:12], 0.0, ALU.max, [b_gt], [b_gt])
                TT("dve", gt3[:, :, 8:12], gt3[:, :, 8:12], gt3[:, :, 12:16], ALU.add, [b_gt], [b_gt])
                TT("dve", LG3[:, :, 4:8], gt3[:, :, 8:12], bc3(negA[0:64, 0:4], [64, NCG, 4], 1), ALU.mult, [b_gt, b_small], [b_LG])
                bkc, b_bkc = bank()
                MM(bkc[0:64, 0:NCG * 8], t1_f[:, :], LG[0:64, 0:NCG * 8], [b_const, b_LG], [b_bkc])
                MM(bkc[0:64, 64:64 + NCG * 8], ones_f[0:64, 0:64], LG[0:64, 0:NCG * 8], [b_const, b_LG], [b_bkc])
                MM(bkc[:, 128:128 + NCG * 8], ones_f[0:64, :], LG[0:64, 0:NCG * 8], [b_const, b_LG], [b_bkc])
                CP("dve", CS[0:64, :], bkc[0:64, 0:NCG * 8], [b_bkc], [b_CS])
                CP("dve", LA[0:64, :], bkc[0:64, 64:64 + NCG * 8], [b_bkc], [b_LA])
                ACT(EG3[:, :, :], bkc[:, 128:128 + NCG * 8].rearrange("p (c n) -> p c n", c=NCG)[:, :, 4:8], AF.Exp, [b_bkc], [b_EG])
                TT("dve", der3[:, :, 0:4], ipre3[:, :, :], CS3[:, :, 0:4], ALU.subtract, [b_ipre, b_CS], [b_der])
                TT("dve", der3[:, :, 4:8], der3[:, :, 0:4], LA3[:, :, 0:4], ALU.add, [b_der, b_LA], [b_der])
                ACT(der3[:, :, 4:8], der3[:, :, 4:8], AF.Exp, [b_der], [b_der])
                ACT(der3[:, :, 8:12], LA3[:, :, 0:4], AF.Exp, [b_LA], [b_der])
                ACT(der3[:, :, 12:16], CS3[:, :, 4:8], AF.Exp, [b_CS], [b_der])
                TT("dve", der3[:, :, 12:16], der3[:, :, 12:16], beta3[:, :, :], ALU.mult, [b_der, b_beta], [b_der])
                TT("dve", der3[:, :, 16:20], LA3[:, :, 4:8], CS3[:, :, 4:8], ALU.subtract, [b_LA, b_CS], [b_der])
                ACT(der3[:, :, 16:20], der3[:, :, 16:20], AF.Exp, [b_der], [b_der])
                TS("dve", der3[:, :, 20:24], beta3[:, :, :], -1.0, ALU.mult, [b_beta], [b_der])

                for c in range(NCG):
                    t0 = c * 64
                    lo = c * 64
                    ci = g * NCG + c
                    xb_ = [b_xTg]
                    bk, b_bk = bank()
                    for k in range(8):
                        MM(bk[0:64, 0:256], xT3[:, k, t0:t0 + 64], win3[:, k, C_MLK:C_MLK + 256], [b_win] + xb_, [b_bk],
                           start=(k == 0), stop=(k == 7))
                    CP("act", mlk_tm[0:64, :], bk[0:64, 0:256], [b_bk], [b_mlk_tm])
                    bk, b_bk = bank()
                    for k in range(8):
                        MM(bk[0:64, 0:512], xT3[:, k, t0:t0 + 64], win3[:, k, C_MLV:C_MLV + 512], [b_win] + xb_, [b_bk],
                           start=(k == 0), stop=(k == 7))
                    CP("dve", mlv13[:, :, 0:128], bk[0:64, 0:512].rearrange("p (h n) -> p h n", h=4), [b_bk], [b_mlv1])
                    bk, b_bk = bank()
                    for k in range(8):
                        MM(bk[0:64, 0:512], xT3[:, k, t0:t0 + 64], win3[:, k, C_MLO:C_MLO + 512], [b_win] + xb_, [b_bk],
                           start=(k == 0), stop=(k == 7))
                    ACT(sigo[0:64, :], bk[0:64, 0:512], AF.Sigmoid, [b_bk], [b_sigo])
                    CP("pool", LcolF3[:, :, :], bc3(LG3[:, c, 0:4], [64, 4, 64], 2), [b_LG], [b_Lcol])
                    CP("pool", LcolG3[:, :, :], bc3(LG3[:, c, 4:8], [64, 4, 128], 2), [b_LG], [b_Lcol])
                    bkM, b_bkM = bank()
                    for h in range(4):
                        MM(bkM[0:64, h * 64:(h + 1) * 64], LcolF3[:, h, :], t1_f[:, :], [b_Lcol, b_const], [b_bkM])
                    for h in range(4):
                        MM(bkM[0:64, 256 + h * 64:256 + (h + 1) * 64], LcolG3[:, h, 0:64], t1_f[:, :], [b_Lcol, b_const], [b_bkM])
                    bkE, b_bkE = bank()
                    for h in range(4):
                        MM(bkE[:, h * 64:(h + 1) * 64], LcolG3[:, h, :], t1_f[:, :], [b_Lcol, b_const], [b_bkE])
                    Mf = bkM[0:64, 0:256].rearrange("p (h n) -> p h n", h=4)
                    Mg = bkM[0:64, 256:512].rearrange("p (h n) -> p h n", h=4)
                    TT("dve", t443, Mf, bc3(der3[:, c, 0:4], [64, 4, 64], 2), ALU.add, [b_bkM, b_der], [b_t44])
                    TT("dve", t443, t443, bc3(negc[:, :], [64, 4, 64], 1), ALU.add, [b_t44, b_const], [b_t44])
                    ACT(DTm3, t443, AF.Exp, [b_t44], [b_DT])
                    ACT(Ef3, Mf, AF.Exp, [b_bkM], [b_Ef])
                    TT("dve", qtil3, mlqT3[:, :, lo:lo + 64], Ef3, ALU.mult, [b_mlqT, b_Ef], [b_qtil])
                    STT(t443, Mg, -1.0, bc3(CS3[:, c, 4:8], [64, 4, 64], 2), ALU.mult, ALU.add, [b_bkM, b_CS, b_t44], [b_t44])
                    TT("dve", t443, t443, bc3(negs[:, :], [64, 4, 64], 1), ALU.add, [b_t44, b_const], [b_t44])
                    ACT(DecS3, t443, AF.Exp, [b_t44], [b_DecS])
                    TT("dve", t443, Mg, bc3(CS3[:, c, 4:8], [64, 4, 64], 2), ALU.subtract, [b_bkM, b_CS, b_t44], [b_t44])
                    TT("dve", t443, t443, bc3(negc[:, :], [64, 4, 64], 1), ALU.add, [b_t44, b_const], [b_t44])
                    ACT(DecT3, t443, AF.Exp, [b_t44], [b_DecT])
                    ACT(E128[:, :], bkE[:, 0:256], AF.Exp, [b_bkE], [b_E128])
                    TT("dve", gqt3, gdq3[:, :, lo:lo + 64], E1283, ALU.mult, [b_gdq, b_E128], [b_gqt])

                    bk, b_bk = bank()
                    for h in range(4):
                        MM(bk[0:64, h * 64:(h + 1) * 64], mlkT3[:, h, lo:lo + 64], mlqT3[:, h, lo:lo + 64], [b_mlkT, b_mlqT], [b_bk])
                    TT("dve", sint3, bk[0:64, 0:256].rearrange("p (h n) -> p h n", h=4), DTm3, ALU.mult, [b_bk, b_DT], [b_sint])
                    for hp in range(2):
                        bk, b_bk = bank()
                        for j in range(2):
                            h = hp * 2 + j
                            MM(bk[0:64, j * 129:(j + 1) * 129], qtil3[:, h, :], Cnb3[:, h, 0:129], [b_qtil, b_Cnb], [b_bk], start=True, stop=False)
                            MM(bk[0:64, j * 129:(j + 1) * 129], sint3[:, h, :], mlv13[:, h, 0:129], [b_sint, b_mlv1], [b_bk], start=False, stop=True)
                        bv_ = bk[0:64, 0:258].rearrange("p (j n) -> p j n", j=2)
                        ACT(rden[0:64, hp * 2:hp * 2 + 2], bv_[:, :, 128], AF.Abs, [b_bk], [b_rden])
                        TS("dve", rden[0:64, hp * 2:hp * 2 + 2], rden[0:64, hp * 2:hp * 2 + 2], 1.0, ALU.max, [b_rden], [b_rden])
                        P.op("dve", lambda e, hp=hp: e.reciprocal(rden[0:64, 4 + hp * 2:4 + hp * 2 + 2], rden[0:64, hp * 2:hp * 2 + 2]), [b_rden], [b_rden])
                        TT("dve", hml3[:, hp * 2:hp * 2 + 2, :], bv_[:, :, 0:128], bc3(rden[0:64, 4 + hp * 2:4 + hp * 2 + 2], [64, 2, 128], 2),
                           ALU.mult, [b_bk, b_rden], [b_hml])
                    TT("pool", hsq3, hml3, hml3, ALU.mult, [b_hml], [b_hsq])
                    P.op("dve", lambda e: e.tensor_reduce(ssum[0:64, 0:4], hsq3, AX.X, ALU.add), [b_hsq], [b_ssum])
                    rsqrt_(ssum[0:64, 4:8], ssum[0:64, 0:4], 1.0 / 128.0, 1e-6, [b_ssum], [b_ssum], (ssum[0:64, 0:4], b_ssum))
                    TT("dve", hml3, hml3, bc3(ssum[0:64, 4:8], [64, 4, 128], 2), ALU.mult, [b_hml, b_ssum], [b_hml])
                    TT("pool", hml[0:64, :], hml[0:64, :], mlnw[0:64, :], ALU.mult, [b_hml, b_small], [b_hml])
                    TT("pool", hml[0:64, :], hml[0:64, :], sigo[0:64, :], ALU.mult, [b_hml, b_sigo], [b_hml])
                    bk, b_bk = bank()
                    for h in range(4):
                        TR(bk[:, h * 64:(h + 1) * 64], hml[0:64, h * 128:(h + 1) * 128], ident_f[0:64, 0:64], [b_hml, b_const], [b_bk])
                    CP("act", mixT3[:, 0:4, :], bk[:, 0:256].rearrange("p (h n) -> p h n", h=4), [b_bk], [b_mixT])
                    TT("dve", kw3, mlk_tm[0:64, :].rearrange("p (h n) -> p h n", h=4), bc3(der3[:, c, 4:8], [64, 4, 64], 2), ALU.mult,
                       [b_mlk_tm, b_der], [b_kw])
                    TT("dve", Cn3, Cn3, bc3(der3[:, c, 8:12], [64, 4, 129], 2), ALU.mult, [b_Cn, b_der], [b_Cn])
                    for hp in range(2):
                        bk, b_bk = bank()
                        for j in range(2):
                            h = hp * 2 + j
                            MM(bk[0:64, j * 129:(j + 1) * 129], kw3[:, h, :], mlv13[:, h, 0:129], [b_kw, b_mlv1], [b_bk])
                        TT("dve", Cn3[:, hp * 2:hp * 2 + 2, :], Cn3[:, hp * 2:hp * 2 + 2, :], bk[0:64, 0:258].rearrange("p (j n) -> p j n", j=2),
                           ALU.add, [b_Cn, b_bk], [b_Cn])
                    CP("pool", Cnb3[:, :, 0:129], Cn3, [b_Cn], [b_Cnb])

                    bkk, b_bkk = bank()
                    bkv, b_bkv = bank()
                    for h in range(4):
                        TR(bkk[0:64, h * 128:(h + 1) * 128], gdkf3[:, h, lo:lo + 64], ident_f[:, :], [b_gdkf, b_const], [b_bkk])
                    for h in range(4):
                        TR(bkv[0:64, h * 128:(h + 1) * 128], gdvf3[:, h, lo:lo + 64], ident_f[:, :], [b_gdvf, b_const], [b_bkv])
                    ktm = bkk[0:64, 0:512].rearrange("p (h n) -> p h n", h=4)
                    vtm = bkv[0:64, 0:512].rearrange("p (h n) -> p h n", h=4)
                    TT("dve", bv3, vtm, bc3(beta3[:, c, :], [64, 4, 128], 2), ALU.mult, [b_bkv, b_beta], [b_bv])
                    TT("dve", kbg3, ktm, bc3(der3[:, c, 12:16], [64, 4, 128], 2), ALU.mult, [b_bkk, b_der], [b_kbg])
                    TT("dve", kend3, ktm, bc3(der3[:, c, 16:20], [64, 4, 128], 2), ALU.mult, [b_bkk, b_der], [b_kend])
                    bk, b_bk = bank()
                    for h in range(4):
                        MM(bk[0:64, h * 64:(h + 1) * 64], gdk3[:, h, lo:lo + 64], gdk3[:, h, lo:lo + 64], [b_gdk], [b_bk])
                    TT("dve", t443, bk[0:64, 0:256].rearrange("p (h n) -> p h n", h=4), bc3(der3[:, c, 20:24], [64, 4, 64], 2), ALU.mult,
                       [b_bk, b_der, b_t44], [b_t44])
                    TT("dve", P0f3, t443, DecS3, ALU.mult, [b_t44, b_DecS], [b_P0f])
                    pm, b_pm = Pm[0]
                    pt, b_pt = PT[0]
                    yb, b_yb = Yb[0]
                    pm3 = pm[0:64, :].rearrange("p (h n) -> p h n", h=4)
                    CP("pool", pm[0:64, :], P0f[0:64, :], [b_P0f], [b_pm])
                    bk, b_bk = bank()
                    for h in range(4):
                        TR(bk[0:64, h * 64:(h + 1) * 64], P0f3[:, h, :], ident_f[0:64, 0:64], [b_P0f, b_const], [b_bk])
                    CP("act", pt[0:64, :], bk[0:64, 0:256], [b_bk], [b_pt])
                    TT("dve", yb[0:64, :].rearrange("p (h n) -> p h n", h=4), bk[0:64, 0:256].rearrange("p (h n) -> p h n", h=4),
                       bc3(ident_f[0:64, 0:64], [64, 4, 64], 1), ALU.add, [b_bk, b_const], [b_yb])
                    cur = 0
                    for lev in range(1, 6):
                        pm, b_pm = Pm[cur]
                        pt, b_pt = PT[cur]
                        yb, b_yb = Yb[cur]
                        pmn, b_pmn = Pm[1 - cur]
                        ptn, b_ptn = PT[1 - cur]
                        ybn, b_ybn = Yb[1 - cur]
                        v3 = lambda a: a[0:64, :].rearrange("p (h n) -> p h n", h=4)
                        bkA, b_bkA = bank()
                        for h in range(4):
                            MM(bkA[0:64, h * 64:(h + 1) * 64], v3(pt)[:, h, :], v3(pm)[:, h, :], [b_pt, b_pm], [b_bkA])
                        CP("act", pmn[0:64, :], bkA[0:64, 0:256], [b_bkA], [b_pmn])
                        if lev < 5:
                            bkB, b_bkB = bank()
                            for h in range(4):
                                MM(bkB[0:64, h * 64:(h + 1) * 64], v3(pm)[:, h, :], v3(pt)[:, h, :], [b_pt, b_pm], [b_bkB])
                            CP("dve", ptn[0:64, :], bkB[0:64, 0:256], [b_bkB], [b_ptn])
                        bkC, b_bkC = bank()
                        for h in range(4):
                            MM(bkC[0:64, h * 64:(h + 1) * 64], v3(pmn)[:, h, :], v3(yb)[:, h, :], [b_pmn, b_yb], [b_bkC])
                        TT("dve", ybn[0:64, :], yb[0:64, :], bkC[0:64, 0:256], ALU.add, [b_yb, b_bkC], [b_ybn])
                        cur = 1 - cur
                    RT, b_RT = Yb[cur]
                    RT3 = RT[0:64, :].rearrange("p (h n) -> p h n", h=4)
                    bk, b_bk = bank()
                    for h in range(4):
                        MM(bk[:, h * 64:(h + 1) * 64], kbg3[:, h, :], RT3[:, h, :], [b_kbg, b_RT], [b_bk])
                    ACT(nwT[:, :], bk[:, 0:256], AF.Copy, [b_bk], [b_nwT], scale=-1.0)
                    bk, b_bk = bank()
                    for h in range(4):
                        MM(bk[0:64, h * 128:(h + 1) * 128], RT3[:, h, :], bv3[:, h, :], [b_RT, b_bv], [b_bk], start=True, stop=False)
                        MM(bk[0:64, h * 128:(h + 1) * 128], nwT3[:, h, :], Sb3[:, h, :], [b_nwT, b_Sb], [b_bk], start=False, stop=True)
                    CP("act", vnew[0:64, :], bk[0:64, 0:512], [b_bk], [b_vnew])
                    bk, b_bk = bank()
                    for h in range(4):
                        MM(bk[0:64, h * 64:(h + 1) * 64], gdk3[:, h, lo:lo + 64], gdq3[:, h, lo:lo + 64], [b_gdk, b_gdq], [b_bk])
                    TT("dve", qkd3, bk[0:64, 0:256].rearrange("p (h n) -> p h n", h=4), DecT3, ALU.mult, [b_bk, b_DecT], [b_qkd])
                    bko, b_bko = bank()
                    for h in range(4):
                        MM(bko[:, h * 64:(h + 1) * 64], Sb3[:, h, :], gqt3[:, h, :], [b_Sb, b_gqt], [b_bko], start=True, stop=False)
                        MM(bko[:, h * 64:(h + 1) * 64], vnew3[:, h, :], qkd3[:, h, :], [b_vnew, b_qkd], [b_bko], start=False, stop=True)
                    bk, b_bk = bank()
                    for h in range(4):
                        MM(bk[:, h * 128:(h + 1) * 128], kend3[:, h, :], vnew3[:, h, :], [b_kend, b_vnew], [b_bk])
                    TT("dve", Sst3, Sst3, bc3(EG3[:, c, :], [128, 4, 128], 2), ALU.mult, [b_S, b_EG], [b_S])
                    TT("dve", Sst[:, :], Sst[:, :], bk[:, 0:512], ALU.add, [b_S, b_bk], [b_S])
                    CP("pool", Sb[:, :], Sst[:, :], [b_S], [b_Sb])
                    ACT(osq[:, :], bko[:, 0:256], AF.Square, [b_bko], [b_osq])
                    bk, b_bk = bank()
                    MM(bk[:, 0:256], ones_f[:, :], osq[:, :], [b_const, b_osq], [b_bk])
                    rsqrt_(orstd[:, :], bk[:, 0:256], 1.0 / 128.0, 1e-6, [b_bk], [b_orstd], (osq[:, :], b_osq))
                    TT("dve", ot1[:, :], bko[:, 0:256], orstd[:, :], ALU.mult, [b_bko, b_orstd], [b_ot1])
                    STT(mixT3[:, 4:8, :], ot1[:, :].rearrange("p (h n) -> p h n", h=4), gdnw[:, 0:1], siluz3[:, :, lo:lo + 64],
                        ALU.mult, ALU.mult, [b_ot1, b_small, b_siluz], [b_mixT])
                    hp_ = []
                    hb_ = []
                    for n in range(2):
                        bk, b_bk = bank()
                        for f in range(8):
                            MM(bk[0:64, 0:512], mixT3[:, f, :], wout3[:, f, n * 512:(n + 1) * 512], [b_mixT, b_wout], [b_bk],
                               start=(f == 0), stop=(f == 7))
                        hp_.append(bk[0:64, 0:512])
                        hb_.append(b_bk)
                    ln_epilogue(64, g * GT + c * 64, hp_, hb_, src, src_bufs_fn(ci), G, Bt, b_gb, dst, dst_bufs_fn(ci), lnw[ci % 2], False)
            P.barrier()


        def xattn(l):
            AR.reset()
            W = []
            for i in range(4):
                w, bw = AR.bf16(8 * 1024)
                w3 = w.rearrange("p (k n) -> p k n", k=8)
                DMA("pool", w3, xa_w[i][l].rearrange("(k p) n -> p k n", p=128), [], [bw])
                W.append((w3, bw))
            (wq3, b_wq), (wk3, b_wk), (wv3, b_wv), (wo3, b_wo) = W
            G, b_gb = AR.f32(1024)
            Bt, _ = AR.f32(1024)
            load_bcast(G, lng[1][l:l + 1, :], 128, [b_gb])
            load_bcast(Bt, lnb[1][l:l + 1, :], 128, [b_gb])
            memT, b_memT = AR.bf16(8 * 256)
            memT3 = memT.rearrange("p (k n) -> p k n", k=8)
            xstg = AR.f32(1024)
            load_xT(mem_in, [], 0, 256, memT3, b_memT, xstg)
            kT, b_kT = AR.bf16(8 * 256)
            kT3 = kT.rearrange("p (k n) -> p k n", k=8)
            for f in range(8):
                bk, b_bk = bank()
                for k in range(8):
                    MM(bk[:, 0:256], wk3[:, k, f * 128:(f + 1) * 128], memT3[:, k, :], [b_wk, b_memT], [b_bk], start=(k == 0), stop=(k == 7))
                CP("dve", kT3[:, f, :], bk[:, 0:256], [b_bk], [b_kT])
            vv, b_vv = AR.bf16(2 * 1024)
            vv3 = vv.rearrange("p (m n) -> p m n", m=2)
            for mc in range(2):
                for n in range(2):
                    bk, b_bk = bank()
                    for k in range(8):
                        MM(bk[:, 0:512], memT3[:, k, mc * 128:(mc + 1) * 128], wv3[:, k, n * 512:(n + 1) * 512], [b_wv, b_memT], [b_bk],
                           start=(k == 0), stop=(k == 7))
                    CP("act", vv3[:, mc, n * 512:(n + 1) * 512], bk[:, 0:512], [b_bk], [b_vv])
            XG = 512
            xTg, b_xTg = AR.bf16(8 * XG)
            xT3 = xTg.rearrange("p (k n) -> p k n", k=8)
            qT, b_qT = AR.bf16(8 * XG)
            qT3 = qT.rearrange("p (k n) -> p k n", k=8)
            expT = [AR.bf16(XG) for _ in range(2)]
            rdn, b_rdn = AR.f32(XG)
            oT, b_oT = AR.bf16(8 * XG)
            oT3 = oT.rearrange("p (k n) -> p k n", k=8)
            lnw = []
            for _ in range(2):
                a1, bb1 = AR.f32(1024)
                a2, bb2 = AR.f32(12)
                a3, bb3 = AR.f32(2)
                a4, bb4 = AR.f32(2)
                lnw.append((a1, bb1, a2[:, :].rearrange("p (a b) -> p a b", a=2), bb2, a3, bb3, a4, bb4))
            for g in range(S // XG):
                rb_ = [b_xres[g * 8 + c] for c in range(8)]
                load_xT(xres, rb_, g * XG, XG, xT3, b_xTg, xstg)
                for f in range(8):
                    bk, b_bk = bank()
                    for k in range(8):
                        MM(bk[:, 0:XG], wq3[:, k, f * 128:(f + 1) * 128], xT3[:, k, :], [b_wq, b_xTg], [b_bk], start=(k == 0), stop=(k == 7))
                    ACT(qT3[:, f, :], bk[:, 0:XG], AF.Copy, [b_bk], [b_qT], scale=1.0 / 16.0)
                for h in range(4):
                    for mc in range(2):
                        bk, b_bk = bank()
                        for j in range(2):
                            MM(bk[:, 0:XG], kT3[:, 2 * h + j, mc * 128:(mc + 1) * 128], qT3[:, 2 * h + j, :], [b_kT, b_qT], [b_bk],
                               start=(j == 0), stop=(j == 1))
                        ACT(expT[mc][0][:, :], bk[:, 0:XG], AF.Exp, [b_bk], [expT[mc][1]])
                    bd, b_bd = bank()
                    for mc in range(2):
                        MM(bd[:, 0:XG], ones_b[:, :], expT[mc][0][:, :], [b_const, expT[mc][1]], [b_bd], start=(mc == 0), stop=(mc == 1))
                    P.op("dve", lambda e, bd=bd: e.reciprocal(rdn[:, :], bd[:, 0:XG]), [b_bd], [b_rdn])
                    for j in range(2):
                        bk, b_bk = bank()
                        for mc in range(2):
                            MM(bk[:, 0:XG], vv3[:, mc, (2 * h + j) * 128:(2 * h + j + 1) * 128], expT[mc][0][:, :], [b_vv, expT[mc][1]], [b_bk],
                               start=(mc == 0), stop=(mc == 1))
                        TT("dve", oT3[:, 2 * h + j, :], bk[:, 0:XG], rdn[:, :], ALU.mult, [b_bk, b_rdn], [b_oT])
                for t in range(XG // 128):
                    hp_, hb_ = [], []
                    for n in range(2):
                        bk, b_bk = bank()
                        for f in range(8):
                            MM(bk[:, 0:512], oT3[:, f, t * 128:(t + 1) * 128], wo3[:, f, n * 512:(n + 1) * 512], [b_oT, b_wo], [b_bk],
                               start=(f == 0), stop=(f == 7))
                        hp_.append(bk[:, 0:512])
                        hb_.append(b_bk)
                    r0 = g * XG + t * 128
                    rb2 = [b_xres[r0 // 64], b_xres[r0 // 64 + 1]]
                    ln_epilogue(128, r0, hp_, hb_, xres, rb2, G, Bt, b_gb, xres, rb2, lnw[t % 2], False)
            P.barrier()

        def moe(l, dst, dst_bufs_fn):
            AR.reset()
            G, b_gb = AR.f32(1024)
            Bt, _ = AR.f32(1024)
            load_bcast(G, lng[2][l:l + 1, :], 128, [b_gb])
            load_bcast(Bt, lnb[2][l:l + 1, :], 128, [b_gb])
            dest = dest_t
            b_dest = Buf("dest")
            gate, b_gate = AR.f32(32 * 4)
            gate3 = gate.rearrange("p (t k) -> p t k", t=32)
            mark = AR.off
            rw, b_rw = AR.f32(8 * NE)
            rw3 = rw.rearrange("p (k e) -> p k e", k=8)
            DMA("sp", rw3, r_w[l].rearrange("(k p) e -> p k e", p=128), [], [b_rw])
            rb, _ = AR.f32(NE)
            load_bcast(rb, r_b[l:l + 1, :], 128, [b_rw])
            cnt, b_cnt = AR.f32(NE)
            P.op("pool", lambda e: e.memset(cnt[:, :], 0.0), [], [b_cnt])
            xs2 = [AR.f32(1024) for _ in range(2)]
            xT32, b_xT32 = AR.f32(8 * 128)
            xT323 = xT32.rearrange("p (k n) -> p k n", k=8)
            xbf = [(xbf_t[i], Buf("xbf%d" % i)) for i in range(2)]
            lg, b_lg = AR.f32(NE)
            mx8, b_mx8 = AR.f32(8)
            sm, b_sm = AR.f32(8)
            ex, b_ex = AR.f32(NE)
            msk, b_msk = AR.f32(NE)
            mskb, b_mskb = AR.bf16(NE)
            pos, b_pos = AR.f32(NE)
            ov, b_ov = AR.f32(NE)
            oh, b_oh = AR.f32(NE)
            tm, b_tm = AR.f32(NE)
            dsel, b_dsel = AR.f32(4)
            dsel16, b_dsel16 = AR.f32(16)
            b_xsall = Buf("xs_all")
            b_ysall = Buf("ys_all")
            for t in range(32):
                xs_, b_xs_ = xs2[t % 2]
                xb_, b_xb_ = xbf[t % 2]
                rbf = [b_xres[2 * t], b_xres[2 * t + 1]]
                DMA("sp", xs_[:, :], xres[t * 128:(t + 1) * 128, :], rbf, [b_xs_])
                for half in range(2):
                    bk, b_bk = bank()
                    for k in range(8):
                        TR(bk[:, k * 64:(k + 1) * 64], xs_[half * 64:(half + 1) * 64, k * 128:(k + 1) * 128],
                           ident_f[half * 64:(half + 1) * 64, half * 64:(half + 1) * 64], [b_xs_, b_const], [b_bk])
                    CP("dve", xT323[:, :, half * 64:(half + 1) * 64], bk[:, 0:512].rearrange("p (k n) -> p k n", k=8), [b_bk], [b_xT32])
                for cp in range(4):
                    CP("act", xb_[cp][:, :], xs_[:, cp * 256:(cp + 1) * 256], [b_xs_], [b_xb_])
                bl, b_bl = bank()
                for k in range(8):
                    MM(bl[:, 0:NE], xT323[:, k, :], rw3[:, k, :], [b_xT32, b_rw], [b_bl], start=(k == 0), stop=(k == 7))
                TT("dve", lg[:, :], bl[:, 0:NE], rb[:, :], ALU.add, [b_bl, b_rw], [b_lg])
                P.op("dve", lambda e: e.max(mx8[:, :], lg[:, :]), [b_lg], [b_mx8])
                TS("dve", sm[:, 0:1], mx8[:, 0:1], -1.0, ALU.mult, [b_mx8], [b_sm])
                ACT(ex[:, :], lg[:, :], AF.Exp, [b_lg, b_sm], [b_ex], bias=sm[:, 0:1])
                TS("dve", msk[:, :], lg[:, :], mx8[:, 3:4], ALU.is_ge, [b_lg, b_mx8], [b_msk])
                TT("dve", ex[:, :], ex[:, :], msk[:, :], ALU.mult, [b_ex, b_msk], [b_ex])
                P.op("dve", lambda e: e.tensor_reduce(sm[:, 1:2], ex[:, :], AX.X, ALU.add), [b_ex], [b_sm])
                P.op("dve", lambda e: e.reciprocal(sm[:, 2:3], sm[:, 1:2]), [b_sm], [b_sm])
                TS("dve", ex[:, :], ex[:, :], sm[:, 2:3], ALU.mult, [b_ex, b_sm], [b_ex])
                CP("pool", mskb[:, :], msk[:, :], [b_msk], [b_mskb])
                bp, b_bp = bank()
                MM(bp[:, 0:NE], tri_b[:, :], mskb[:, :], [b_const, b_mskb], [b_bp])
                MM(bp[:, NE:2 * NE], ones_b[:, :], mskb[:, :], [b_const, b_mskb], [b_bp])
                TT("dve", pos[:, :], bp[:, 0:NE], cnt[:, :], ALU.add, [b_bp, b_cnt], [b_pos])
                TT("dve", cnt[:, :], cnt[:, :], bp[:, NE:2 * NE], ALU.add, [b_bp, b_cnt], [b_cnt])
                TS("dve", ov[:, :], pos[:, :], float(CAP), ALU.is_ge, [b_pos], [b_ov])
                STT(pos[:, :], ov[:, :], 1.0e7, pos[:, :], ALU.mult, ALU.add, [b_ov, b_pos], [b_pos])
                TT("dve", pos[:, :], pos[:, :], eoff[:, :], ALU.add, [b_pos, b_const], [b_pos])
                TS("dve", ov[:, :], ov[:, :], -1.0, ALU.mult, [b_ov], [b_ov], s2=1.0, op1=ALU.add)
                TT("dve", ex[:, :], ex[:, :], ov[:, :], ALU.mult, [b_ex, b_ov], [b_ex])
                for k in range(4):
                    TS("dve", oh[:, :], lg[:, :], mx8[:, k:k + 1], ALU.is_equal, [b_lg, b_mx8], [b_oh])
                    TT("dve", tm[:, :], oh[:, :], pos[:, :], ALU.mult, [b_oh, b_pos], [b_tm])
                    P.op("dve", lambda e, k=k: e.tensor_reduce(dsel[:, k:k + 1], tm[:, :], AX.X, ALU.add), [b_tm], [b_dsel])
                    TT("dve", tm[:, :], oh[:, :], ex[:, :], ALU.mult, [b_oh, b_ex], [b_tm])
                    P.op("dve", lambda e, k=k, t=t: e.tensor_reduce(gate3[:, t, k:k + 1], tm[:, :], AX.X, ALU.add), [b_tm], [b_gate])
                TS("dve", dsel16[:, :].rearrange("p (k c) -> p k c", k=4), bc3(dsel[:, 0:4], [128, 4, 4], 2), 4.0, ALU.mult, [b_dsel], [b_dsel16])
                TT("dve", dsel16[:, :], dsel16[:, :], cpoff[:, :], ALU.add, [b_dsel16, b_const], [b_dsel16])
                CP("dve", dest[:, t * 16:t * 16 + 16], dsel16[:, :], [b_dsel16], [b_dest])
                for k in range(4):
                    for cp in range(4):
                        q = idx_ctr[0] % 8
                        idx_ctr[0] += 1
                        CP("dve", idx_t[q][:, :], dest[:, t * 16 + k * 4 + cp:t * 16 + k * 4 + cp + 1], [b_dest], [b_idx[q]])
                        P.dma("pool", lambda e, q=q, xb_=xb_, cp=cp: e.indirect_dma_start(
                            out=xs_d, out_offset=bass.IndirectOffsetOnAxis(ap=idx_t[q][:, :], axis=0),
                            in_=xb_[cp][:, :], in_offset=None, bounds_check=bc_reg(e), oob_is_err=False), [b_xb_, b_idx[q]], [b_xsall])
            P.barrier()
            AR.off = mark
            wgu = [AR.bf16(8 * 2048) for _ in range(2)]
            wdn = [AR.bf16(8 * 1024) for _ in range(2)]
            bdn = [AR.f32(1024) for _ in range(1)]
            bgr, b_bgr = AR.f32(128)
            bguT, b_bguT = AR.f32(NE * 16)
            for q in range(4):
                DMA("sp", bgr[:, :], b_gu[l].rearrange("e (c p) -> (e c) p", p=128)[q * 128:(q + 1) * 128, :], [], [b_bgr])
                bk, b_bk = bank()
                TR(bk[:, 0:128], bgr[:, :], ident_f[:, :], [b_bgr, b_const], [b_bk])
                CP("dve", bguT[:, q * 128:(q + 1) * 128], bk[:, 0:128], [b_bk], [b_bguT])
            xsel, b_xsel = AR.bf16(NCAP * 1024)
            xsel3 = xsel.rearrange("p (j d) -> p j d", j=NCAP)
            xselT, b_xselT = AR.bf16(8 * CAP)
            xselT3 = xselT.rearrange("p (k n) -> p k n", k=8)
            actT, b_actT = AR.bf16(8 * CAP)
            actT3 = actT.rearrange("p (k n) -> p k n", k=8)
            HC = CAP // 2
            gtmp, b_gtmp = AR.f32(HC)
            stmp, b_stmp = AR.f32(HC)
            ltmp, b_ltmp = AR.f32(HC)
            ysb = [AR.f32(1024) for _ in range(1)]
            for e_ in range(NE):
                wg, b_wg = wgu[e_ % 2]
                wg3 = wg.rearrange("p (k n) -> p k n", k=8)
                wd, b_wd = wdn[e_ % 2]
                wd3 = wd.rearrange("p (k n) -> p k n", k=8)
                bd_, b_bd_ = bdn[0]
                for k in range(8):
                    DMA("pool", wg3[:, k, :], w_gu[l, e_, k * 128:(k + 1) * 128, :], [], [b_wg])
                DMA("pool", wd3, w_dn[l, e_].rearrange("(k p) n -> p k n", p=128), [], [b_wd])
                DMA("sp", bd_[:, :], b_dn[l, e_:e_ + 1, :].partition_broadcast(128), [], [b_bd_])
                DMA("sp", xsel3, xs_d[e_ * CAP * 4:(e_ + 1) * CAP * 4, :].rearrange("(j p c) d -> p j (c d)", p=128, c=4), [b_xsall], [b_xsel])
                for j in range(NCAP):
                    for k in range(8):
                        TR(psb[:, k * 128:(k + 1) * 128], xsel3[:, j, k * 128:(k + 1) * 128], ident_b[:, :], [b_xsel, b_const], [b_psb])
                    CP("act", xselT3[:, :, j * 128:(j + 1) * 128], psb[:, :].rearrange("p (k n) -> p k n", k=8), [b_psb], [b_xselT])
                for f in range(8):
                    for hh in range(2):
                        bg, b_bg = bank()
                        for k in range(8):
                            MM(bg[:, 0:HC], wg3[:, k, f * 128:(f + 1) * 128], xselT3[:, k, hh * HC:(hh + 1) * HC], [b_wg, b_xselT], [b_bg],
                               start=(k == 0), stop=(k == 7))
                        bl, b_bl = bank()
                        for k in range(8):
                            MM(bl[:, 0:HC], wg3[:, k, 1024 + f * 128:1024 + (f + 1) * 128], xselT3[:, k, hh * HC:(hh + 1) * HC], [b_wg, b_xselT], [b_bl],
                               start=(k == 0), stop=(k == 7))
                        c0 = e_ * 16 + f
                        TS("dve", gtmp[:, :], bg[:, 0:HC], bguT[:, c0:c0 + 1], ALU.add, [b_bg, b_bguT], [b_gtmp], s2=7.0, op1=ALU.min)
                        ACT(stmp[:, :], gtmp[:, :], AF.Sigmoid, [b_gtmp], [b_stmp], scale=1.702)
                        TS("dve", ltmp[:, :], bl[:, 0:HC], bguT[:, c0 + 8:c0 + 9], ALU.add, [b_bl, b_bguT], [b_ltmp], s2=7.0, op1=ALU.min)
                        TS("dve", ltmp[:, :], ltmp[:, :], -7.0, ALU.max, [b_ltmp], [b_ltmp], s2=1.0, op1=ALU.add)
                        TT("pool", gtmp[:, :], gtmp[:, :], stmp[:, :], ALU.mult, [b_gtmp, b_stmp], [b_gtmp])
                        TT("pool", actT3[:, f, hh * HC:(hh + 1) * HC], gtmp[:, :], ltmp[:, :], ALU.mult, [b_gtmp, b_ltmp], [b_actT])
                for j in range(NCAP):
                    ys_, b_ys_ = ysb[0]
                    for n in range(2):
                        bk, b_bk = bank()
                        for f in range(8):
                            MM(bk[:, 0:512], actT3[:, f, j * 128:(j + 1) * 128], wd3[:, f, n * 512:(n + 1) * 512], [b_actT, b_wd], [b_bk],
                               start=(f == 0), stop=(f == 7))
                        TT("dve", ys_[:, n * 512:(n + 1) * 512], bk[:, 0:512], bd_[:, n * 512:(n + 1) * 512], ALU.add, [b_bk, b_bd_], [b_ys_])
                    DMA("sp", ys_d[(e_ * CAP + j * 128) * 4:(e_ * CAP + (j + 1) * 128) * 4, :].rearrange("(p c) d -> p (c d)", c=4), ys_[:, :], [b_ys_], [b_ysall])
            P.barrier()
            AR.off = mark
            gth = [(gth_t[i], Buf("gth%d" % i)) for i in range(4)]
            for k in range(4):
                for cp in range(4):
                    P.op("pool", lambda e, k=k, cp=cp: e.memset(gth[k][0][cp][:, :], 0.0), [], [gth[k][1]])
            accs = [AR.f32(1024) for _ in range(2)]
            lnw = []
            for _ in range(2):
                a1, bb1 = AR.f32(1024)
                a2, bb2 = AR.f32(12)
                a3, bb3 = AR.f32(2)
                a4, bb4 = AR.f32(2)
                lnw.append((a1, bb1, a2[:, :].rearrange("p (a b) -> p a b", a=2), bb2, a3, bb3, a4, bb4))
            for t in range(32):
                for k in range(4):
                    for cp in range(4):
                        q = idx_ctr[0] % 8
                        idx_ctr[0] += 1
                        CP("dve", idx_t[q][:, :], dest[:, t * 16 + k * 4 + cp:t * 16 + k * 4 + cp + 1], [b_dest], [b_idx[q]])
                        P.dma("pool", lambda e, k=k, q=q, cp=cp: e.indirect_dma_start(
                            out=gth[k][0][cp][:, :], out_offset=None, in_=ys_d,
                            in_offset=bass.IndirectOffsetOnAxis(ap=idx_t[q][:, :], axis=0),
                            bounds_check=bc_reg(e), oob_is_err=False), [b_ysall, b_idx[q]], [gth[k][1]])
                ac, b_ac = accs[t % 2]
                for cp in range(4):
                    cs_ = slice(cp * 256, (cp + 1) * 256)
                    TS("dve", ac[:, cs_], gth[0][0][cp][:, :], gate3[:, t, 0:1], ALU.mult, [gth[0][1], b_gate], [b_ac])
                    for k in range(1, 4):
                        STT(ac[:, cs_], gth[k][0][cp][:, :], gate3[:, t, k:k + 1], ac[:, cs_], ALU.mult, ALU.add, [gth[k][1], b_gate, b_ac], [b_ac])
                rb2 = [b_xres[2 * t], b_xres[2 * t + 1]]
                ln_epilogue(128, t * 128, [ac[:, 0:512], ac[:, 512:1024]], [b_ac, b_ac], xres, rb2, G, Bt, b_gb, dst, dst_bufs_fn(t), lnw[t % 2], False)
            P.barrier()

        for l in range(n_layers):
            if "mixer" in skip:
                AR.reset()
                cpb = [AR.f32(1024) for _ in range(2)]
                for t in range(32):
                    a, bb = cpb[t % 2]
                    DMA("sp", a[:, :], x_in[t * 128:(t + 1) * 128, :], [], [bb])
                    DMA("sp", xres[t * 128:(t + 1) * 128, :], a[:, :], [bb], [b_xres[2 * t], b_xres[2 * t + 1]])
                P.barrier()
            elif l == 0:
                mixer(l, x_in, lambda ci: [], xres, lambda ci: [b_xres[ci]])
            else:
                mixer(l, xres, lambda ci: [b_xres[ci]], xres, lambda ci: [b_xres[ci]])
            if stop_after == (l, "mixer"):
                break
            if "xattn" not in skip:
                xattn(l)
            if stop_after == (l, "xattn"):
                break
            last = (l == n_layers - 1) and stop_after is None
            if last:
                moe(l, out_d, lambda t: [b_out])
            else:
                moe(l, xres, lambda t: [b_xres[2 * t], b_xres[2 * t + 1]])
            if stop_after == (l, "moe"):
                break
        if stop_after is not None:
            AR.reset()
            fin = [AR.f32(1024) for _ in range(2)]
            for t in range(32):
                a, bb = fin[t % 2]
                DMA("sp", a[:, :], xres[t * 128:(t + 1) * 128, :], [b_xres[2 * t], b_xres[2 * t + 1]], [bb])
                DMA("sp", out_d[t * 128:(t + 1) * 128, :], a[:, :], [bb], [b_out])
        P.barrier()
        print("ops:", P.n_ops, {e: len(s) for e, s in P.streams.items()})
        P.emit()
    return nc


def make_consts():
    i = np.arange(64)
    c = {
        "c_ident": np.eye(128, dtype=np.float32),
        "c_t1": (i[:, None] <= i[None, :]).astype(np.float32),
        "c_negc": np.where(i[None, :] >= i[:, None], 0.0, NEG).astype(np.float32),
        "c_negs": np.where(i[None, :] < i[:, None], 0.0, NEG).astype(np.float32),
        "c_tri": (np.arange(128)[:, None] < np.arange(128)[None, :]).astype(np.float32),
        "c_eoff": np.tile((np.arange(NE) * CAP).astype(np.float32)[None, :], (128, 1)),
        "c_ones": np.ones((128, 128), np.float32),
        "c_cp": np.tile(np.arange(4, dtype=np.float32)[None, :], (128, 4)),
    }
    return c


_NAMES = ["w_in", "mlstm_b_i", "mlstm_b_f", "mlstm_norm_w", "gdn_conv_w", "gdn_a_log", "gdn_dt_bias", "gdn_norm_w", "w_out",
          "ln1_g", "ln1_b", "xa_wq", "xa_wk", "xa_wv", "xa_wo", "ln2_g", "ln2_b", "router_w", "router_b",
          "exp_w_gu", "exp_b_gu", "exp_w_down", "exp_b_down", "ln3_g", "ln3_b"]


def run(inputs, n_layers=L, stop_after=None, cores=8, max_groups=None, small=False, skip=(), trace=False):
    nc = build(n_layers, stop_after, max_groups=max_groups, small=small, skip=skip)
    consts = make_consts()
    shared = {k: np.ascontiguousarray(np.asarray(inputs[k], dtype=np.float32)) for k in _NAMES}
    if small:
        names = small if isinstance(small, (set, tuple, list)) else ("xa_wq", "xa_wk", "xa_wv", "xa_wo", "router_w", "exp_w_gu", "exp_b_gu", "exp_w_down", "exp_b_down")
        for k in names:
            shared[k] = np.zeros([1] * shared[k].ndim, np.float32)
    in_maps = []
    for c in range(cores):
        m = dict(shared)
        m.update(consts)
        m["x"] = np.ascontiguousarray(np.asarray(inputs["x"][c], dtype=np.float32))
        m["mem"] = np.ascontiguousarray(np.asarray(inputs["mem"][c], dtype=np.float32))
        in_maps.append(m)
    res = run_bass_kernel_spmd(nc, in_maps, core_ids=list(range(cores)), **({"trace": True} if trace else {}))
    if trace:
        print("EXEC_TIME_NS", res.exec_time_ns)
    return np.stack([np.asarray(r["out"]) for r in res.results], axis=0)


def kernel(**inputs):
    return run(inputs).astype(np.float32)
```

```python
import numpy as np
from contextlib import ExitStack
import concourse.bass as bass
import concourse.mybir as mybir
from concourse.bass_utils import run_bass_kernel_spmd

F32 = mybir.dt.float32
BF16 = mybir.dt.bfloat16
I32 = mybir.dt.int32
AF = mybir.ActivationFunctionType
ALU = mybir.AluOpType
AX = mybir.AxisListType

S = 4096
D = 1024
L = 4
NE = 32
CAP = 1024
NCAP = CAP // 128
ALPHA = (2.0 * L) ** 0.25
NEG = -30000.0
GT = 256
NCG = GT // 64

import os as _os
SAME_ENGINE_SYNC = _os.environ.get("SES", "1") == "1"
EPOCH = 16000
N_DMA_SEMS = 24

C_MLQ, C_MLK, C_MLV, C_MLO, C_MLI, C_MLF = 0, 256, 512, 1024, 1536, 1540
C_GDQ, C_GDK, C_GDV, C_GDZ, C_GDB, C_GDA = 1544, 2056, 2568, 3080, 3592, 3596


class Buf:
    __slots__ = ("name", "w", "r", "excl")

    def __init__(self, name, excl=False):
        self.name = name
        self.w = None
        self.r = []
        self.excl = excl


class Prog:
    ENGS = ("pe", "dve", "act", "pool", "sp")

    def __init__(self, nc, es):
        self.nc = nc
        self.es = es
        self.streams = {e: [] for e in self.ENGS}
        self.count = {e: 0 for e in self.ENGS}
        self.known = {e: {} for e in self.ENGS}
        self.sems = {}
        self.dma_next = {e: 0 for e in self.ENGS}
        self.dma_uses = {}
        self.n_ops = 0

    def sem(self, key):
        s = self.sems.get(key)
        if s is None:
            s = self.es.enter_context(self.nc.semaphore("s_%s_%s" % key))
            self.sems[key] = s
        return s

    def _wait(self, eng, tok):
        key, val = tok
        if key[0] == eng and (eng == "pe" or not SAME_ENGINE_SYNC):
            return
        if self.known[eng].get(key, 0) >= val:
            return
        self.known[eng][key] = val
        self.streams[eng].append(("wait", key, val))

    def _deps(self, eng, reads, writes):
        for b in reads:
            if b.w is not None:
                self._wait(eng, b.w)
        for b in writes:
            if b.w is not None:
                self._wait(eng, b.w)
            for t in b.r:
                self._wait(eng, t)

    def _commit(self, tok, reads, writes):
        for b in reads:
            b.r.append(tok)
        for b in writes:
            b.w = tok
            b.r = []

    def op(self, eng, fn, reads=(), writes=()):
        ex = [b for b in reads if b.excl]
        if ex:
            reads = [b for b in reads if not b.excl]
            writes = list(writes) + ex
        self._deps(eng, reads, writes)
        n = self.count[eng]
        self.count[eng] = n + 1
        key = (eng, n // EPOCH)
        self.sem(key)
        tok = (key, n % EPOCH + 1)
        self.streams[eng].append(("op", fn, key, 1))
        self._commit(tok, reads, writes)
        self.n_ops += 1
        return tok

    def dma(self, eng, fn, reads=(), writes=()):
        self._deps(eng, reads, writes)
        i = self.dma_next[eng]
        self.dma_next[eng] = (i + 1) % N_DMA_SEMS
        key = ("d" + eng, i)
        uses = self.dma_uses.get(key, 0)
        self.sem(key)
        if uses > 0:
            self._wait(eng, (key, 16 * uses))
        self.dma_uses[key] = uses + 1
        tok = (key, 16 * (uses + 1))
        self.streams[eng].append(("op", fn, key, 16))
        self._commit(tok, reads, writes)
        self.n_ops += 1
        return tok

    def barrier(self):
        toks = []
        for e in self.ENGS:
            n = self.count[e]
            if n > 0:
                toks.append(((e, (n - 1) // EPOCH), (n - 1) % EPOCH + 1))
        for k, u in self.dma_uses.items():
            toks.append((k, 16 * u))
        for e in self.ENGS:
            for t in toks:
                if t[0][0] == e:
                    continue
                self._wait(e, t)

    def emit(self):
        nc = self.nc
        engmap = {"pe": "tensor", "dve": "vector", "act": "scalar", "pool": "gpsimd", "sp": "sync"}
        with nc.Block() as block:
            for e in self.ENGS:
                stream = self.streams[e]
                if not stream:
                    continue

                def body(engine, stream=stream):
                    for it in stream:
                        if it[0] == "wait":
                            engine.wait_ge(self.sems[it[1]], it[2])
                        else:
                            it[1](engine).then_inc(self.sems[it[2]], it[3])

                getattr(block, engmap[e])(body)


class Arena:
    def __init__(self, t, ncols):
        self.t = t
        self.n = ncols
        self.off = 0
        self.k = 0

    def reset(self):
        self.off = 0

    def _take(self, c32):
        assert self.off + c32 <= self.n, ("arena overflow", self.off, c32, self.n)
        a = self.t[:, self.off:self.off + c32]
        self.off += c32
        self.k += 1
        return a, Buf("ar%d" % self.k)

    def f32(self, cols):
        return self._take(cols)

    def bf16(self, cols):
        a, b = self._take((cols + 1) // 2)
        return a.bitcast(BF16), b

    def i32(self, cols):
        a, b = self._take(cols)
        return a.bitcast(I32), b


def bc3(ap, shape, axis):
    return ap.unsqueeze(axis).to_broadcast(list(shape))


def build(n_layers=L, stop_after=None, dbg=False, max_groups=None, small=False, skip=()):
    nc = bass.Bass("TRN2", target_bir_lowering=False)
    es = ExitStack()

    BIG = ("xa_wq", "xa_wk", "xa_wv", "xa_wo", "router_w", "exp_w_gu", "exp_b_gu", "exp_w_down", "exp_b_down")

    def din(name, shape, dt=F32):
        if small and name in (small if isinstance(small, (set, tuple, list)) else BIG):
            shape = [1] * len(shape)
        return nc.dram_tensor(name, list(shape), dt, kind="ExternalInput").ap()

    x_in = din("x", [S, D])
    mem_in = din("mem", [256, D])
    w_in = din("w_in", [L, D, 3600])
    b_i = din("mlstm_b_i", [L, 4])
    b_f = din("mlstm_b_f", [L, 4])
    ml_nw = din("mlstm_norm_w", [L, 512])
    conv_w = din("gdn_conv_w", [L, 4, 1536])
    a_log = din("gdn_a_log", [L, 4])
    dt_b = din("gdn_dt_bias", [L, 4])
    gd_nw = din("gdn_norm_w", [L, 128])
    w_out = din("w_out", [L, D, D])
    lng = [din("ln%d_g" % i, [L, D]) for i in (1, 2, 3)]
    lnb = [din("ln%d_b" % i, [L, D]) for i in (1, 2, 3)]
    xa_w = [din("xa_w" + n, [L, D, D]) for n in "qkvo"]
    r_w = din("router_w", [L, D, NE])
    r_b = din("router_b", [L, NE])
    w_gu = din("exp_w_gu", [L, NE, D, 2 * D])
    b_gu = din("exp_b_gu", [L, NE, 2 * D])
    w_dn = din("exp_w_down", [L, NE, D, D])
    b_dn = din("exp_b_down", [L, NE, D])
    c_ident = din("c_ident", [128, 128])
    c_t1 = din("c_t1", [64, 64])
    c_negc = din("c_negc", [64, 64])
    c_negs = din("c_negs", [64, 64])
    c_tri = din("c_tri", [128, 128])
    c_eoff = din("c_eoff", [128, NE])
    c_ones = din("c_ones", [128, 128])
    c_cp = din("c_cp", [128, 16])
    out_d = nc.dram_tensor("out", [S, D], F32, kind="ExternalOutput").ap()
    xres = nc.dram_tensor("xres", [S, D], F32, kind="Internal").ap()
    xs_d = nc.dram_tensor("xs_d", [NE * CAP * 4, 256], BF16, kind="Internal").ap()
    ys_d = nc.dram_tensor("ys_d", [NE * CAP * 4, 256], F32, kind="Internal").ap()
    b_xres = [Buf("xres%d" % i) for i in range(64)]
    b_out = Buf("out")
    b_xs = [Buf("xs%d" % e) for e in range(NE)]
    b_ys = [Buf("ys%d" % e) for e in range(NE)]

    P = Prog(nc, es)

    def sb(name, shape, dt):
        return es.enter_context(nc.sbuf_tensor(name, list(shape), dt))

    with es:
        ident_f = sb("ident_f", [128, 128], F32)
        ident_b = sb("ident_b", [128, 128], BF16)
        ones_f = sb("ones_f", [128, 128], F32)
        ones_b = sb("ones_b", [128, 128], BF16)
        tri_b = sb("tri_b", [128, 128], BF16)
        t1_f = sb("t1_f", [64, 64], F32)
        negc = sb("negc", [64, 64], F32)
        negs = sb("negs", [64, 64], F32)
        eoff = sb("eoff", [128, NE], F32)
        dest_t = sb("dest_t", [128, 512], I32)
        cpoff = sb("cpoff", [128, 16], F32)
        idx_t = [sb("idx%d" % i, [128, 1], I32) for i in range(8)]
        b_idx = [Buf("idx%d" % i) for i in range(8)]
        idx_ctr = [0]
        b_const = Buf("const")
        ARENA_COLS = 49000
        arena_t = sb("arena", [128, ARENA_COLS], F32)
        AR = Arena(arena_t, ARENA_COLS)
        banks = [es.enter_context(nc.psum_tensor("ps%d" % i, [128, 512], F32)) for i in range(7)]
        b_banks = [Buf("bank%d" % i, True) for i in range(7)]
        psb = es.enter_context(nc.psum_tensor("psb", [128, 1024], BF16))
        b_psb = Buf("psb", True)
        bank_ctr = [0]

        def bank():
            i = bank_ctr[0] % 7
            bank_ctr[0] += 1
            return banks[i], b_banks[i]

        def MM(out, lhsT, rhs, R, W, start=True, stop=True):
            P.op("pe", lambda e: e.matmul(out, lhsT, rhs, start=start, stop=stop), R, W)

        def TR(out, in_, idn, R, W):
            P.op("pe", lambda e: e.transpose(out, in_, idn), R, W)

        def TT(eng, out, a, b, op, R, W):
            P.op(eng, lambda e: e.tensor_tensor(out, a, b, op), R, W)

        def TS(eng, out, a, s1, op0, R, W, s2=None, op1=None):
            if op1 is None:
                P.op(eng, lambda e: e.tensor_scalar(out, a, s1, None, op0), R, W)
            else:
                P.op(eng, lambda e: e.tensor_scalar(out, a, s1, s2, op0, op1), R, W)

        def STT(out, a, s, b, op0, op1, R, W):
            P.op("dve", lambda e: e.scalar_tensor_tensor(out, a, s, b, op0, op1), R, W)

        def ACT(out, in_, func, R, W, bias=0.0, scale=1.0):
            P.op("act", lambda e: e.activation(out, in_, func, bias=bias, scale=scale), R, W)

        def CP(eng, out, in_, R, W):
            if eng == "act":
                P.op("act", lambda e: e.copy(out, in_), R, W)
            else:
                P.op(eng, lambda e: e.tensor_copy(out, in_), R, W)

        def DMA(eng, out, in_, R, W):
            return P.dma(eng, lambda e: e.dma_start(out=out, in_=in_), R, W)

        def rsqrt_(out, in_, scale, eps, R, W, tmp):
            ACT(tmp[0], in_, AF.Sqrt, R, [tmp[1]], bias=eps, scale=scale)
            P.op("dve", lambda e: e.reciprocal(out, tmp[0]), [tmp[1]], W)

        DMA("sp", ident_f[:], c_ident, [], [b_const])
        DMA("sp", ones_f[:], c_ones, [], [b_const])
        DMA("sp", t1_f[:], c_t1, [], [b_const])
        DMA("sp", negc[:], c_negc, [], [b_const])
        DMA("sp", negs[:], c_negs, [], [b_const])
        DMA("sp", eoff[:], c_eoff, [], [b_const])
        DMA("sp", cpoff[:], c_cp, [], [b_const])
        DMA("pool", ident_b[:], c_ident, [], [b_const])
        DMA("pool", ones_b[:], c_ones, [], [b_const])
        DMA("pool", tri_b[:], c_tri, [], [b_const])
        P.barrier()

        bc_state = {}

        def bc_reg(e):
            if "r" not in bc_state:
                bc_state["r"] = e.alloc_register("bc")
                e.reg_mov(bc_state["r"], NE * CAP * 4 - 1)
            return bc_state["r"]

        def ln_epilogue(Pn, row0, hparts, hbufs, xsrc, xsrc_bufs, G, Bt, b_gb, dst, dst_bufs, work, want_xT):
            xo, b_xo, stt_, b_st, mv, b_mv, sc, b_sc = work
            DMA("sp", xo[0:Pn, :], xsrc[row0:row0 + Pn, :], xsrc_bufs, [b_xo])
            for n in range(2):
                STT(xo[0:Pn, n * 512:(n + 1) * 512], xo[0:Pn, n * 512:(n + 1) * 512], ALPHA, hparts[n],
                    ALU.mult, ALU.add, [b_xo, hbufs[n]], [b_xo])
            for n in range(2):
                P.op("dve", lambda e, n=n: e.bn_stats(stt_[0:Pn, n, :], xo[0:Pn, n * 512:(n + 1) * 512]), [b_xo], [b_st])
            P.op("dve", lambda e: e.bn_aggr(mv[0:Pn, :], stt_[0:Pn, :, :]), [b_st], [b_mv])
            rsqrt_(sc[0:Pn, 1:2], mv[0:Pn, 1:2], 1.0, 1e-5, [b_mv], [b_sc], (sc[0:Pn, 0:1], b_sc))
            TS("dve", xo[0:Pn, :], xo[0:Pn, :], mv[0:Pn, 0:1], ALU.subtract, [b_xo, b_mv, b_sc], [b_xo],
               s2=sc[0:Pn, 1:2], op1=ALU.mult)
            TT("pool", xo[0:Pn, :], xo[0:Pn, :], G[0:Pn, :], ALU.mult, [b_xo, b_gb], [b_xo])
            TT("pool", xo[0:Pn, :], xo[0:Pn, :], Bt[0:Pn, :], ALU.add, [b_xo, b_gb], [b_xo])
            DMA("sp", dst[row0:row0 + Pn, :], xo[0:Pn, :], [b_xo], dst_bufs)

        def load_bcast(dst, src_row, Pn, W):
            DMA("sp", dst[0:Pn, :], src_row.partition_broadcast(Pn), [], W)

        def load_xT(src, src_bufs, row0, ntok, xTg3, b_xTg, stg):
            for t in range(ntok // 128):
                st_, b_st_ = stg
                DMA("sp", st_[:, :], src[row0 + t * 128:row0 + (t + 1) * 128, :], src_bufs, [b_st_])
                for half in range(2):
                    bk, b_bk = bank()
                    for k in range(8):
                        TR(bk[:, k * 64:(k + 1) * 64], st_[half * 64:(half + 1) * 64, k * 128:(k + 1) * 128],
                           ident_f[half * 64:(half + 1) * 64, half * 64:(half + 1) * 64], [b_st_, b_const], [b_bk])
                    o = t * 128 + half * 64
                    CP("act", xTg3[:, :, o:o + 64], bk[:, 0:512].rearrange("p (k n) -> p k n", k=8), [b_bk], [b_xTg])

        def mixer(l, src, src_bufs_fn, dst, dst_bufs_fn):
            AR.reset()
            win, b_win = AR.bf16(8 * 3600)
            win3 = win.rearrange("p (k n) -> p k n", k=8)
            wout, b_wout = AR.bf16(8 * 1024)
            wout3 = wout.rearrange("p (k n) -> p k n", k=8)
            for k in range(8):
                DMA("pool", win3[:, k, :], w_in[l, k * 128:(k + 1) * 128, :], [], [b_win])
            DMA("pool", wout3, w_out[l].rearrange("(k p) n -> p k n", p=128), [], [b_wout])
            G, b_gb = AR.f32(1024)
            Bt, _ = AR.f32(1024)
            load_bcast(G, lng[0][l:l + 1, :], 64, [b_gb])
            load_bcast(Bt, lnb[0][l:l + 1, :], 64, [b_gb])
            mlnw, b_small = AR.f32(512)
            load_bcast(mlnw, ml_nw[l:l + 1, :], 64, [b_small])
            bif, _ = AR.f32(8)
            load_bcast(bif[:, 0:4], b_i[l:l + 1, :], 64, [b_small])
            load_bcast(bif[:, 4:8], b_f[l:l + 1, :], 64, [b_small])
            negA, _ = AR.f32(4)
            dtb, _ = AR.f32(4)
            load_bcast(negA, a_log[l:l + 1, :], 64, [b_small])
            load_bcast(dtb, dt_b[l:l + 1, :], 64, [b_small])
            ACT(negA[0:64, :], negA[0:64, :], AF.Exp, [b_small], [b_small])
            TS("dve", negA[0:64, :], negA[0:64, :], -1.0, ALU.mult, [b_small], [b_small])
            gdnw, _ = AR.f32(1)
            DMA("sp", gdnw[:, 0:1], gd_nw[l].rearrange("(p o) -> p o", o=1), [], [b_small])
            cwr, b_cwr = AR.f32(128)
            cw, _ = AR.f32(48)
            DMA("sp", cwr[0:48, :], conv_w[l].rearrange("j (c p) -> (j c) p", p=128), [], [b_cwr])
            bk, b_bk = bank()
            TR(bk[:, 0:48], cwr[0:48, :], ident_f[0:48, 0:48], [b_cwr, b_const], [b_bk])
            CP("dve", cw[:, :], bk[:, 0:48], [b_bk], [b_small])
            Cn, b_Cn = AR.f32(4 * 129)
            Cn3 = Cn[0:64, :].rearrange("p (h n) -> p h n", h=4)
            Cnb, b_Cnb = AR.bf16(4 * 130)
            Cnb3 = Cnb[0:64, 0:4 * 130].rearrange("p (h n) -> p h n", h=4)
            Sst, b_S = AR.f32(512)
            Sst3 = Sst.rearrange("p (h n) -> p h n", h=4)
            Sb, b_Sb = AR.bf16(512)
            Sb3 = Sb.rearrange("p (h n) -> p h n", h=4)
            carry, b_carry = AR.f32(36)
            carry3 = carry.rearrange("p (c n) -> p c n", c=12)
            P.op("pool", lambda e: e.memset(Cn[:, :], 0.0), [], [b_Cn])
            P.op("pool", lambda e: e.memset(Cnb[:, :], 0.0), [], [b_Cnb])
            P.op("pool", lambda e: e.memset(Sst[:, :], 0.0), [], [b_S])
            P.op("pool", lambda e: e.memset(Sb[:, :], 0.0), [], [b_Sb])
            P.op("pool", lambda e: e.memset(carry[:, :], 0.0), [], [b_carry])
            mlqT, b_mlqT = AR.bf16(4 * GT)
            mlqT3 = mlqT[0:64, :].rearrange("p (h n) -> p h n", h=4)
            mlkT, b_mlkT = AR.bf16(4 * GT)
            mlkT3 = mlkT[0:64, :].rearrange("p (h n) -> p h n", h=4)
            siluz, b_siluz = AR.f32(4 * GT)
            siluz3 = siluz.rearrange("p (h n) -> p h n", h=4)
            gdq, b_gdq = AR.bf16(4 * GT)
            gdq3 = gdq.rearrange("p (h n) -> p h n", h=4)
            gdk, b_gdk = AR.bf16(4 * GT)
            gdk3 = gdk.rearrange("p (h n) -> p h n", h=4)
            gdkf, b_gdkf = AR.f32(4 * GT)
            gdkf3 = gdkf.rearrange("p (h n) -> p h n", h=4)
            gdvf, b_gdvf = AR.f32(4 * GT)
            gdvf3 = gdvf.rearrange("p (h n) -> p h n", h=4)
            stage, b_stage = AR.f32(GT + 3)
            acc, b_acc = AR.f32(GT)
            post, b_post = AR.f32(GT)
            sq, b_sq = AR.f32(GT)
            rn, b_rn = AR.f32(GT)
            gates, b_gates = AR.f32(NCG * 16)
            gates3 = gates[0:64, :].rearrange("p (c n) -> p c n", c=NCG)
            gt, b_gt = AR.f32(NCG * 16)
            gt3 = gt[0:64, :].rearrange("p (c n) -> p c n", c=NCG)
            LG, b_LG = AR.f32(NCG * 8)
            LG3 = LG[0:64, :].rearrange("p (c n) -> p c n", c=NCG)
            ipre, b_ipre = AR.f32(NCG * 4)
            ipre3 = ipre[0:64, :].rearrange("p (c n) -> p c n", c=NCG)
            beta, b_beta = AR.f32(NCG * 4)
            beta3 = beta[0:64, :].rearrange("p (c n) -> p c n", c=NCG)
            CS, b_CS = AR.f32(NCG * 8)
            CS3 = CS[0:64, :].rearrange("p (c n) -> p c n", c=NCG)
            LA, b_LA = AR.f32(NCG * 8)
            LA3 = LA[0:64, :].rearrange("p (c n) -> p c n", c=NCG)
            EG128, b_EG = AR.f32(NCG * 4)
            EG3 = EG128.rearrange("p (c n) -> p c n", c=NCG)
            der, b_der = AR.f32(NCG * 24)
            der3 = der[0:64, :].rearrange("p (c n) -> p c n", c=NCG)
            mlk_tm, b_mlk_tm = AR.f32(256)
            mlv1, b_mlv1 = AR.bf16(4 * 130)
            mlv13 = mlv1[0:64, 0:4 * 130].rearrange("p (h n) -> p h n", h=4)
            P.op("pool", lambda e: e.memset(mlv1[:, :], 1.0), [], [b_mlv1])
            sigo, b_sigo = AR.f32(512)
            LcolF, b_Lcol = AR.f32(4 * 64)
            LcolF3 = LcolF[0:64, :].rearrange("p (h n) -> p h n", h=4)
            LcolG, _ = AR.f32(4 * 128)
            LcolG3 = LcolG[0:64, :].rearrange("p (h n) -> p h n", h=4)
            t44, b_t44 = AR.f32(256)
            t443 = t44[0:64, :].rearrange("p (h n) -> p h n", h=4)
            DTm, b_DT = AR.f32(256)
            DTm3 = DTm[0:64, :].rearrange("p (h n) -> p h n", h=4)
            Ef, b_Ef = AR.f32(256)
            Ef3 = Ef[0:64, :].rearrange("p (h n) -> p h n", h=4)
            DecS, b_DecS = AR.f32(256)
            DecS3 = DecS[0:64, :].rearrange("p (h n) -> p h n", h=4)
            DecT, b_DecT = AR.f32(256)
            DecT3 = DecT[0:64, :].rearrange("p (h n) -> p h n", h=4)
            E128, b_E128 = AR.f32(256)
            E1283 = E128.rearrange("p (h n) -> p h n", h=4)
            qtil, b_qtil = AR.bf16(256)
            qtil3 = qtil[0:64, :].rearrange("p (h n) -> p h n", h=4)
            gqt, b_gqt = AR.bf16(256)
            gqt3 = gqt.rearrange("p (h n) -> p h n", h=4)
            sint, b_sint = AR.bf16(256)
            sint3 = sint[0:64, :].rearrange("p (h n) -> p h n", h=4)
            rden, b_rden = AR.f32(8)
            hml, b_hml = AR.f32(512)
            hml3 = hml[0:64, :].rearrange("p (h n) -> p h n", h=4)
            hsq, b_hsq = AR.f32(512)
            hsq3 = hsq[0:64, :].rearrange("p (h n) -> p h n", h=4)
            ssum, b_ssum = AR.f32(8)
            kw, b_kw = AR.bf16(256)
            kw3 = kw[0:64, :].rearrange("p (h n) -> p h n", h=4)
            mixT, b_mixT = AR.bf16(8 * 64)
            mixT3 = mixT.rearrange("p (f n) -> p f n", f=8)
            bv, b_bv = AR.bf16(512)
            bv3 = bv[0:64, :].rearrange("p (h n) -> p h n", h=4)
            kbg, b_kbg = AR.bf16(512)
            kbg3 = kbg[0:64, :].rearrange("p (h n) -> p h n", h=4)
            kend, b_kend = AR.bf16(512)
            kend3 = kend[0:64, :].rearrange("p (h n) -> p h n", h=4)
            P0f, b_P0f = AR.f32(256)
            P0f3 = P0f[0:64, :].rearrange("p (h n) -> p h n", h=4)
            Pm = [AR.bf16(256) for _ in range(2)]
            PT = [AR.bf16(256) for _ in range(2)]
            Yb = [AR.bf16(256) for _ in range(2)]
            nwT, b_nwT = AR.bf16(256)
            nwT3 = nwT.rearrange("p (h n) -> p h n", h=4)
            vnew, b_vnew = AR.bf16(512)
            vnew3 = vnew[0:64, :].rearrange("p (h n) -> p h n", h=4)
            qkd, b_qkd = AR.bf16(256)
            qkd3 = qkd[0:64, :].rearrange("p (h n) -> p h n", h=4)
            osq, b_osq = AR.f32(256)
            orstd, b_orstd = AR.f32(256)
            ot1, b_ot1 = AR.f32(256)
            lnw = []
            for _ in range(2):
                a1, bb1 = AR.f32(1024)
                a2, bb2 = AR.f32(12)
                a3, bb3 = AR.f32(2)
                a4, bb4 = AR.f32(2)
                lnw.append((a1, bb1, a2[:, :].rearrange("p (a b) -> p a b", a=2), bb2, a3, bb3, a4, bb4))

            xTg, b_xTg = AR.bf16(8 * GT)
            xT3 = xTg.rearrange("p (k n) -> p k n", k=8)
            xstg = AR.f32(1024)
            for g in range(max_groups or (S // GT)):
                tok0 = 0
                xbufs = [b_xTg]
                load_xT(src, [bb for c in range(NCG) for bb in src_bufs_fn(g * NCG + c)], g * GT, GT, xT3, b_xTg, xstg)
                for qk in range(2):
                    for h in range(4):
                        col0 = (C_MLQ if qk == 0 else C_MLK) + h * 64
                        bk, b_bk = bank()
                        for k in range(8):
                            MM(bk[0:64, 0:GT], win3[:, k, col0:col0 + 64], xT3[:, k, 0:GT],
                               [b_win] + xbufs, [b_bk], start=(k == 0), stop=(k == 7))
                        if qk == 0:
                            ACT(mlqT3[:, h, :], bk[0:64, 0:GT], AF.Copy, [b_bk], [b_mlqT], scale=0.125)
                        else:
                            CP("dve", mlkT3[:, h, :], bk[0:64, 0:GT], [b_bk], [b_mlkT])
                for h in range(4):
                    bk, b_bk = bank()
                    for k in range(8):
                        MM(bk[:, 0:GT], win3[:, k, C_GDZ + h * 128:C_GDZ + (h + 1) * 128], xT3[:, k, 0:GT],
                           [b_win] + xbufs, [b_bk], start=(k == 0), stop=(k == 7))
                    ACT(siluz3[:, h, :], bk[:, 0:GT], AF.Silu, [b_bk], [b_siluz])
                for c in range(12):
                    bk, b_bk = bank()
                    for k in range(8):
                        MM(bk[:, 0:GT], win3[:, k, C_GDQ + c * 128:C_GDQ + (c + 1) * 128], xT3[:, k, 0:GT],
                           [b_win] + xbufs, [b_bk], start=(k == 0), stop=(k == 7))
                    CP("pool", stage[:, 0:3], carry3[:, c, :], [b_carry], [b_stage])
                    CP("act", stage[:, 3:GT + 3], bk[:, 0:GT], [b_bk], [b_stage])
                    CP("pool", carry3[:, c, :], stage[:, GT:GT + 3], [b_stage], [b_carry])
                    TS("dve", acc[:, :], stage[:, 0:GT], cw[:, c:c + 1], ALU.mult, [b_stage, b_small], [b_acc])
                    for j in range(1, 4):
                        STT(acc[:, :], stage[:, j:j + GT], cw[:, j * 12 + c:j * 12 + c + 1], acc[:, :], ALU.mult, ALU.add,
                            [b_stage, b_small, b_acc], [b_acc])
                    if c >= 8:
                        ACT(gdvf3[:, c - 8, :], acc[:, :], AF.Silu, [b_acc], [b_gdvf])
                    else:
                        ACT(post[:, :], acc[:, :], AF.Silu, [b_acc], [b_post])
                        TT("pool", sq[:, :], post[:, :], post[:, :], ALU.mult, [b_post], [b_sq])
                        bk2, b_bk2 = bank()
                        MM(bk2[:, 0:GT], ones_f[:, :], sq[:, :], [b_const, b_sq], [b_bk2])
                        rsqrt_(rn[:, :], bk2[:, 0:GT], 1.0, 1e-6, [b_bk2], [b_rn], (sq[:, :], b_sq))
                        if c < 4:
                            STT(gdq3[:, c, :], post[:, :], 128.0 ** -0.5, rn[:, :], ALU.mult, ALU.mult, [b_post, b_rn], [b_gdq])
                        else:
                            TT("dve", gdkf3[:, c - 4, :], post[:, :], rn[:, :], ALU.mult, [b_post, b_rn], [b_gdkf])
                            CP("pool", gdk3[:, c - 4, :], gdkf3[:, c - 4, :], [b_gdkf], [b_gdk])
                bkg, b_bkg = bank()
                for c in range(NCG):
                    t0 = c * 64
                    for part, col in ((0, C_MLI), (1, C_GDB)):
                        for k in range(8):
                            MM(bkg[0:64, c * 16 + part * 8:c * 16 + part * 8 + 8], xT3[:, k, t0:t0 + 64], win3[:, k, col:col + 8],
                               [b_win, b_xTg], [b_bkg], start=(k == 0), stop=(k == 7))
                CP("dve", gates[0:64, :], bkg[0:64, 0:NCG * 16], [b_bkg], [b_gates])
                TT("dve", gt3[:, :, 0:8], gates3[:, :, 0:8], bc3(bif[0:64, 0:8], [64, NCG, 8], 1), ALU.add, [b_gates, b_small], [b_gt])
                ACT(gt3[:, :, 0:8], gt3[:, :, 0:8], AF.Tanh, [b_gt], [b_gt], scale=1.0 / 15.0)
                TS("dve", ipre3[:, :, :], gt3[:, :, 0:4], 15.0, ALU.mult, [b_gt], [b_ipre])
                ACT(gt3[:, :, 4:8], gt3[:, :, 4:8], AF.Exp, [b_gt], [b_gt], scale=-15.0)
                ACT(gt3[:, :, 4:8], gt3[:, :, 4:8], AF.Ln, [b_gt], [b_gt], bias=1.0)
                TS("dve", LG3[:, :, 0:4], gt3[:, :, 4:8], -1.0, ALU.mult, [b_gt], [b_LG])
                ACT(beta3[:, :, :], gates3[:, :, 8:12], AF.Sigmoid, [b_gates], [b_beta])
                TT("dve", gt3[:, :, 8:12], gates3[:, :, 12:16], bc3(dtb[0:64, 0:4], [64, NCG, 4], 1), ALU.add, [b_gates, b_small], [b_gt])
                ACT(gt3[:, :, 12:16], gt3[:, :, 8:12], AF.Abs, [b_gt], [b_gt])
                ACT(gt3[:, :, 12:16], gt3[:, :, 12:16], AF.Exp, [b_gt], [b_gt], scale=-1.0)
                ACT(gt3[:, :, 12:16], gt3[:, :, 12:16], AF.Ln, [b_gt], [b_gt], bias=1.0)
                TS("dve", gt3[:, :, 8:12], gt3[:, :, 8:12], 0.0, ALU.max, [b_gt], [b_gt])
                TT("dve", gt3[:, :, 8:12], gt3[:, :, 8:12], gt3[:, :, 12:16], ALU.add, [b_gt], [b_gt])
                TT("dve", LG3[:, :, 4:8], gt3[:, :, 8:12], bc3(negA[0:64, 0:4], [64, NCG, 4], 1), ALU.mult, [b_gt, b_small], [b_LG])
                bkc, b_bkc = bank()
                MM(bkc[0:64, 0:NCG * 8], t1_f[:, :], LG[0:64, 0:NCG * 8], [b_const, b_LG], [b_bkc])
                MM(bkc[0:64, 64:64 + NCG * 8], ones_f[0:64, 0:64], LG[0:64, 0:NCG * 8], [b_const, b_LG], [b_bkc])
                MM(bkc[:, 128:128 + NCG * 8], ones_f[0:64, :], LG[0:64, 0:NCG * 8], [b_const, b_LG], [b_bkc])
                CP("dve", CS[0:64, :], bkc[0:64, 0:NCG * 8], [b_bkc], [b_CS])
                CP("dve", LA[0:64, :], bkc[0:64, 64:64 + NCG * 8], [b_bkc], [b_LA])
                ACT(EG3[:, :, :], bkc[:, 128:128 + NCG * 8].rearrange("p (c n) -> p c n", c=NCG)[:, :, 4:8], AF.Exp, [b_bkc], [b_EG])
                TT("dve", der3[:, :, 0:4], ipre3[:, :, :], CS3[:, :, 0:4], ALU.subtract, [b_ipre, b_CS], [b_der])
                TT("dve", der3[:, :, 4:8], der3[:, :, 0:4], LA3[:, :, 0:4], ALU.add, [b_der, b_LA], [b_der])
                ACT(der3[:, :, 4:8], der3[:, :, 4:8], AF.Exp, [b_der], [b_der])
                ACT(der3[:, :, 8:12], LA3[:, :, 0:4], AF.Exp, [b_LA], [b_der])
                ACT(der3[:, :, 12:16], CS3[:, :, 4:8], AF.Exp, [b_CS], [b_der])
                TT("dve", der3[:, :, 12:16], der3[:, :, 12:16], beta3[:, :, :], ALU.mult, [b_der, b_beta], [b_der])
                TT("dve", der3[:, :, 16:20], LA3[:, :, 4:8], CS3[:, :, 4:8], ALU.subtract, [b_LA, b_CS], [b_der])
                ACT(der3[:, :, 16:20], der3[:, :, 16:20], AF.Exp, [b_der], [b_der])
                TS("dve", der3[:, :, 20:24], beta3[:, :, :], -1.0, ALU.mult, [b_beta], [b_der])

                for c in range(NCG):
                    t0 = c * 64
                    lo = c * 64
                    ci = g * NCG + c
                    xb_ = [b_xTg]
                    bk, b_bk = bank()
                    for k in range(8):
                        MM(bk[0:64, 0:256], xT3[:, k, t0:t0 + 64], win3[:, k, C_MLK:C_MLK + 256], [b_win] + xb_, [b_bk],
                           start=(k == 0), stop=(k == 7))
                    CP("act", mlk_tm[0:64, :], bk[0:64, 0:256], [b_bk], [b_mlk_tm])
                    bk, b_bk = bank()
                    for k in range(8):
                        MM(bk[0:64, 0:512], xT3[:, k, t0:t0 + 64], win3[:, k, C_MLV:C_MLV + 512], [b_win] + xb_, [b_bk],
                           start=(k == 0), stop=(k == 7))
                    CP("dve", mlv13[:, :, 0:128], bk[0:64, 0:512].rearrange("p (h n) -> p h n", h=4), [b_bk], [b_mlv1])
                    bk, b_bk = bank()
                    for k in range(8):
                        MM(bk[0:64, 0:512], xT3[:, k, t0:t0 + 64], win3[:, k, C_MLO:C_MLO + 512], [b_win] + xb_, [b_bk],
                           start=(k == 0), stop=(k == 7))
                    ACT(sigo[0:64, :], bk[0:64, 0:512], AF.Sigmoid, [b_bk], [b_sigo])
                    CP("pool", LcolF3[:, :, :], bc3(LG3[:, c, 0:4], [64, 4, 64], 2), [b_LG], [b_Lcol])
                    CP("pool", LcolG3[:, :, :], bc3(LG3[:, c, 4:8], [64, 4, 128], 2), [b_LG], [b_Lcol])
                    bkM, b_bkM = bank()
                    for h in range(4):
                        MM(bkM[0:64, h * 64:(h + 1) * 64], LcolF3[:, h, :], t1_f[:, :], [b_Lcol, b_const], [b_bkM])
                    for h in range(4):
                        MM(bkM[0:64, 256 + h * 64:256 + (h + 1) * 64], LcolG3[:, h, 0:64], t1_f[:, :], [b_Lcol, b_const], [b_bkM])
                    bkE, b_bkE = bank()
                    for h in range(4):
                        MM(bkE[:, h * 64:(h + 1) * 64], LcolG3[:, h, :], t1_f[:, :], [b_Lcol, b_const], [b_bkE])
                    Mf = bkM[0:64, 0:256].rearrange("p (h n) -> p h n", h=4)
                    Mg = bkM[0:64, 256:512].rearrange("p (h n) -> p h n", h=4)
                    TT("dve", t443, Mf, bc3(der3[:, c, 0:4], [64, 4, 64], 2), ALU.add, [b_bkM, b_der], [b_t44])
                    TT("dve", t443, t443, bc3(negc[:, :], [64, 4, 64], 1), ALU.add, [b_t44, b_const], [b_t44])
                    ACT(DTm3, t443, AF.Exp, [b_t44], [b_DT])
                    ACT(Ef3, Mf, AF.Exp, [b_bkM], [b_Ef])
                    TT("dve", qtil3, mlqT3[:, :, lo:lo + 64], Ef3, ALU.mult, [b_mlqT, b_Ef], [b_qtil])
                    STT(t443, Mg, -1.0, bc3(CS3[:, c, 4:8], [64, 4, 64], 2), ALU.mult, ALU.add, [b_bkM, b_CS, b_t44], [b_t44])
                    TT("dve", t443, t443, bc3(negs[:, :], [64, 4, 64], 1), ALU.add, [b_t44, b_const], [b_t44])
                    ACT(DecS3, t443, AF.Exp, [b_t44], [b_DecS])
                    TT("dve", t443, Mg, bc3(CS3[:, c, 4:8], [64, 4, 64], 2), ALU.subtract, [b_bkM, b_CS, b_t44], [b_t44])
                    TT("dve", t443, t443, bc3(negc[:, :], [64, 4, 64], 1), ALU.add, [b_t44, b_const], [b_t44])
                    ACT(DecT3, t443, AF.Exp, [b_t44], [b_DecT])
                    ACT(E128[:, :], bkE[:, 0:256], AF.Exp, [b_bkE], [b_E128])
                    TT("dve", gqt3, gdq3[:, :, lo:lo + 64], E1283, ALU.mult, [b_gdq, b_E128], [b_gqt])

                    bk, b_bk = bank()
                    for h in range(4):
                        MM(bk[0:64, h * 64:(h + 1) * 64], mlkT3[:, h, lo:lo + 64], mlqT3[:, h, lo:lo + 64], [b_mlkT, b_mlqT], [b_bk])
                    TT("dve", sint3, bk[0:64, 0:256].rearrange("p (h n) -> p h n", h=4), DTm3, ALU.mult, [b_bk, b_DT], [b_sint])
                    for hp in range(2):
                        bk, b_bk = bank()
                        for j in range(2):
                            h = hp * 2 + j
                            MM(bk[0:64, j * 129:(j + 1) * 129], qtil3[:, h, :], Cnb3[:, h, 0:129], [b_qtil, b_Cnb], [b_bk], start=True, stop=False)
                            MM(bk[0:64, j * 129:(j + 1) * 129], sint3[:, h, :], mlv13[:, h, 0:129], [b_sint, b_mlv1], [b_bk], start=False, stop=True)
                        bv_ = bk[0:64, 0:258].rearrange("p (j n) -> p j n", j=2)
                        ACT(rden[0:64, hp * 2:hp * 2 + 2], bv_[:, :, 128], AF.Abs, [b_bk], [b_rden])
                        TS("dve", rden[0:64, hp * 2:hp * 2 + 2], rden[0:64, hp * 2:hp * 2 + 2], 1.0, ALU.max, [b_rden], [b_rden])
                        P.op("dve", lambda e, hp=hp: e.reciprocal(rden[0:64, 4 + hp * 2:4 + hp * 2 + 2], rden[0:64, hp * 2:hp * 2 + 2]), [b_rden], [b_rden])
                        TT("dve", hml3[:, hp * 2:hp * 2 + 2, :], bv_[:, :, 0:128], bc3(rden[0:64, 4 + hp * 2:4 + hp * 2 + 2], [64, 2, 128], 2),
                           ALU.mult, [b_bk, b_rden], [b_hml])
                    TT("pool", hsq3, hml3, hml3, ALU.mult, [b_hml], [b_hsq])
                    P.op("dve", lambda e: e.tensor_reduce(ssum[0:64, 0:4], hsq3, AX.X, ALU.add), [b_hsq], [b_ssum])
                    rsqrt_(ssum[0:64, 4:8], ssum[0:64, 0:4], 1.0 / 128.0, 1e-6, [b_ssum], [b_ssum], (ssum[0:64, 0:4], b_ssum))
                    TT("dve", hml3, hml3, bc3(ssum[0:64, 4:8], [64, 4, 128], 2), ALU.mult, [b_hml, b_ssum], [b_hml])
                    TT("pool", hml[0:64, :], hml[0:64, :], mlnw[0:64, :], ALU.mult, [b_hml, b_small], [b_hml])
                    TT("pool", hml[0:64, :], hml[0:64, :], sigo[0:64, :], ALU.mult, [b_hml, b_sigo], [b_hml])
                    bk, b_bk = bank()
                    for h in range(4):
                        TR(bk[:, h * 64:(h + 1) * 64], hml[0:64, h * 128:(h + 1) * 128], ident_f[0:64, 0:64], [b_hml, b_const], [b_bk])
                    CP("act", mixT3[:, 0:4, :], bk[:, 0:256].rearrange("p (h n) -> p h n", h=4), [b_bk], [b_mixT])
                    TT("dve", kw3, mlk_tm[0:64, :].rearrange("p (h n) -> p h n", h=4), bc3(der3[:, c, 4:8], [64, 4, 64], 2), ALU.mult,
                       [b_mlk_tm, b_der], [b_kw])
                    TT("dve", Cn3, Cn3, bc3(der3[:, c, 8:12], [64, 4, 129], 2), ALU.mult, [b_Cn, b_der], [b_Cn])
                    for hp in range(2):
                        bk, b_bk = bank()
                        for j in range(2):
                            h = hp * 2 + j
                            MM(bk[0:64, j * 129:(j + 1) * 129], kw3[:, h, :], mlv13[:, h, 0:129], [b_kw, b_mlv1], [b_bk])
                        TT("dve", Cn3[:, hp * 2:hp * 2 + 2, :], Cn3[:, hp * 2:hp * 2 + 2, :], bk[0:64, 0:258].rearrange("p (j n) -> p j n", j=2),
                           ALU.add, [b_Cn, b_bk], [b_Cn])
                    CP("pool", Cnb3[:, :, 0:129], Cn3, [b_Cn], [b_Cnb])

                    bkk, b_bkk = bank()
                    bkv, b_bkv = bank()
                    for h in range(4):
                        TR(bkk[0:64, h * 128:(h + 1) * 128], gdkf3[:, h, lo:lo + 64], ident_f[:, :], [b_gdkf, b_const], [b_bkk])
                    for h in range(4):
                        TR(bkv[0:64, h * 128:(h + 1) * 128], gdvf3[:, h, lo:lo + 64], ident_f[:, :], [b_gdvf, b_const], [b_bkv])
                    ktm = bkk[0:64, 0:512].rearrange("p (h n) -> p h n", h=4)
                    vtm = bkv[0:64, 0:512].rearrange("p (h n) -> p h n", h=4)
                    TT("dve", bv3, vtm, bc3(beta3[:, c, :], [64, 4, 128], 2), ALU.mult, [b_bkv, b_beta], [b_bv])
                    TT("dve", kbg3, ktm, bc3(der3[:, c, 12:16], [64, 4, 128], 2), ALU.mult, [b_bkk, b_der], [b_kbg])
                    TT("dve", kend3, ktm, bc3(der3[:, c, 16:20], [64, 4, 128], 2), ALU.mult, [b_bkk, b_der], [b_kend])
                    bk, b_bk = bank()
                    for h in range(4):
                        MM(bk[0:64, h * 64:(h + 1) * 64], gdk3[:, h, lo:lo + 64], gdk3[:, h, lo:lo + 64], [b_gdk], [b_bk])
                    TT("dve", t443, bk[0:64, 0:256].rearrange("p (h n) -> p h n", h=4), bc3(der3[:, c, 20:24], [64, 4, 64], 2), ALU.mult,
                       [b_bk, b_der, b_t44], [b_t44])
                    TT("dve", P0f3, t443, DecS3, ALU.mult, [b_t44, b_DecS], [b_P0f])
                    pm, b_pm = Pm[0]
                    pt, b_pt = PT[0]
                    yb, b_yb = Yb[0]
                    pm3 = pm[0:64, :].rearrange("p (h n) -> p h n", h=4)
                    CP("pool", pm[0:64, :], P0f[0:64, :], [b_P0f], [b_pm])
                    bk, b_bk = bank()
                    for h in range(4):
                        TR(bk[0:64, h * 64:(h + 1) * 64], P0f3[:, h, :], ident_f[0:64, 0:64], [b_P0f, b_const], [b_bk])
                    CP("act", pt[0:64, :], bk[0:64, 0:256], [b_bk], [b_pt])
                    TT("dve", yb[0:64, :].rearrange("p (h n) -> p h n", h=4), bk[0:64, 0:256].rearrange("p (h n) -> p h n", h=4),
                       bc3(ident_f[0:64, 0:64], [64, 4, 64], 1), ALU.add, [b_bk, b_const], [b_yb])
                    cur = 0
                    for lev in range(1, 6):
                        pm, b_pm = Pm[cur]
                        pt, b_pt = PT[cur]
                        yb, b_yb = Yb[cur]
                        pmn, b_pmn = Pm[1 - cur]
                        ptn, b_ptn = PT[1 - cur]
                        ybn, b_ybn = Yb[1 - cur]
                        v3 = lambda a: a[0:64, :].rearrange("p (h n) -> p h n", h=4)
                        bkA, b_bkA = bank()
                        for h in range(4):
                            MM(bkA[0:64, h * 64:(h + 1) * 64], v3(pt)[:, h, :], v3(pm)[:, h, :], [b_pt, b_pm], [b_bkA])
                        CP("act", pmn[0:64, :], bkA[0:64, 0:256], [b_bkA], [b_pmn])
                        if lev < 5:
                            bkB, b_bkB = bank()
                            for h in range(4):
                                MM(bkB[0:64, h * 64:(h + 1) * 64], v3(pm)[:, h, :], v3(pt)[:, h, :], [b_pt, b_pm], [b_bkB])
                            CP("dve", ptn[0:64, :], bkB[0:64, 0:256], [b_bkB], [b_ptn])
                        bkC, b_bkC = bank()
                        for h in range(4):
                            MM(bkC[0:64, h * 64:(h + 1) * 64], v3(pmn)[:, h, :], v3(yb)[:, h, :], [b_pmn, b_yb], [b_bkC])
                        TT("dve", ybn[0:64, :], yb[0:64, :], bkC[0:64, 0:256], ALU.add, [b_yb, b_bkC], [b_ybn])
                        cur = 1 - cur
                    RT, b_RT = Yb[cur]
                    RT3 = RT[0:64, :].rearrange("p (h n) -> p h n", h=4)
                    bk, b_bk = bank()
                    for h in range(4):
                        MM(bk[:, h * 64:(h + 1) * 64], kbg3[:, h, :], RT3[:, h, :], [b_kbg, b_RT], [b_bk])
                    ACT(nwT[:, :], bk[:, 0:256], AF.Copy, [b_bk], [b_nwT], scale=-1.0)
                    bk, b_bk = bank()
                    for h in range(4):
                        MM(bk[0:64, h * 128:(h + 1) * 128], RT3[:, h, :], bv3[:, h, :], [b_RT, b_bv], [b_bk], start=True, stop=False)
                        MM(bk[0:64, h * 128:(h + 1) * 128], nwT3[:, h, :], Sb3[:, h, :], [b_nwT, b_Sb], [b_bk], start=False, stop=True)
                    CP("act", vnew[0:64, :], bk[0:64, 0:512], [b_bk], [b_vnew])
                    bk, b_bk = bank()
                    for h in range(4):
                        MM(bk[0:64, h * 64:(h + 1) * 64], gdk3[:, h, lo:lo + 64], gdq3[:, h, lo:lo + 64], [b_gdk, b_gdq], [b_bk])
                    TT("dve", qkd3, bk[0:64, 0:256].rearrange("p (h n) -> p h n", h=4), DecT3, ALU.mult, [b_bk, b_DecT], [b_qkd])
                    bko, b_bko = bank()
                    for h in range(4):
                        MM(bko[:, h * 64:(h + 1) * 64], Sb3[:, h, :], gqt3[:, h, :], [b_Sb, b_gqt], [b_bko], start=True, stop=False)
                        MM(bko[:, h * 64:(h + 1) * 64], vnew3[:, h, :], qkd3[:, h, :], [b_vnew, b_qkd], [b_bko], start=False, stop=True)
                    bk, b_bk = bank()
                    for h in range(4):
                        MM(bk[:, h * 128:(h + 1) * 128], kend3[:, h, :], vnew3[:, h, :], [b_kend, b_vnew], [b_bk])
                    TT("dve", Sst3, Sst3, bc3(EG3[:, c, :], [128, 4, 128], 2), ALU.mult, [b_S, b_EG], [b_S])
                    TT("dve", Sst[:, :], Sst[:, :], bk[:, 0:512], ALU.add, [b_S, b_bk], [b_S])
                    CP("pool", Sb[:, :], Sst[:, :], [b_S], [b_Sb])
                    ACT(osq[:, :], bko[:, 0:256], AF.Square, [b_bko], [b_osq])
                    bk, b_bk = bank()
                    MM(bk[:, 0:256], ones_f[:, :], osq[:, :], [b_const, b_osq], [b_bk])
                    rsqrt_(orstd[:, :], bk[:, 0:256], 1.0 / 128.0, 1e-6, [b_bk], [b_orstd], (osq[:, :], b_osq))
                    TT("dve", ot1[:, :], bko[:, 0:256], orstd[:, :], ALU.mult, [b_bko, b_orstd], [b_ot1])
                    STT(mixT3[:, 4:8, :], ot1[:, :].rearrange("p (h n) -> p h n", h=4), gdnw[:, 0:1], siluz3[:, :, lo:lo + 64],
                        ALU.mult, ALU.mult, [b_ot1, b_small, b_siluz], [b_mixT])
                    hp_ = []
                    hb_ = []
                    for n in range(2):
                        bk, b_bk = bank()
                        for f in range(8):
                            MM(bk[0:64, 0:512], mixT3[:, f, :], wout3[:, f, n * 512:(n + 1) * 512], [b_mixT, b_wout], [b_bk],
                               start=(f == 0), stop=(f == 7))
                        hp_.append(bk[0:64, 0:512])
                        hb_.append(b_bk)
                    ln_epilogue(64, g * GT + c * 64, hp_, hb_, src, src_bufs_fn(ci), G, Bt, b_gb, dst, dst_bufs_fn(ci), lnw[ci % 2], False)
            P.barrier()


        def xattn(l):
            AR.reset()
            W = []
            for i in range(4):
                w, bw = AR.bf16(8 * 1024)
                w3 = w.rearrange("p (k n) -> p k n", k=8)
                DMA("pool", w3, xa_w[i][l].rearrange("(k p) n -> p k n", p=128), [], [bw])
                W.append((w3, bw))
            (wq3, b_wq), (wk3, b_wk), (wv3, b_wv), (wo3, b_wo) = W
            G, b_gb = AR.f32(1024)
            Bt, _ = AR.f32(1024)
            load_bcast(G, lng[1][l:l + 1, :], 128, [b_gb])
            load_bcast(Bt, lnb[1][l:l + 1, :], 128, [b_gb])
            memT, b_memT = AR.bf16(8 * 256)
            memT3 = memT.rearrange("p (k n) -> p k n", k=8)
            xstg = AR.f32(1024)
            load_xT(mem_in, [], 0, 256, memT3, b_memT, xstg)
            kT, b_kT = AR.bf16(8 * 256)
            kT3 = kT.rearrange("p (k n) -> p k n", k=8)
            for f in range(8):
                bk, b_bk = bank()
                for k in range(8):
                    MM(bk[:, 0:256], wk3[:, k, f * 128:(f + 1) * 128], memT3[:, k, :], [b_wk, b_memT], [b_bk], start=(k == 0), stop=(k == 7))
                CP("dve", kT3[:, f, :], bk[:, 0:256], [b_bk], [b_kT])
            vv, b_vv = AR.bf16(2 * 1024)
            vv3 = vv.rearrange("p (m n) -> p m n", m=2)
            for mc in range(2):
                for n in range(2):
                    bk, b_bk = bank()
                    for k in range(8):
                        MM(bk[:, 0:512], memT3[:, k, mc * 128:(mc + 1) * 128], wv3[:, k, n * 512:(n + 1) * 512], [b_wv, b_memT], [b_bk],
                           start=(k == 0), stop=(k == 7))
                    CP("act", vv3[:, mc, n * 512:(n + 1) * 512], bk[:, 0:512], [b_bk], [b_vv])
            XG = 512
            xTg, b_xTg = AR.bf16(8 * XG)
            xT3 = xTg.rearrange("p (k n) -> p k n", k=8)
            qT, b_qT = AR.bf16(8 * XG)
            qT3 = qT.rearrange("p (k n) -> p k n", k=8)
            expT = [AR.bf16(XG) for _ in range(2)]
            rdn, b_rdn = AR.f32(XG)
            oT, b_oT = AR.bf16(8 * XG)
            oT3 = oT.rearrange("p (k n) -> p k n", k=8)
            lnw = []
            for _ in range(2):
                a1, bb1 = AR.f32(1024)
                a2, bb2 = AR.f32(12)
                a3, bb3 = AR.f32(2)
                a4, bb4 = AR.f32(2)
                lnw.append((a1, bb1, a2[:, :].rearrange("p (a b) -> p a b", a=2), bb2, a3, bb3, a4, bb4))
            for g in range(S // XG):
                rb_ = [b_xres[g * 8 + c] for c in range(8)]
                load_xT(xres, rb_, g * XG, XG, xT3, b_xTg, xstg)
                for f in range(8):
                    bk, b_bk = bank()
                    for k in range(8):
                        MM(bk[:, 0:XG], wq3[:, k, f * 128:(f + 1) * 128], xT3[:, k, :], [b_wq, b_xTg], [b_bk], start=(k == 0), stop=(k == 7))
                    ACT(qT3[:, f, :], bk[:, 0:XG], AF.Copy, [b_bk], [b_qT], scale=1.0 / 16.0)
                for h in range(4):
                    for mc in range(2):
                        bk, b_bk = bank()
                        for j in range(2):
                            MM(bk[:, 0:XG], kT3[:, 2 * h + j, mc * 128:(mc + 1) * 128], qT3[:, 2 * h + j, :], [b_kT, b_qT], [b_bk],
                               start=(j == 0), stop=(j == 1))
                        ACT(expT[mc][0][:, :], bk[:, 0:XG], AF.Exp, [b_bk], [expT[mc][1]])
                    bd, b_bd = bank()
                    for mc in range(2):
                        MM(bd[:, 0:XG], ones_b[:, :], expT[mc][0][:, :], [b_const, expT[mc][1]], [b_bd], start=(mc == 0), stop=(mc == 1))
                    P.op("dve", lambda e, bd=bd: e.reciprocal(rdn[:, :], bd[:, 0:XG]), [b_bd], [b_rdn])
                    for j in range(2):
                        bk, b_bk = bank()
                        for mc in range(2):
                            MM(bk[:, 0:XG], vv3[:, mc, (2 * h + j) * 128:(2 * h + j + 1) * 128], expT[mc][0][:, :], [b_vv, expT[mc][1]], [b_bk],
                               start=(mc == 0), stop=(mc == 1))
                        TT("dve", oT3[:, 2 * h + j, :], bk[:, 0:XG], rdn[:, :], ALU.mult, [b_bk, b_rdn], [b_oT])
                for t in range(XG // 128):
                    hp_, hb_ = [], []
                    for n in range(2):
                        bk, b_bk = bank()
                        for f in range(8):
                            MM(bk[:, 0:512], oT3[:, f, t * 128:(t + 1) * 128], wo3[:, f, n * 512:(n + 1) * 512], [b_oT, b_wo], [b_bk],
                               start=(f == 0), stop=(f == 7))
                        hp_.append(bk[:, 0:512])
                        hb_.append(b_bk)
                    r0 = g * XG + t * 128
                    rb2 = [b_xres[r0 // 64], b_xres[r0 // 64 + 1]]
                    ln_epilogue(128, r0, hp_, hb_, xres, rb2, G, Bt, b_gb, xres, rb2, lnw[t % 2], False)
            P.barrier()

        def moe(l, dst, dst_bufs_fn):
            AR.reset()
            G, b_gb = AR.f32(1024)
            Bt, _ = AR.f32(1024)
            load_bcast(G, lng[2][l:l + 1, :], 128, [b_gb])
            load_bcast(Bt, lnb[2][l:l + 1, :], 128, [b_gb])
            dest = dest_t
            b_dest = Buf("dest")
            gate, b_gate = AR.f32(32 * 4)
            gate3 = gate.rearrange("p (t k) -> p t k", t=32)
            mark = AR.off
            rw, b_rw = AR.f32(8 * NE)
            rw3 = rw.rearrange("p (k e) -> p k e", k=8)
            DMA("sp", rw3, r_w[l].rearrange("(k p) e -> p k e", p=128), [], [b_rw])
            rb, _ = AR.f32(NE)
            load_bcast(rb, r_b[l:l + 1, :], 128, [b_rw])
            cnt, b_cnt = AR.f32(NE)
            P.op("pool", lambda e: e.memset(cnt[:, :], 0.0), [], [b_cnt])
            xs2 = [AR.f32(1024) for _ in range(2)]
            xT32, b_xT32 = AR.f32(8 * 128)
            xT323 = xT32.rearrange("p (k n) -> p k n", k=8)
            xbf = [([AR.bf16(256)[0] for _c in range(4)], Buf("xbf%d" % i)) for i in range(2)]
            lg, b_lg = AR.f32(NE)
            mx8, b_mx8 = AR.f32(8)
            sm, b_sm = AR.f32(8)
            ex, b_ex = AR.f32(NE)
            msk, b_msk = AR.f32(NE)
            mskb, b_mskb = AR.bf16(NE)
            pos, b_pos = AR.f32(NE)
            ov, b_ov = AR.f32(NE)
            oh, b_oh = AR.f32(NE)
            tm, b_tm = AR.f32(NE)
            dsel, b_dsel = AR.f32(4)
            dsel16, b_dsel16 = AR.f32(16)
            b_xsall = Buf("xs_all")
            b_ysall = Buf("ys_all")
            for t in range(32):
                xs_, b_xs_ = xs2[t % 2]
                xb_, b_xb_ = xbf[t % 2]
                rbf = [b_xres[2 * t], b_xres[2 * t + 1]]
                DMA("sp", xs_[:, :], xres[t * 128:(t + 1) * 128, :], rbf, [b_xs_])
                for half in range(2):
                    bk, b_bk = bank()
                    for k in range(8):
                        TR(bk[:, k * 64:(k + 1) * 64], xs_[half * 64:(half + 1) * 64, k * 128:(k + 1) * 128],
                           ident_f[half * 64:(half + 1) * 64, half * 64:(half + 1) * 64], [b_xs_, b_const], [b_bk])
                    CP("dve", xT323[:, :, half * 64:(half + 1) * 64], bk[:, 0:512].rearrange("p (k n) -> p k n", k=8), [b_bk], [b_xT32])
                for cp in range(4):
                    CP("act", xb_[cp][:, :], xs_[:, cp * 256:(cp + 1) * 256], [b_xs_], [b_xb_])
                bl, b_bl = bank()
                for k in range(8):
                    MM(bl[:, 0:NE], xT323[:, k, :], rw3[:, k, :], [b_xT32, b_rw], [b_bl], start=(k == 0), stop=(k == 7))
                TT("dve", lg[:, :], bl[:, 0:NE], rb[:, :], ALU.add, [b_bl, b_rw], [b_lg])
                P.op("dve", lambda e: e.max(mx8[:, :], lg[:, :]), [b_lg], [b_mx8])
                TS("dve", sm[:, 0:1], mx8[:, 0:1], -1.0, ALU.mult, [b_mx8], [b_sm])
                ACT(ex[:, :], lg[:, :], AF.Exp, [b_lg, b_sm], [b_ex], bias=sm[:, 0:1])
                TS("dve", msk[:, :], lg[:, :], mx8[:, 3:4], ALU.is_ge, [b_lg, b_mx8], [b_msk])
                TT("dve", ex[:, :], ex[:, :], msk[:, :], ALU.mult, [b_ex, b_msk], [b_ex])
                P.op("dve", lambda e: e.tensor_reduce(sm[:, 1:2], ex[:, :], AX.X, ALU.add), [b_ex], [b_sm])
                P.op("dve", lambda e: e.reciprocal(sm[:, 2:3], sm[:, 1:2]), [b_sm], [b_sm])
                TS("dve", ex[:, :], ex[:, :], sm[:, 2:3], ALU.mult, [b_ex, b_sm], [b_ex])
                CP("pool", mskb[:, :], msk[:, :], [b_msk], [b_mskb])
                bp, b_bp = bank()
                MM(bp[:, 0:NE], tri_b[:, :], mskb[:, :], [b_const, b_mskb], [b_bp])
                MM(bp[:, NE:2 * NE], ones_b[:, :], mskb[:, :], [b_const, b_mskb], [b_bp])
                TT("dve", pos[:, :], bp[:, 0:NE], cnt[:, :], ALU.add, [b_bp, b_cnt], [b_pos])
                TT("dve", cnt[:, :], cnt[:, :], bp[:, NE:2 * NE], ALU.add, [b_bp, b_cnt], [b_cnt])
                TS("dve", ov[:, :], pos[:, :], float(CAP), ALU.is_ge, [b_pos], [b_ov])
                STT(pos[:, :], ov[:, :], 1.0e7, pos[:, :], ALU.mult, ALU.add, [b_ov, b_pos], [b_pos])
                TT("dve", pos[:, :], pos[:, :], eoff[:, :], ALU.add, [b_pos, b_const], [b_pos])
                TS("dve", ov[:, :], ov[:, :], -1.0, ALU.mult, [b_ov], [b_ov], s2=1.0, op1=ALU.add)
                TT("dve", ex[:, :], ex[:, :], ov[:, :], ALU.mult, [b_ex, b_ov], [b_ex])
                for k in range(4):
                    TS("dve", oh[:, :], lg[:, :], mx8[:, k:k + 1], ALU.is_equal, [b_lg, b_mx8], [b_oh])
                    TT("dve", tm[:, :], oh[:, :], pos[:, :], ALU.mult, [b_oh, b_pos], [b_tm])
                    P.op("dve", lambda e, k=k: e.tensor_reduce(dsel[:, k:k + 1], tm[:, :], AX.X, ALU.add), [b_tm], [b_dsel])
                    TT("dve", tm[:, :], oh[:, :], ex[:, :], ALU.mult, [b_oh, b_ex], [b_tm])
                    P.op("dve", lambda e, k=k, t=t: e.tensor_reduce(gate3[:, t, k:k + 1], tm[:, :], AX.X, ALU.add), [b_tm], [b_gate])
                TS("dve", dsel16[:, :].rearrange("p (k c) -> p k c", k=4), bc3(dsel[:, 0:4], [128, 4, 4], 2), 4.0, ALU.mult, [b_dsel], [b_dsel16])
                TT("dve", dsel16[:, :], dsel16[:, :], cpoff[:, :], ALU.add, [b_dsel16, b_const], [b_dsel16])
                CP("dve", dest[:, t * 16:t * 16 + 16], dsel16[:, :], [b_dsel16], [b_dest])
                for k in range(4):
                    for cp in range(4):
                        q = idx_ctr[0] % 8
                        idx_ctr[0] += 1
                        CP("dve", idx_t[q][:, :], dest[:, t * 16 + k * 4 + cp:t * 16 + k * 4 + cp + 1], [b_dest], [b_idx[q]])
                        P.dma("pool", lambda e, q=q, xb_=xb_, cp=cp: e.indirect_dma_start(
                            out=xs_d, out_offset=bass.IndirectOffsetOnAxis(ap=idx_t[q][:, :], axis=0),
                            in_=xb_[cp][:, :], in_offset=None, bounds_check=bc_reg(e), oob_is_err=False), [b_xb_, b_idx[q]], [b_xsall])
            P.barrier()
            AR.off = mark
            wgu = [AR.bf16(8 * 2048) for _ in range(2)]
            wdn = [AR.bf16(8 * 1024) for _ in range(2)]
            bdn = [AR.f32(1024) for _ in range(1)]
            bgr, b_bgr = AR.f32(128)
            bguT, b_bguT = AR.f32(NE * 16)
            for q in range(4):
                DMA("sp", bgr[:, :], b_gu[l].rearrange("e (c p) -> (e c) p", p=128)[q * 128:(q + 1) * 128, :], [], [b_bgr])
                bk, b_bk = bank()
                TR(bk[:, 0:128], bgr[:, :], ident_f[:, :], [b_bgr, b_const], [b_bk])
                CP("dve", bguT[:, q * 128:(q + 1) * 128], bk[:, 0:128], [b_bk], [b_bguT])
            bgv = bguT[:, :].rearrange("p (e c) -> p e c", c=16)
            TS("dve", bgv[:, :, 8:16], bgv[:, :, 8:16], 1.0, ALU.add, [b_bguT], [b_bguT])
            xsel, b_xsel = AR.bf16(NCAP * 1024)
            xsel3 = xsel.rearrange("p (j d) -> p j d", j=NCAP)
            xselT, b_xselT = AR.bf16(8 * CAP)
            xselT3 = xselT.rearrange("p (k n) -> p k n", k=8)
            actT, b_actT = AR.bf16(8 * CAP)
            actT3 = actT.rearrange("p (k n) -> p k n", k=8)
            HC = CAP // 2
            gtmp, b_gtmp = AR.f32(HC)
            stmp, b_stmp = AR.f32(HC)
            ltmp, b_ltmp = AR.f32(HC)
            ysb = [AR.f32(1024) for _ in range(1)]
            NST, PD = 5, 4
            wstage = [AR.f32(1024) for _ in range(NST)]
            wctr = [0]

            def wsteps(e2):
                wg_, b_wg_ = wgu[e2 % 2]
                wg3_ = wg_.rearrange("p (k n) -> p k n", k=8)
                wd_, b_wd_ = wdn[e2 % 2]
                wd3_ = wd_.rearrange("p (k n) -> p k n", k=8)
                pieces = []
                for k in range(8):
                    for hf in range(2):
                        pieces.append((w_gu[l, e2, k * 128:(k + 1) * 128, hf * 1024:(hf + 1) * 1024], wg3_[:, k, hf * 1024:(hf + 1) * 1024], b_wg_))
                for k in range(8):
                    pieces.append((w_dn[l, e2, k * 128:(k + 1) * 128, :], wd3_[:, k, :], b_wd_))
                slots = []

                def mk_dma(i):
                    def f():
                        sl = wctr[0] % NST
                        wctr[0] += 1
                        slots.append(sl)
                        DMA("sp", wstage[sl][0][:, :], pieces[i][0], [], [wstage[sl][1]])
                    return f

                def mk_cast(i):
                    def f():
                        sl = slots[i]
                        CP("act", pieces[i][1], wstage[sl][0][:, :], [wstage[sl][1]], [pieces[i][2]])
                    return f
                n = len(pieces)
                steps = []
                for i in range(n + PD):
                    fs = []
                    if i - PD >= 0:
                        fs.append(mk_cast(i - PD))
                    if i < n:
                        fs.append(mk_dma(i))
                    steps.append(lambda fs=fs: [f() for f in fs])
                return steps

            for e_ in range(NE):
                wg, b_wg = wgu[e_ % 2]
                wg3 = wg.rearrange("p (k n) -> p k n", k=8)
                wd, b_wd = wdn[e_ % 2]
                wd3 = wd.rearrange("p (k n) -> p k n", k=8)
                bd_, b_bd_ = bdn[0]
                if e_ == 0:
                    for step in wsteps(0):
                        step()
                pending = wsteps(e_ + 1) if e_ + 1 < NE else []
                DMA("sp", bd_[:, :], b_dn[l, e_:e_ + 1, :].partition_broadcast(128), [], [b_bd_])
                DMA("sp", xsel3, xs_d[e_ * CAP * 4:(e_ + 1) * CAP * 4, :].rearrange("(j p c) d -> p j (c d)", p=128, c=4), [b_xsall], [b_xsel])
                for j in range(NCAP):
                    for k in range(8):
                        TR(psb[:, k * 128:(k + 1) * 128], xsel3[:, j, k * 128:(k + 1) * 128], ident_b[:, :], [b_xsel, b_const], [b_psb])
                    CP("act", xselT3[:, :, j * 128:(j + 1) * 128], psb[:, :].rearrange("p (k n) -> p k n", k=8), [b_psb], [b_xselT])
                for f in range(8):
                    for hh in range(2):
                        bg, b_bg = bank()
                        for k in range(8):
                            MM(bg[:, 0:HC], wg3[:, k, f * 128:(f + 1) * 128], xselT3[:, k, hh * HC:(hh + 1) * HC], [b_wg, b_xselT], [b_bg],
                               start=(k == 0), stop=(k == 7))
                        bl, b_bl = bank()
                        for k in range(8):
                            MM(bl[:, 0:HC], wg3[:, k, 1024 + f * 128:1024 + (f + 1) * 128], xselT3[:, k, hh * HC:(hh + 1) * HC], [b_wg, b_xselT], [b_bl],
                               start=(k == 0), stop=(k == 7))
                        c0 = e_ * 16 + f
                        TS("dve", gtmp[:, :], bg[:, 0:HC], bguT[:, c0:c0 + 1], ALU.add, [b_bg, b_bguT], [b_gtmp], s2=7.0, op1=ALU.min)
                        ACT(stmp[:, :], gtmp[:, :], AF.Sigmoid, [b_gtmp], [b_stmp], scale=1.702)
                        TS("dve", ltmp[:, :], bl[:, 0:HC], bguT[:, c0 + 8:c0 + 9], ALU.add, [b_bl, b_bguT], [b_ltmp], s2=8.0, op1=ALU.min)
                        TT("pool", gtmp[:, :], gtmp[:, :], stmp[:, :], ALU.mult, [b_gtmp, b_stmp], [b_gtmp])
                        STT(actT3[:, f, hh * HC:(hh + 1) * HC], ltmp[:, :], -6.0, gtmp[:, :], ALU.max, ALU.mult, [b_gtmp, b_ltmp], [b_actT])
                        if pending:
                            pending.pop(0)()
                for j in range(NCAP):
                    ys_, b_ys_ = ysb[0]
                    for n in range(2):
                        bk, b_bk = bank()
                        for f in range(8):
                            MM(bk[:, 0:512], actT3[:, f, j * 128:(j + 1) * 128], wd3[:, f, n * 512:(n + 1) * 512], [b_actT, b_wd], [b_bk],
                               start=(f == 0), stop=(f == 7))
                        TT("dve", ys_[:, n * 512:(n + 1) * 512], bk[:, 0:512], bd_[:, n * 512:(n + 1) * 512], ALU.add, [b_bk, b_bd_], [b_ys_])
                        if pending:
                            pending.pop(0)()
                    DMA("sp", ys_d[(e_ * CAP + j * 128) * 4:(e_ * CAP + (j + 1) * 128) * 4, :].rearrange("(p c) d -> p (c d)", c=4), ys_[:, :], [b_ys_], [b_ysall])
                while pending:
                    pending.pop(0)()
            P.barrier()
            AR.off = mark
            gth = [([AR.f32(256)[0] for _c in range(4)], Buf("gth%d" % i)) for i in range(4)]
            for k in range(4):
                for cp in range(4):
                    P.op("pool", lambda e, k=k, cp=cp: e.memset(gth[k][0][cp][:, :], 0.0), [], [gth[k][1]])
            accs = [AR.f32(1024) for _ in range(2)]
            lnw = []
            for _ in range(2):
                a1, bb1 = AR.f32(1024)
                a2, bb2 = AR.f32(12)
                a3, bb3 = AR.f32(2)
                a4, bb4 = AR.f32(2)
                lnw.append((a1, bb1, a2[:, :].rearrange("p (a b) -> p a b", a=2), bb2, a3, bb3, a4, bb4))
            for t in range(32):
                for k in range(4):
                    for cp in range(4):
                        q = idx_ctr[0] % 8
                        idx_ctr[0] += 1
                        CP("dve", idx_t[q][:, :], dest[:, t * 16 + k * 4 + cp:t * 16 + k * 4 + cp + 1], [b_dest], [b_idx[q]])
                        P.dma("pool", lambda e, k=k, q=q, cp=cp: e.indirect_dma_start(
                            out=gth[k][0][cp][:, :], out_offset=None, in_=ys_d,
                            in_offset=bass.IndirectOffsetOnAxis(ap=idx_t[q][:, :], axis=0),
                            bounds_check=bc_reg(e), oob_is_err=False), [b_ysall, b_idx[q]], [gth[k][1]])
                ac, b_ac = accs[t % 2]
                for cp in range(4):
                    cs_ = slice(cp * 256, (cp + 1) * 256)
                    TS("dve", ac[:, cs_], gth[0][0][cp][:, :], gate3[:, t, 0:1], ALU.mult, [gth[0][1], b_gate], [b_ac])
                    for k in range(1, 4):
                        STT(ac[:, cs_], gth[k][0][cp][:, :], gate3[:, t, k:k + 1], ac[:, cs_], ALU.mult, ALU.add, [gth[k][1], b_gate, b_ac], [b_ac])
                rb2 = [b_xres[2 * t], b_xres[2 * t + 1]]
                ln_epilogue(128, t * 128, [ac[:, 0:512], ac[:, 512:1024]], [b_ac, b_ac], xres, rb2, G, Bt, b_gb, dst, dst_bufs_fn(t), lnw[t % 2], False)
            P.barrier()

        for l in range(n_layers):
            if "mixer" in skip:
                AR.reset()
                cpb = [AR.f32(1024) for _ in range(2)]
                for t in range(32):
                    a, bb = cpb[t % 2]
                    DMA("sp", a[:, :], x_in[t * 128:(t + 1) * 128, :], [], [bb])
                    DMA("sp", xres[t * 128:(t + 1) * 128, :], a[:, :], [bb], [b_xres[2 * t], b_xres[2 * t + 1]])
                P.barrier()
            elif l == 0:
                mixer(l, x_in, lambda ci: [], xres, lambda ci: [b_xres[ci]])
            else:
                mixer(l, xres, lambda ci: [b_xres[ci]], xres, lambda ci: [b_xres[ci]])
            if stop_after == (l, "mixer"):
                break
            if "xattn" not in skip:
                xattn(l)
            if stop_after == (l, "xattn"):
                break
            last = (l == n_layers - 1) and stop_after is None
            if last:
                moe(l, out_d, lambda t: [b_out])
            else:
                moe(l, xres, lambda t: [b_xres[2 * t], b_xres[2 * t + 1]])
            if stop_after == (l, "moe"):
                break
        if stop_after is not None:
            AR.reset()
            fin = [AR.f32(1024) for _ in range(2)]
            for t in range(32):
                a, bb = fin[t % 2]
                DMA("sp", a[:, :], xres[t * 128:(t + 1) * 128, :], [b_xres[2 * t], b_xres[2 * t + 1]], [bb])
                DMA("sp", out_d[t * 128:(t + 1) * 128, :], a[:, :], [bb], [b_out])
        P.barrier()
        print("ops:", P.n_ops, {e: len(s) for e, s in P.streams.items()})
        P.emit()
    return nc


def make_consts():
    i = np.arange(64)
    c = {
        "c_ident": np.eye(128, dtype=np.float32),
        "c_t1": (i[:, None] <= i[None, :]).astype(np.float32),
        "c_negc": np.where(i[None, :] >= i[:, None], 0.0, NEG).astype(np.float32),
        "c_negs": np.where(i[None, :] < i[:, None], 0.0, NEG).astype(np.float32),
        "c_tri": (np.arange(128)[:, None] < np.arange(128)[None, :]).astype(np.float32),
        "c_eoff": np.tile((np.arange(NE) * CAP).astype(np.float32)[None, :], (128, 1)),
        "c_ones": np.ones((128, 128), np.float32),
        "c_cp": np.tile(np.arange(4, dtype=np.float32)[None, :], (128, 4)),
    }
    return c


_NAMES = ["w_in", "mlstm_b_i", "mlstm_b_f", "mlstm_norm_w", "gdn_conv_w", "gdn_a_log", "gdn_dt_bias", "gdn_norm_w", "w_out",
          "ln1_g", "ln1_b", "xa_wq", "xa_wk", "xa_wv", "xa_wo", "ln2_g", "ln2_b", "router_w", "router_b",
          "exp_w_gu", "exp_b_gu", "exp_w_down", "exp_b_down", "ln3_g", "ln3_b"]


def run(inputs, n_layers=L, stop_after=None, cores=8, max_groups=None, small=False, skip=(), trace=False):
    nc = build(n_layers, stop_after, max_groups=max_groups, small=small, skip=skip)
    consts = make_consts()
    shared = {k: np.ascontiguousarray(np.asarray(inputs[k], dtype=np.float32)) for k in _NAMES}
    if small:
        names = small if isinstance(small, (set, tuple, list)) else ("xa_wq", "xa_wk", "xa_wv", "xa_wo", "router_w", "exp_w_gu", "exp_b_gu", "exp_w_down", "exp_b_down")
        for k in names:
            shared[k] = np.zeros([1] * shared[k].ndim, np.float32)
    in_maps = []
    for c in range(cores):
        m = dict(shared)
        m.update(consts)
        m["x"] = np.ascontiguousarray(np.asarray(inputs["x"][c], dtype=np.float32))
        m["mem"] = np.ascontiguousarray(np.asarray(inputs["mem"][c], dtype=np.float32))
        in_maps.append(m)
    res = run_bass_kernel_spmd(nc, in_maps, core_ids=list(range(cores)), **({"trace": True} if trace else {}))
    if trace:
        print("EXEC_TIME_NS", res.exec_time_ns)
    return np.stack([np.asarray(r["out"]) for r in res.results], axis=0)


def kernel(**inputs):
    return run(inputs).astype(np.float32)
```

```python
import numpy as np
from contextlib import ExitStack
import concourse.bass as bass
import concourse.mybir as mybir
from concourse.bass_utils import run_bass_kernel_spmd

F32 = mybir.dt.float32
BF16 = mybir.dt.bfloat16
I32 = mybir.dt.int32
AF = mybir.ActivationFunctionType
ALU = mybir.AluOpType
AX = mybir.AxisListType

S = 4096
D = 1024
L = 4
NE = 32
CAP = 1024
NCAP = CAP // 128
ALPHA = (2.0 * L) ** 0.25
NEG = -30000.0
GT = 256
NCG = GT // 64

import os as _os
SAME_ENGINE_SYNC = _os.environ.get("SES", "1") == "1"
EPOCH = 16000
N_DMA_SEMS = 24

C_MLQ, C_MLK, C_MLV, C_MLO, C_MLI, C_MLF = 0, 256, 512, 1024, 1536, 1540
C_GDQ, C_GDK, C_GDV, C_GDZ, C_GDB, C_GDA = 1544, 2056, 2568, 3080, 3592, 3596


class Buf:
    __slots__ = ("name", "w", "r", "excl")

    def __init__(self, name, excl=False):
        self.name = name
        self.w = None
        self.r = []
        self.excl = excl


class Prog:
    ENGS = ("pe", "dve", "act", "pool", "sp")

    def __init__(self, nc, es):
        self.nc = nc
        self.es = es
        self.streams = {e: [] for e in self.ENGS}
        self.count = {e: 0 for e in self.ENGS}
        self.known = {e: {} for e in self.ENGS}
        self.sems = {}
        self.dma_next = {e: 0 for e in self.ENGS}
        self.dma_uses = {}
        self.n_ops = 0

    def sem(self, key):
        s = self.sems.get(key)
        if s is None:
            s = self.es.enter_context(self.nc.semaphore("s_%s_%s" % key))
            self.sems[key] = s
        return s

    def _wait(self, eng, tok):
        key, val = tok
        if key[0] == eng and (eng == "pe" or not SAME_ENGINE_SYNC):
            return
        if self.known[eng].get(key, 0) >= val:
            return
        self.known[eng][key] = val
        self.streams[eng].append(("wait", key, val))

    def _deps(self, eng, reads, writes):
        for b in reads:
            if b.w is not None:
                self._wait(eng, b.w)
        for b in writes:
            if b.w is not None:
                self._wait(eng, b.w)
            for t in b.r:
                self._wait(eng, t)

    def _commit(self, tok, reads, writes):
        for b in reads:
            b.r.append(tok)
        for b in writes:
            b.w = tok
            b.r = []

    def op(self, eng, fn, reads=(), writes=()):
        ex = [b for b in reads if b.excl]
        if ex:
            reads = [b for b in reads if not b.excl]
            writes = list(writes) + ex
        self._deps(eng, reads, writes)
        n = self.count[eng]
        self.count[eng] = n + 1
        key = (eng, n // EPOCH)
        self.sem(key)
        tok = (key, n % EPOCH + 1)
        self.streams[eng].append(("op", fn, key, 1))
        self._commit(tok, reads, writes)
        self.n_ops += 1
        return tok

    def dma(self, eng, fn, reads=(), writes=()):
        self._deps(eng, reads, writes)
        i = self.dma_next[eng]
        self.dma_next[eng] = (i + 1) % N_DMA_SEMS
        key = ("d" + eng, i)
        uses = self.dma_uses.get(key, 0)
        self.sem(key)
        if uses > 0:
            self._wait(eng, (key, 16 * uses))
        self.dma_uses[key] = uses + 1
        tok = (key, 16 * (uses + 1))
        self.streams[eng].append(("op", fn, key, 16))
        self._commit(tok, reads, writes)
        self.n_ops += 1
        return tok

    def barrier(self):
        toks = []
        for e in self.ENGS:
            n = self.count[e]
            if n > 0:
                toks.append(((e, (n - 1) // EPOCH), (n - 1) % EPOCH + 1))
        for k, u in self.dma_uses.items():
            toks.append((k, 16 * u))
        for e in self.ENGS:
            for t in toks:
                if t[0][0] == e:
                    continue
                self._wait(e, t)

    def emit(self):
        nc = self.nc
        engmap = {"pe": "tensor", "dve": "vector", "act": "scalar", "pool": "gpsimd", "sp": "sync"}
        with nc.Block() as block:
            for e in self.ENGS:
                stream = self.streams[e]
                if not stream:
                    continue

                def body(engine, stream=stream):
                    for it in stream:
                        if it[0] == "wait":
                            engine.wait_ge(self.sems[it[1]], it[2])
                        else:
                            it[1](engine).then_inc(self.sems[it[2]], it[3])

                getattr(block, engmap[e])(body)


class Arena:
    def __init__(self, t, ncols):
        self.t = t
        self.n = ncols
        self.off = 0
        self.k = 0

    def reset(self):
        self.off = 0

    def _take(self, c32):
        assert self.off + c32 <= self.n, ("arena overflow", self.off, c32, self.n)
        a = self.t[:, self.off:self.off + c32]
        self.off += c32
        self.k += 1
        return a, Buf("ar%d" % self.k)

    def f32(self, cols):
        return self._take(cols)

    def bf16(self, cols):
        a, b = self._take((cols + 1) // 2)
        return a.bitcast(BF16), b

    def i32(self, cols):
        a, b = self._take(cols)
        return a.bitcast(I32), b


def bc3(ap, shape, axis):
    return ap.unsqueeze(axis).to_broadcast(list(shape))


def build(n_layers=L, stop_after=None, dbg=False, max_groups=None, small=False, skip=()):
    nc = bass.Bass("TRN2", target_bir_lowering=False)
    es = ExitStack()

    BIG = ("xa_wq", "xa_wk", "xa_wv", "xa_wo", "router_w", "exp_w_gu", "exp_b_gu", "exp_w_down", "exp_b_down")

    def din(name, shape, dt=F32):
        if small and name in (small if isinstance(small, (set, tuple, list)) else BIG):
            shape = [1] * len(shape)
        return nc.dram_tensor(name, list(shape), dt, kind="ExternalInput").ap()

    x_in = din("x", [S, D])
    mem_in = din("mem", [256, D])
    w_in = din("w_in", [L, D, 3600])
    b_i = din("mlstm_b_i", [L, 4])
    b_f = din("mlstm_b_f", [L, 4])
    ml_nw = din("mlstm_norm_w", [L, 512])
    conv_w = din("gdn_conv_w", [L, 4, 1536])
    a_log = din("gdn_a_log", [L, 4])
    dt_b = din("gdn_dt_bias", [L, 4])
    gd_nw = din("gdn_norm_w", [L, 128])
    w_out = din("w_out", [L, D, D])
    lng = [din("ln%d_g" % i, [L, D]) for i in (1, 2, 3)]
    lnb = [din("ln%d_b" % i, [L, D]) for i in (1, 2, 3)]
    xa_w = [din("xa_w" + n, [L, D, D]) for n in "qkvo"]
    r_w = din("router_w", [L, D, NE])
    r_b = din("router_b", [L, NE])
    w_gu = din("exp_w_gu", [L, NE, D, 2 * D])
    b_gu = din("exp_b_gu", [L, NE, 2 * D])
    w_dn = din("exp_w_down", [L, NE, D, D])
    b_dn = din("exp_b_down", [L, NE, D])
    c_ident = din("c_ident", [128, 128])
    c_t1 = din("c_t1", [64, 64])
    c_negc = din("c_negc", [64, 64])
    c_negs = din("c_negs", [64, 64])
    c_tri = din("c_tri", [128, 128])
    c_eoff = din("c_eoff", [128, NE])
    c_ones = din("c_ones", [128, 128])
    c_cp = din("c_cp", [128, 16])
    out_d = nc.dram_tensor("out", [S, D], F32, kind="ExternalOutput").ap()
    xres = nc.dram_tensor("xres", [S, D], F32, kind="Internal").ap()
    xs_d = nc.dram_tensor("xs_d", [NE * CAP, D], BF16, kind="Internal").ap()
    ys_d = nc.dram_tensor("ys_d", [NE * CAP, D], F32, kind="Internal").ap()
    b_xres = [Buf("xres%d" % i) for i in range(64)]
    b_out = Buf("out")
    b_xs = [Buf("xs%d" % e) for e in range(NE)]
    b_ys = [Buf("ys%d" % e) for e in range(NE)]

    P = Prog(nc, es)

    def sb(name, shape, dt):
        return es.enter_context(nc.sbuf_tensor(name, list(shape), dt))

    with es:
        ident_f = sb("ident_f", [128, 128], F32)
        ident_b = sb("ident_b", [128, 128], BF16)
        ones_f = sb("ones_f", [128, 128], F32)
        ones_b = sb("ones_b", [128, 128], BF16)
        tri_b = sb("tri_b", [128, 128], BF16)
        t1_f = sb("t1_f", [64, 64], F32)
        negc = sb("negc", [64, 64], F32)
        negs = sb("negs", [64, 64], F32)
        eoff = sb("eoff", [128, NE], F32)
        dest_t = sb("dest_t", [128, 512], I32)
        cpoff = sb("cpoff", [128, 16], F32)
        idx_t = [sb("idx%d" % i, [128, 1], I32) for i in range(8)]
        b_idx = [Buf("idx%d" % i) for i in range(8)]
        idx_ctr = [0]
        b_const = Buf("const")
        ARENA_COLS = 49000
        arena_t = sb("arena", [128, ARENA_COLS], F32)
        AR = Arena(arena_t, ARENA_COLS)
        banks = [es.enter_context(nc.psum_tensor("ps%d" % i, [128, 512], F32)) for i in range(7)]
        b_banks = [Buf("bank%d" % i, True) for i in range(7)]
        psb = es.enter_context(nc.psum_tensor("psb", [128, 1024], BF16))
        b_psb = Buf("psb", True)
        bank_ctr = [0]

        def bank():
            i = bank_ctr[0] % 7
            bank_ctr[0] += 1
            return banks[i], b_banks[i]

        def MM(out, lhsT, rhs, R, W, start=True, stop=True):
            P.op("pe", lambda e: e.matmul(out, lhsT, rhs, start=start, stop=stop), R, W)

        def TR(out, in_, idn, R, W):
            P.op("pe", lambda e: e.transpose(out, in_, idn), R, W)

        def TT(eng, out, a, b, op, R, W):
            P.op(eng, lambda e: e.tensor_tensor(out, a, b, op), R, W)

        def TS(eng, out, a, s1, op0, R, W, s2=None, op1=None):
            if op1 is None:
                P.op(eng, lambda e: e.tensor_scalar(out, a, s1, None, op0), R, W)
            else:
                P.op(eng, lambda e: e.tensor_scalar(out, a, s1, s2, op0, op1), R, W)

        def STT(out, a, s, b, op0, op1, R, W):
            P.op("dve", lambda e: e.scalar_tensor_tensor(out, a, s, b, op0, op1), R, W)

        def ACT(out, in_, func, R, W, bias=0.0, scale=1.0):
            P.op("act", lambda e: e.activation(out, in_, func, bias=bias, scale=scale), R, W)

        def CP(eng, out, in_, R, W):
            if eng == "act":
                P.op("act", lambda e: e.copy(out, in_), R, W)
            else:
                P.op(eng, lambda e: e.tensor_copy(out, in_), R, W)

        def DMA(eng, out, in_, R, W):
            return P.dma(eng, lambda e: e.dma_start(out=out, in_=in_), R, W)

        def rsqrt_(out, in_, scale, eps, R, W, tmp):
            ACT(tmp[0], in_, AF.Sqrt, R, [tmp[1]], bias=eps, scale=scale)
            P.op("dve", lambda e: e.reciprocal(out, tmp[0]), [tmp[1]], W)

        DMA("sp", ident_f[:], c_ident, [], [b_const])
        DMA("sp", ones_f[:], c_ones, [], [b_const])
        DMA("sp", t1_f[:], c_t1, [], [b_const])
        DMA("sp", negc[:], c_negc, [], [b_const])
        DMA("sp", negs[:], c_negs, [], [b_const])
        DMA("sp", eoff[:], c_eoff, [], [b_const])
        DMA("sp", cpoff[:], c_cp, [], [b_const])
        DMA("pool", ident_b[:], c_ident, [], [b_const])
        DMA("pool", ones_b[:], c_ones, [], [b_const])
        DMA("pool", tri_b[:], c_tri, [], [b_const])
        P.barrier()

        bc_state = {}

        def bc_reg(e):
            if "r" not in bc_state:
                bc_state["r"] = e.alloc_register("bc")
                e.reg_mov(bc_state["r"], NE * CAP - 1)
            return bc_state["r"]

        def ln_epilogue(Pn, row0, hparts, hbufs, xsrc, xsrc_bufs, G, Bt, b_gb, dst, dst_bufs, work, want_xT):
            xo, b_xo, stt_, b_st, mv, b_mv, sc, b_sc = work
            DMA("sp", xo[0:Pn, :], xsrc[row0:row0 + Pn, :], xsrc_bufs, [b_xo])
            for n in range(2):
                STT(xo[0:Pn, n * 512:(n + 1) * 512], xo[0:Pn, n * 512:(n + 1) * 512], ALPHA, hparts[n],
                    ALU.mult, ALU.add, [b_xo, hbufs[n]], [b_xo])
            for n in range(2):
                P.op("dve", lambda e, n=n: e.bn_stats(stt_[0:Pn, n, :], xo[0:Pn, n * 512:(n + 1) * 512]), [b_xo], [b_st])
            P.op("dve", lambda e: e.bn_aggr(mv[0:Pn, :], stt_[0:Pn, :, :]), [b_st], [b_mv])
            rsqrt_(sc[0:Pn, 1:2], mv[0:Pn, 1:2], 1.0, 1e-5, [b_mv], [b_sc], (sc[0:Pn, 0:1], b_sc))
            TS("dve", xo[0:Pn, :], xo[0:Pn, :], mv[0:Pn, 0:1], ALU.subtract, [b_xo, b_mv, b_sc], [b_xo],
               s2=sc[0:Pn, 1:2], op1=ALU.mult)
            TT("pool", xo[0:Pn, :], xo[0:Pn, :], G[0:Pn, :], ALU.mult, [b_xo, b_gb], [b_xo])
            TT("pool", xo[0:Pn, :], xo[0:Pn, :], Bt[0:Pn, :], ALU.add, [b_xo, b_gb], [b_xo])
            DMA("sp", dst[row0:row0 + Pn, :], xo[0:Pn, :], [b_xo], dst_bufs)

        def load_bcast(dst, src_row, Pn, W):
            DMA("sp", dst[0:Pn, :], src_row.partition_broadcast(Pn), [], W)

        def load_xT(src, src_bufs, row0, ntok, xTg3, b_xTg, stg):
            for t in range(ntok // 128):
                st_, b_st_ = stg
                DMA("sp", st_[:, :], src[row0 + t * 128:row0 + (t + 1) * 128, :], src_bufs, [b_st_])
                for half in range(2):
                    bk, b_bk = bank()
                    for k in range(8):
                        TR(bk[:, k * 64:(k + 1) * 64], st_[half * 64:(half + 1) * 64, k * 128:(k + 1) * 128],
                           ident_f[half * 64:(half + 1) * 64, half * 64:(half + 1) * 64], [b_st_, b_const], [b_bk])
                    o = t * 128 + half * 64
                    CP("act", xTg3[:, :, o:o + 64], bk[:, 0:512].rearrange("p (k n) -> p k n", k=8), [b_bk], [b_xTg])

        def mixer(l, src, src_bufs_fn, dst, dst_bufs_fn):
            AR.reset()
            win, b_win = AR.bf16(8 * 3600)
            win3 = win.rearrange("p (k n) -> p k n", k=8)
            wout, b_wout = AR.bf16(8 * 1024)
            wout3 = wout.rearrange("p (k n) -> p k n", k=8)
            for k in range(8):
                DMA("pool", win3[:, k, :], w_in[l, k * 128:(k + 1) * 128, :], [], [b_win])
            DMA("pool", wout3, w_out[l].rearrange("(k p) n -> p k n", p=128), [], [b_wout])
            G, b_gb = AR.f32(1024)
            Bt, _ = AR.f32(1024)
            load_bcast(G, lng[0][l:l + 1, :], 64, [b_gb])
            load_bcast(Bt, lnb[0][l:l + 1, :], 64, [b_gb])
            mlnw, b_small = AR.f32(512)
            load_bcast(mlnw, ml_nw[l:l + 1, :], 64, [b_small])
            bif, _ = AR.f32(8)
            load_bcast(bif[:, 0:4], b_i[l:l + 1, :], 64, [b_small])
            load_bcast(bif[:, 4:8], b_f[l:l + 1, :], 64, [b_small])
            negA, _ = AR.f32(4)
            dtb, _ = AR.f32(4)
            load_bcast(negA, a_log[l:l + 1, :], 64, [b_small])
            load_bcast(dtb, dt_b[l:l + 1, :], 64, [b_small])
            ACT(negA[0:64, :], negA[0:64, :], AF.Exp, [b_small], [b_small])
            TS("dve", negA[0:64, :], negA[0:64, :], -1.0, ALU.mult, [b_small], [b_small])
            gdnw, _ = AR.f32(1)
            DMA("sp", gdnw[:, 0:1], gd_nw[l].rearrange("(p o) -> p o", o=1), [], [b_small])
            cwr, b_cwr = AR.f32(128)
            cw, _ = AR.f32(48)
            DMA("sp", cwr[0:48, :], conv_w[l].rearrange("j (c p) -> (j c) p", p=128), [], [b_cwr])
            bk, b_bk = bank()
            TR(bk[:, 0:48], cwr[0:48, :], ident_f[0:48, 0:48], [b_cwr, b_const], [b_bk])
            CP("dve", cw[:, :], bk[:, 0:48], [b_bk], [b_small])
            Cn, b_Cn = AR.f32(4 * 129)
            Cn3 = Cn[0:64, :].rearrange("p (h n) -> p h n", h=4)
            Cnb, b_Cnb = AR.bf16(4 * 130)
            Cnb3 = Cnb[0:64, 0:4 * 130].rearrange("p (h n) -> p h n", h=4)
            Sst, b_S = AR.f32(512)
            Sst3 = Sst.rearrange("p (h n) -> p h n", h=4)
            Sb, b_Sb = AR.bf16(512)
            Sb3 = Sb.rearrange("p (h n) -> p h n", h=4)
            carry, b_carry = AR.f32(36)
            carry3 = carry.rearrange("p (c n) -> p c n", c=12)
            P.op("pool", lambda e: e.memset(Cn[:, :], 0.0), [], [b_Cn])
            P.op("pool", lambda e: e.memset(Cnb[:, :], 0.0), [], [b_Cnb])
            P.op("pool", lambda e: e.memset(Sst[:, :], 0.0), [], [b_S])
            P.op("pool", lambda e: e.memset(Sb[:, :], 0.0), [], [b_Sb])
            P.op("pool", lambda e: e.memset(carry[:, :], 0.0), [], [b_carry])
            mlqT, b_mlqT = AR.bf16(4 * GT)
            mlqT3 = mlqT[0:64, :].rearrange("p (h n) -> p h n", h=4)
            mlkT, b_mlkT = AR.bf16(4 * GT)
            mlkT3 = mlkT[0:64, :].rearrange("p (h n) -> p h n", h=4)
            siluz, b_siluz = AR.f32(4 * GT)
            siluz3 = siluz.rearrange("p (h n) -> p h n", h=4)
            gdq, b_gdq = AR.bf16(4 * GT)
            gdq3 = gdq.rearrange("p (h n) -> p h n", h=4)
            gdk, b_gdk = AR.bf16(4 * GT)
            gdk3 = gdk.rearrange("p (h n) -> p h n", h=4)
            gdkf, b_gdkf = AR.f32(4 * GT)
            gdkf3 = gdkf.rearrange("p (h n) -> p h n", h=4)
            gdvf, b_gdvf = AR.f32(4 * GT)
            gdvf3 = gdvf.rearrange("p (h n) -> p h n", h=4)
            stage, b_stage = AR.f32(GT + 3)
            acc, b_acc = AR.f32(GT)
            post, b_post = AR.f32(GT)
            sq, b_sq = AR.f32(GT)
            rn, b_rn = AR.f32(GT)
            gates, b_gates = AR.f32(NCG * 16)
            gates3 = gates[0:64, :].rearrange("p (c n) -> p c n", c=NCG)
            gt, b_gt = AR.f32(NCG * 16)
            gt3 = gt[0:64, :].rearrange("p (c n) -> p c n", c=NCG)
            LG, b_LG = AR.f32(NCG * 8)
            LG3 = LG[0:64, :].rearrange("p (c n) -> p c n", c=NCG)
            ipre, b_ipre = AR.f32(NCG * 4)
            ipre3 = ipre[0:64, :].rearrange("p (c n) -> p c n", c=NCG)
            beta, b_beta = AR.f32(NCG * 4)
            beta3 = beta[0:64, :].rearrange("p (c n) -> p c n", c=NCG)
            CS, b_CS = AR.f32(NCG * 8)
            CS3 = CS[0:64, :].rearrange("p (c n) -> p c n", c=NCG)
            LA, b_LA = AR.f32(NCG * 8)
            LA3 = LA[0:64, :].rearrange("p (c n) -> p c n", c=NCG)
            EG128, b_EG = AR.f32(NCG * 4)
            EG3 = EG128.rearrange("p (c n) -> p c n", c=NCG)
            der, b_der = AR.f32(NCG * 24)
            der3 = der[0:64, :].rearrange("p (c n) -> p c n", c=NCG)
            mlk_tm, b_mlk_tm = AR.f32(256)
            mlv1, b_mlv1 = AR.bf16(4 * 130)
            mlv13 = mlv1[0:64, 0:4 * 130].rearrange("p (h n) -> p h n", h=4)
            P.op("pool", lambda e: e.memset(mlv1[:, :], 1.0), [], [b_mlv1])
            sigo, b_sigo = AR.f32(512)
            LcolF, b_Lcol = AR.f32(4 * 64)
            LcolF3 = LcolF[0:64, :].rearrange("p (h n) -> p h n", h=4)
            LcolG, _ = AR.f32(4 * 128)
            LcolG3 = LcolG[0:64, :].rearrange("p (h n) -> p h n", h=4)
            t44, b_t44 = AR.f32(256)
            t443 = t44[0:64, :].rearrange("p (h n) -> p h n", h=4)
            DTm, b_DT = AR.f32(256)
            DTm3 = DTm[0:64, :].rearrange("p (h n) -> p h n", h=4)
            Ef, b_Ef = AR.f32(256)
            Ef3 = Ef[0:64, :].rearrange("p (h n) -> p h n", h=4)
            DecS, b_DecS = AR.f32(256)
            DecS3 = DecS[0:64, :].rearrange("p (h n) -> p h n", h=4)
            DecT, b_DecT = AR.f32(256)
            DecT3 = DecT[0:64, :].rearrange("p (h n) -> p h n", h=4)
            E128, b_E128 = AR.f32(256)
            E1283 = E128.rearrange("p (h n) -> p h n", h=4)
            qtil, b_qtil = AR.bf16(256)
            qtil3 = qtil[0:64, :].rearrange("p (h n) -> p h n", h=4)
            gqt, b_gqt = AR.bf16(256)
            gqt3 = gqt.rearrange("p (h n) -> p h n", h=4)
            sint, b_sint = AR.bf16(256)
            sint3 = sint[0:64, :].rearrange("p (h n) -> p h n", h=4)
            rden, b_rden = AR.f32(8)
            hml, b_hml = AR.f32(512)
            hml3 = hml[0:64, :].rearrange("p (h n) -> p h n", h=4)
            hsq, b_hsq = AR.f32(512)
            hsq3 = hsq[0:64, :].rearrange("p (h n) -> p h n", h=4)
            ssum, b_ssum = AR.f32(8)
            kw, b_kw = AR.bf16(256)
            kw3 = kw[0:64, :].rearrange("p (h n) -> p h n", h=4)
            mixT, b_mixT = AR.bf16(8 * 64)
            mixT3 = mixT.rearrange("p (f n) -> p f n", f=8)
            bv, b_bv = AR.bf16(512)
            bv3 = bv[0:64, :].rearrange("p (h n) -> p h n", h=4)
            kbg, b_kbg = AR.bf16(512)
            kbg3 = kbg[0:64, :].rearrange("p (h n) -> p h n", h=4)
            kend, b_kend = AR.bf16(512)
            kend3 = kend[0:64, :].rearrange("p (h n) -> p h n", h=4)
            P0f, b_P0f = AR.f32(256)
            P0f3 = P0f[0:64, :].rearrange("p (h n) -> p h n", h=4)
            Pm = [AR.bf16(256) for _ in range(2)]
            PT = [AR.bf16(256) for _ in range(2)]
            Yb = [AR.bf16(256) for _ in range(2)]
            nwT, b_nwT = AR.bf16(256)
            nwT3 = nwT.rearrange("p (h n) -> p h n", h=4)
            vnew, b_vnew = AR.bf16(512)
            vnew3 = vnew[0:64, :].rearrange("p (h n) -> p h n", h=4)
            qkd, b_qkd = AR.bf16(256)
            qkd3 = qkd[0:64, :].rearrange("p (h n) -> p h n", h=4)
            osq, b_osq = AR.f32(256)
            orstd, b_orstd = AR.f32(256)
            ot1, b_ot1 = AR.f32(256)
            lnw = []
            for _ in range(2):
                a1, bb1 = AR.f32(1024)
                a2, bb2 = AR.f32(12)
                a3, bb3 = AR.f32(2)
                a4, bb4 = AR.f32(2)
                lnw.append((a1, bb1, a2[:, :].rearrange("p (a b) -> p a b", a=2), bb2, a3, bb3, a4, bb4))

            xTg, b_xTg = AR.bf16(8 * GT)
            xT3 = xTg.rearrange("p (k n) -> p k n", k=8)
            xstg = AR.f32(1024)
            for g in range(max_groups or (S // GT)):
                tok0 = 0
                xbufs = [b_xTg]
                load_xT(src, [bb for c in range(NCG) for bb in src_bufs_fn(g * NCG + c)], g * GT, GT, xT3, b_xTg, xstg)
                for qk in range(2):
                    for h in range(4):
                        col0 = (C_MLQ if qk == 0 else C_MLK) + h * 64
                        bk, b_bk = bank()
                        for k in range(8):
                            MM(bk[0:64, 0:GT], win3[:, k, col0:col0 + 64], xT3[:, k, 0:GT],
                               [b_win] + xbufs, [b_bk], start=(k == 0), stop=(k == 7))
                        if qk == 0:
                            ACT(mlqT3[:, h, :], bk[0:64, 0:GT], AF.Copy, [b_bk], [b_mlqT], scale=0.125)
                        else:
                            CP("dve", mlkT3[:, h, :], bk[0:64, 0:GT], [b_bk], [b_mlkT])
                for h in range(4):
                    bk, b_bk = bank()
                    for k in range(8):
                        MM(bk[:, 0:GT], win3[:, k, C_GDZ + h * 128:C_GDZ + (h + 1) * 128], xT3[:, k, 0:GT],
                           [b_win] + xbufs, [b_bk], start=(k == 0), stop=(k == 7))
                    ACT(siluz3[:, h, :], bk[:, 0:GT], AF.Silu, [b_bk], [b_siluz])
                for c in range(12):
                    bk, b_bk = bank()
                    for k in range(8):
                        MM(bk[:, 0:GT], win3[:, k, C_GDQ + c * 128:C_GDQ + (c + 1) * 128], xT3[:, k, 0:GT],
                           [b_win] + xbufs, [b_bk], start=(k == 0), stop=(k == 7))
                    CP("pool", stage[:, 0:3], carry3[:, c, :], [b_carry], [b_stage])
                    CP("act", stage[:, 3:GT + 3], bk[:, 0:GT], [b_bk], [b_stage])
                    CP("pool", carry3[:, c, :], stage[:, GT:GT + 3], [b_stage], [b_carry])
                    TS("dve", acc[:, :], stage[:, 0:GT], cw[:, c:c + 1], ALU.mult, [b_stage, b_small], [b_acc])
                    for j in range(1, 4):
                        STT(acc[:, :], stage[:, j:j + GT], cw[:, j * 12 + c:j * 12 + c + 1], acc[:, :], ALU.mult, ALU.add,
                            [b_stage, b_small, b_acc], [b_acc])
                    if c >= 8:
                        ACT(gdvf3[:, c - 8, :], acc[:, :], AF.Silu, [b_acc], [b_gdvf])
                    else:
                        ACT(post[:, :], acc[:, :], AF.Silu, [b_acc], [b_post])
                        TT("pool", sq[:, :], post[:, :], post[:, :], ALU.mult, [b_post], [b_sq])
                        bk2, b_bk2 = bank()
                        MM(bk2[:, 0:GT], ones_f[:, :], sq[:, :], [b_const, b_sq], [b_bk2])
                        rsqrt_(rn[:, :], bk2[:, 0:GT], 1.0, 1e-6, [b_bk2], [b_rn], (sq[:, :], b_sq))
                        if c < 4:
                            STT(gdq3[:, c, :], post[:, :], 128.0 ** -0.5, rn[:, :], ALU.mult, ALU.mult, [b_post, b_rn], [b_gdq])
                        else:
                            TT("dve", gdkf3[:, c - 4, :], post[:, :], rn[:, :], ALU.mult, [b_post, b_rn], [b_gdkf])
                            CP("pool", gdk3[:, c - 4, :], gdkf3[:, c - 4, :], [b_gdkf], [b_gdk])
                bkg, b_bkg = bank()
                for c in range(NCG):
                    t0 = c * 64
                    for part, col in ((0, C_MLI), (1, C_GDB)):
                        for k in range(8):
                            MM(bkg[0:64, c * 16 + part * 8:c * 16 + part * 8 + 8], xT3[:, k, t0:t0 + 64], win3[:, k, col:col + 8],
                               [b_win, b_xTg], [b_bkg], start=(k == 0), stop=(k == 7))
                CP("dve", gates[0:64, :], bkg[0:64, 0:NCG * 16], [b_bkg], [b_gates])
                TT("dve", gt3[:, :, 0:8], gates3[:, :, 0:8], bc3(bif[0:64, 0:8], [64, NCG, 8], 1), ALU.add, [b_gates, b_small], [b_gt])
                ACT(gt3[:, :, 0:8], gt3[:, :, 0:8], AF.Tanh, [b_gt], [b_gt], scale=1.0 / 15.0)
                TS("dve", ipre3[:, :, :], gt3[:, :, 0:4], 15.0, ALU.mult, [b_gt], [b_ipre])
                ACT(gt3[:, :, 4:8], gt3[:, :, 4:8], AF.Exp, [b_gt], [b_gt], scale=-15.0)
                ACT(gt3[:, :, 4:8], gt3[:, :, 4:8], AF.Ln, [b_gt], [b_gt], bias=1.0)
                TS("dve", LG3[:, :, 0:4], gt3[:, :, 4:8], -1.0, ALU.mult, [b_gt], [b_LG])
                ACT(beta3[:, :, :], gates3[:, :, 8:12], AF.Sigmoid, [b_gates], [b_beta])
                TT("dve", gt3[:, :, 8:12], gates3[:, :, 12:16], bc3(dtb[0:64, 0:4], [64, NCG, 4], 1), ALU.add, [b_gates, b_small], [b_gt])
                ACT(gt3[:, :, 12:16], gt3[:, :, 8:12], AF.Abs, [b_gt], [b_gt])
                ACT(gt3[:, :, 12:16], gt3[:, :, 12:16], AF.Exp, [b_gt], [b_gt], scale=-1.0)
                ACT(gt3[:, :, 12:16], gt3[:, :, 12:16], AF.Ln, [b_gt], [b_gt], bias=1.0)
                TS("dve", gt3[:, :, 8:12], gt3[:, :, 8:12], 0.0, ALU.max, [b_gt], [b_gt])
                TT("dve", gt3[:, :, 8:12], gt3[:, :, 8:12], gt3[:, :, 12:16], ALU.add, [b_gt], [b_gt])
                TT("dve", LG3[:, :, 4:8], gt3[:, :, 8:12], bc3(negA[0:64, 0:4], [64, NCG, 4], 1), ALU.mult, [b_gt, b_small], [b_LG])
                bkc, b_bkc = bank()
                MM(bkc[0:64, 0:NCG * 8], t1_f[:, :], LG[0:64, 0:NCG * 8], [b_const, b_LG], [b_bkc])
                MM(bkc[0:64, 64:64 + NCG * 8], ones_f[0:64, 0:64], LG[0:64, 0:NCG * 8], [b_const, b_LG], [b_bkc])
                MM(bkc[:, 128:128 + NCG * 8], ones_f[0:64, :], LG[0:64, 0:NCG * 8], [b_const, b_LG], [b_bkc])
                CP("dve", CS[0:64, :], bkc[0:64, 0:NCG * 8], [b_bkc], [b_CS])
                CP("dve", LA[0:64, :], bkc[0:64, 64:64 + NCG * 8], [b_bkc], [b_LA])
                ACT(EG3[:, :, :], bkc[:, 128:128 + NCG * 8].rearrange("p (c n) -> p c n", c=NCG)[:, :, 4:8], AF.Exp, [b_bkc], [b_EG])
                TT("dve", der3[:, :, 0:4], ipre3[:, :, :], CS3[:, :, 0:4], ALU.subtract, [b_ipre, b_CS], [b_der])
                TT("dve", der3[:, :, 4:8], der3[:, :, 0:4], LA3[:, :, 0:4], ALU.add, [b_der, b_LA], [b_der])
                ACT(der3[:, :, 4:8], der3[:, :, 4:8], AF.Exp, [b_der], [b_der])
                ACT(der3[:, :, 8:12], LA3[:, :, 0:4], AF.Exp, [b_LA], [b_der])
                ACT(der3[:, :, 12:16], CS3[:, :, 4:8], AF.Exp, [b_CS], [b_der])
                TT("dve", der3[:, :, 12:16], der3[:, :, 12:16], beta3[:, :, :], ALU.mult, [b_der, b_beta], [b_der])
                TT("dve", der3[:, :, 16:20], LA3[:, :, 4:8], CS3[:, :, 4:8], ALU.subtract, [b_LA, b_CS], [b_der])
                ACT(der3[:, :, 16:20], der3[:, :, 16:20], AF.Exp, [b_der], [b_der])
                TS("dve", der3[:, :, 20:24], beta3[:, :, :], -1.0, ALU.mult, [b_beta], [b_der])

                for c in range(NCG):
                    t0 = c * 64
                    lo = c * 64
                    ci = g * NCG + c
                    xb_ = [b_xTg]
                    bk, b_bk = bank()
                    for k in range(8):
                        MM(bk[0:64, 0:256], xT3[:, k, t0:t0 + 64], win3[:, k, C_MLK:C_MLK + 256], [b_win] + xb_, [b_bk],
                           start=(k == 0), stop=(k == 7))
                    CP("act", mlk_tm[0:64, :], bk[0:64, 0:256], [b_bk], [b_mlk_tm])
                    bk, b_bk = bank()
                    for k in range(8):
                        MM(bk[0:64, 0:512], xT3[:, k, t0:t0 + 64], win3[:, k, C_MLV:C_MLV + 512], [b_win] + xb_, [b_bk],
                           start=(k == 0), stop=(k == 7))
                    CP("dve", mlv13[:, :, 0:128], bk[0:64, 0:512].rearrange("p (h n) -> p h n", h=4), [b_bk], [b_mlv1])
                    bk, b_bk = bank()
                    for k in range(8):
                        MM(bk[0:64, 0:512], xT3[:, k, t0:t0 + 64], win3[:, k, C_MLO:C_MLO + 512], [b_win] + xb_, [b_bk],
                           start=(k == 0), stop=(k == 7))
                    ACT(sigo[0:64, :], bk[0:64, 0:512], AF.Sigmoid, [b_bk], [b_sigo])
                    CP("pool", LcolF3[:, :, :], bc3(LG3[:, c, 0:4], [64, 4, 64], 2), [b_LG], [b_Lcol])
                    CP("pool", LcolG3[:, :, :], bc3(LG3[:, c, 4:8], [64, 4, 128], 2), [b_LG], [b_Lcol])
                    bkM, b_bkM = bank()
                    for h in range(4):
                        MM(bkM[0:64, h * 64:(h + 1) * 64], LcolF3[:, h, :], t1_f[:, :], [b_Lcol, b_const], [b_bkM])
                    for h in range(4):
                        MM(bkM[0:64, 256 + h * 64:256 + (h + 1) * 64], LcolG3[:, h, 0:64], t1_f[:, :], [b_Lcol, b_const], [b_bkM])
                    bkE, b_bkE = bank()
                    for h in range(4):
                        MM(bkE[:, h * 64:(h + 1) * 64], LcolG3[:, h, :], t1_f[:, :], [b_Lcol, b_const], [b_bkE])
                    Mf = bkM[0:64, 0:256].rearrange("p (h n) -> p h n", h=4)
                    Mg = bkM[0:64, 256:512].rearrange("p (h n) -> p h n", h=4)
                    TT("dve", t443, Mf, bc3(der3[:, c, 0:4], [64, 4, 64], 2), ALU.add, [b_bkM, b_der], [b_t44])
                    TT("dve", t443, t443, bc3(negc[:, :], [64, 4, 64], 1), ALU.add, [b_t44, b_const], [b_t44])
                    ACT(DTm3, t443, AF.Exp, [b_t44], [b_DT])
                    ACT(Ef3, Mf, AF.Exp, [b_bkM], [b_Ef])
                    TT("dve", qtil3, mlqT3[:, :, lo:lo + 64], Ef3, ALU.mult, [b_mlqT, b_Ef], [b_qtil])
                    STT(t443, Mg, -1.0, bc3(CS3[:, c, 4:8], [64, 4, 64], 2), ALU.mult, ALU.add, [b_bkM, b_CS, b_t44], [b_t44])
                    TT("dve", t443, t443, bc3(negs[:, :], [64, 4, 64], 1), ALU.add, [b_t44, b_const], [b_t44])
                    ACT(DecS3, t443, AF.Exp, [b_t44], [b_DecS])
                    TT("dve", t443, Mg, bc3(CS3[:, c, 4:8], [64, 4, 64], 2), ALU.subtract, [b_bkM, b_CS, b_t44], [b_t44])
                    TT("dve", t443, t443, bc3(negc[:, :], [64, 4, 64], 1), ALU.add, [b_t44, b_const], [b_t44])
                    ACT(DecT3, t443, AF.Exp, [b_t44], [b_DecT])
                    ACT(E128[:, :], bkE[:, 0:256], AF.Exp, [b_bkE], [b_E128])
                    TT("dve", gqt3, gdq3[:, :, lo:lo + 64], E1283, ALU.mult, [b_gdq, b_E128], [b_gqt])

                    bk, b_bk = bank()
                    for h in range(4):
                        MM(bk[0:64, h * 64:(h + 1) * 64], mlkT3[:, h, lo:lo + 64], mlqT3[:, h, lo:lo + 64], [b_mlkT, b_mlqT], [b_bk])
                    TT("dve", sint3, bk[0:64, 0:256].rearrange("p (h n) -> p h n", h=4), DTm3, ALU.mult, [b_bk, b_DT], [b_sint])
                    for hp in range(2):
                        bk, b_bk = bank()
                        for j in range(2):
                            h = hp * 2 + j
                            MM(bk[0:64, j * 129:(j + 1) * 129], qtil3[:, h, :], Cnb3[:, h, 0:129], [b_qtil, b_Cnb], [b_bk], start=True, stop=False)
                            MM(bk[0:64, j * 129:(j + 1) * 129], sint3[:, h, :], mlv13[:, h, 0:129], [b_sint, b_mlv1], [b_bk], start=False, stop=True)
                        bv_ = bk[0:64, 0:258].rearrange("p (j n) -> p j n", j=2)
                        ACT(rden[0:64, hp * 2:hp * 2 + 2], bv_[:, :, 128], AF.Abs, [b_bk], [b_rden])
                        TS("dve", rden[0:64, hp * 2:hp * 2 + 2], rden[0:64, hp * 2:hp * 2 + 2], 1.0, ALU.max, [b_rden], [b_rden])
                        P.op("dve", lambda e, hp=hp: e.reciprocal(rden[0:64, 4 + hp * 2:4 + hp * 2 + 2], rden[0:64, hp * 2:hp * 2 + 2]), [b_rden], [b_rden])
                        TT("dve", hml3[:, hp * 2:hp * 2 + 2, :], bv_[:, :, 0:128], bc3(rden[0:64, 4 + hp * 2:4 + hp * 2 + 2], [64, 2, 128], 2),
                           ALU.mult, [b_bk, b_rden], [b_hml])
                    TT("pool", hsq3, hml3, hml3, ALU.mult, [b_hml], [b_hsq])
                    P.op("dve", lambda e: e.tensor_reduce(ssum[0:64, 0:4], hsq3, AX.X, ALU.add), [b_hsq], [b_ssum])
                    rsqrt_(ssum[0:64, 4:8], ssum[0:64, 0:4], 1.0 / 128.0, 1e-6, [b_ssum], [b_ssum], (ssum[0:64, 0:4], b_ssum))
                    TT("dve", hml3, hml3, bc3(ssum[0:64, 4:8], [64, 4, 128], 2), ALU.mult, [b_hml, b_ssum], [b_hml])
                    TT("pool", hml[0:64, :], hml[0:64, :], mlnw[0:64, :], ALU.mult, [b_hml, b_small], [b_hml])
                    TT("pool", hml[0:64, :], hml[0:64, :], sigo[0:64, :], ALU.mult, [b_hml, b_sigo], [b_hml])
                    bk, b_bk = bank()
                    for h in range(4):
                        TR(bk[:, h * 64:(h + 1) * 64], hml[0:64, h * 128:(h + 1) * 128], ident_f[0:64, 0:64], [b_hml, b_const], [b_bk])
                    CP("act", mixT3[:, 0:4, :], bk[:, 0:256].rearrange("p (h n) -> p h n", h=4), [b_bk], [b_mixT])
                    TT("dve", kw3, mlk_tm[0:64, :].rearrange("p (h n) -> p h n", h=4), bc3(der3[:, c, 4:8], [64, 4, 64], 2), ALU.mult,
                       [b_mlk_tm, b_der], [b_kw])
                    TT("dve", Cn3, Cn3, bc3(der3[:, c, 8:12], [64, 4, 129], 2), ALU.mult, [b_Cn, b_der], [b_Cn])
                    for hp in range(2):
                        bk, b_bk = bank()
                        for j in range(2):
                            h = hp * 2 + j
                            MM(bk[0:64, j * 129:(j + 1) * 129], kw3[:, h, :], mlv13[:, h, 0:129], [b_kw, b_mlv1], [b_bk])
                        TT("dve", Cn3[:, hp * 2:hp * 2 + 2, :], Cn3[:, hp * 2:hp * 2 + 2, :], bk[0:64, 0:258].rearrange("p (j n) -> p j n", j=2),
                           ALU.add, [b_Cn, b_bk], [b_Cn])
                    CP("pool", Cnb3[:, :, 0:129], Cn3, [b_Cn], [b_Cnb])

                    bkk, b_bkk = bank()
                    bkv, b_bkv = bank()
                    for h in range(4):
                        TR(bkk[0:64, h * 128:(h + 1) * 128], gdkf3[:, h, lo:lo + 64], ident_f[:, :], [b_gdkf, b_const], [b_bkk])
                    for h in range(4):
                        TR(bkv[0:64, h * 128:(h + 1) * 128], gdvf3[:, h, lo:lo + 64], ident_f[:, :], [b_gdvf, b_const], [b_bkv])
                    ktm = bkk[0:64, 0:512].rearrange("p (h n) -> p h n", h=4)
                    vtm = bkv[0:64, 0:512].rearrange("p (h n) -> p h n", h=4)
                    TT("dve", bv3, vtm, bc3(beta3[:, c, :], [64, 4, 128], 2), ALU.mult, [b_bkv, b_beta], [b_bv])
                    TT("dve", kbg3, ktm, bc3(der3[:, c, 12:16], [64, 4, 128], 2), ALU.mult, [b_bkk, b_der], [b_kbg])
                    TT("dve", kend3, ktm, bc3(der3[:, c, 16:20], [64, 4, 128], 2), ALU.mult, [b_bkk, b_der], [b_kend])
                    bk, b_bk = bank()
                    for h in range(4):
                        MM(bk[0:64, h * 64:(h + 1) * 64], gdk3[:, h, lo:lo + 64], gdk3[:, h, lo:lo + 64], [b_gdk], [b_bk])
                    TT("dve", t443, bk[0:64, 0:256].rearrange("p (h n) -> p h n", h=4), bc3(der3[:, c, 20:24], [64, 4, 64], 2), ALU.mult,
                       [b_bk, b_der, b_t44], [b_t44])
                    TT("dve", P0f3, t443, DecS3, ALU.mult, [b_t44, b_DecS], [b_P0f])
                    pm, b_pm = Pm[0]
                    pt, b_pt = PT[0]
                    yb, b_yb = Yb[0]
                    pm3 = pm[0:64, :].rearrange("p (h n) -> p h n", h=4)
                    CP("pool", pm[0:64, :], P0f[0:64, :], [b_P0f], [b_pm])
                    bk, b_bk = bank()
                    for h in range(4):
                        TR(bk[0:64, h * 64:(h + 1) * 64], P0f3[:, h, :], ident_f[0:64, 0:64], [b_P0f, b_const], [b_bk])
                    CP("act", pt[0:64, :], bk[0:64, 0:256], [b_bk], [b_pt])
                    TT("dve", yb[0:64, :].rearrange("p (h n) -> p h n", h=4), bk[0:64, 0:256].rearrange("p (h n) -> p h n", h=4),
                       bc3(ident_f[0:64, 0:64], [64, 4, 64], 1), ALU.add, [b_bk, b_const], [b_yb])
                    cur = 0
                    for lev in range(1, 6):
                        pm, b_pm = Pm[cur]
                        pt, b_pt = PT[cur]
                        yb, b_yb = Yb[cur]
                        pmn, b_pmn = Pm[1 - cur]
                        ptn, b_ptn = PT[1 - cur]
                        ybn, b_ybn = Yb[1 - cur]
                        v3 = lambda a: a[0:64, :].rearrange("p (h n) -> p h n", h=4)
                        bkA, b_bkA = bank()
                        for h in range(4):
                            MM(bkA[0:64, h * 64:(h + 1) * 64], v3(pt)[:, h, :], v3(pm)[:, h, :], [b_pt, b_pm], [b_bkA])
                        CP("act", pmn[0:64, :], bkA[0:64, 0:256], [b_bkA], [b_pmn])
                        if lev < 5:
                            bkB, b_bkB = bank()
                            for h in range(4):
                                MM(bkB[0:64, h * 64:(h + 1) * 64], v3(pm)[:, h, :], v3(pt)[:, h, :], [b_pt, b_pm], [b_bkB])
                            CP("dve", ptn[0:64, :], bkB[0:64, 0:256], [b_bkB], [b_ptn])
                        bkC, b_bkC = bank()
                        for h in range(4):
                            MM(bkC[0:64, h * 64:(h + 1) * 64], v3(pmn)[:, h, :], v3(yb)[:, h, :], [b_pmn, b_yb], [b_bkC])
                        TT("dve", ybn[0:64, :], yb[0:64, :], bkC[0:64, 0:256], ALU.add, [b_yb, b_bkC], [b_ybn])
                        cur = 1 - cur
                    RT, b_RT = Yb[cur]
                    RT3 = RT[0:64, :].rearrange("p (h n) -> p h n", h=4)
                    bk, b_bk = bank()
                    for h in range(4):
                        MM(bk[:, h * 64:(h + 1) * 64], kbg3[:, h, :], RT3[:, h, :], [b_kbg, b_RT], [b_bk])
                    ACT(nwT[:, :], bk[:, 0:256], AF.Copy, [b_bk], [b_nwT], scale=-1.0)
                    bk, b_bk = bank()
                    for h in range(4):
                        MM(bk[0:64, h * 128:(h + 1) * 128], RT3[:, h, :], bv3[:, h, :], [b_RT, b_bv], [b_bk], start=True, stop=False)
                        MM(bk[0:64, h * 128:(h + 1) * 128], nwT3[:, h, :], Sb3[:, h, :], [b_nwT, b_Sb], [b_bk], start=False, stop=True)
                    CP("act", vnew[0:64, :], bk[0:64, 0:512], [b_bk], [b_vnew])
                    bk, b_bk = bank()
                    for h in range(4):
                        MM(bk[0:64, h * 64:(h + 1) * 64], gdk3[:, h, lo:lo + 64], gdq3[:, h, lo:lo + 64], [b_gdk, b_gdq], [b_bk])
                    TT("dve", qkd3, bk[0:64, 0:256].rearrange("p (h n) -> p h n", h=4), DecT3, ALU.mult, [b_bk, b_DecT], [b_qkd])
                    bko, b_bko = bank()
                    for h in range(4):
                        MM(bko[:, h * 64:(h + 1) * 64], Sb3[:, h, :], gqt3[:, h, :], [b_Sb, b_gqt], [b_bko], start=True, stop=False)
                        MM(bko[:, h * 64:(h + 1) * 64], vnew3[:, h, :], qkd3[:, h, :], [b_vnew, b_qkd], [b_bko], start=False, stop=True)
                    bk, b_bk = bank()
                    for h in range(4):
                        MM(bk[:, h * 128:(h + 1) * 128], kend3[:, h, :], vnew3[:, h, :], [b_kend, b_vnew], [b_bk])
                    TT("dve", Sst3, Sst3, bc3(EG3[:, c, :], [128, 4, 128], 2), ALU.mult, [b_S, b_EG], [b_S])
                    TT("dve", Sst[:, :], Sst[:, :], bk[:, 0:512], ALU.add, [b_S, b_bk], [b_S])
                    CP("pool", Sb[:, :], Sst[:, :], [b_S], [b_Sb])
                    ACT(osq[:, :], bko[:, 0:256], AF.Square, [b_bko], [b_osq])
                    bk, b_bk = bank()
                    MM(bk[:, 0:256], ones_f[:, :], osq[:, :], [b_const, b_osq], [b_bk])
                    rsqrt_(orstd[:, :], bk[:, 0:256], 1.0 / 128.0, 1e-6, [b_bk], [b_orstd], (osq[:, :], b_osq))
                    TT("dve", ot1[:, :], bko[:, 0:256], orstd[:, :], ALU.mult, [b_bko, b_orstd], [b_ot1])
                    STT(mixT3[:, 4:8, :], ot1[:, :].rearrange("p (h n) -> p h n", h=4), gdnw[:, 0:1], siluz3[:, :, lo:lo + 64],
                        ALU.mult, ALU.mult, [b_ot1, b_small, b_siluz], [b_mixT])
                    hp_ = []
                    hb_ = []
                    for n in range(2):
                        bk, b_bk = bank()
                        for f in range(8):
                            MM(bk[0:64, 0:512], mixT3[:, f, :], wout3[:, f, n * 512:(n + 1) * 512], [b_mixT, b_wout], [b_bk],
                               start=(f == 0), stop=(f == 7))
                        hp_.append(bk[0:64, 0:512])
                        hb_.append(b_bk)
                    ln_epilogue(64, g * GT + c * 64, hp_, hb_, src, src_bufs_fn(ci), G, Bt, b_gb, dst, dst_bufs_fn(ci), lnw[ci % 2], False)
            P.barrier()


        def xattn(l):
            AR.reset()
            W = []
            for i in range(4):
                w, bw = AR.bf16(8 * 1024)
                w3 = w.rearrange("p (k n) -> p k n", k=8)
                DMA("pool", w3, xa_w[i][l].rearrange("(k p) n -> p k n", p=128), [], [bw])
                W.append((w3, bw))
            (wq3, b_wq), (wk3, b_wk), (wv3, b_wv), (wo3, b_wo) = W
            G, b_gb = AR.f32(1024)
            Bt, _ = AR.f32(1024)
            load_bcast(G, lng[1][l:l + 1, :], 128, [b_gb])
            load_bcast(Bt, lnb[1][l:l + 1, :], 128, [b_gb])
            memT, b_memT = AR.bf16(8 * 256)
            memT3 = memT.rearrange("p (k n) -> p k n", k=8)
            xstg = AR.f32(1024)
            load_xT(mem_in, [], 0, 256, memT3, b_memT, xstg)
            kT, b_kT = AR.bf16(8 * 256)
            kT3 = kT.rearrange("p (k n) -> p k n", k=8)
            for f in range(8):
                bk, b_bk = bank()
                for k in range(8):
                    MM(bk[:, 0:256], wk3[:, k, f * 128:(f + 1) * 128], memT3[:, k, :], [b_wk, b_memT], [b_bk], start=(k == 0), stop=(k == 7))
                CP("dve", kT3[:, f, :], bk[:, 0:256], [b_bk], [b_kT])
            vv, b_vv = AR.bf16(2 * 1024)
            vv3 = vv.rearrange("p (m n) -> p m n", m=2)
            for mc in range(2):
                for n in range(2):
                    bk, b_bk = bank()
                    for k in range(8):
                        MM(bk[:, 0:512], memT3[:, k, mc * 128:(mc + 1) * 128], wv3[:, k, n * 512:(n + 1) * 512], [b_wv, b_memT], [b_bk],
                           start=(k == 0), stop=(k == 7))
                    CP("act", vv3[:, mc, n * 512:(n + 1) * 512], bk[:, 0:512], [b_bk], [b_vv])
            XG = 512
            xTg, b_xTg = AR.bf16(8 * XG)
            xT3 = xTg.rearrange("p (k n) -> p k n", k=8)
            qT, b_qT = AR.bf16(8 * XG)
            qT3 = qT.rearrange("p (k n) -> p k n", k=8)
            expT = [AR.bf16(XG) for _ in range(2)]
            rdn, b_rdn = AR.f32(XG)
            oT, b_oT = AR.bf16(8 * XG)
            oT3 = oT.rearrange("p (k n) -> p k n", k=8)
            lnw = []
            for _ in range(2):
                a1, bb1 = AR.f32(1024)
                a2, bb2 = AR.f32(12)
                a3, bb3 = AR.f32(2)
                a4, bb4 = AR.f32(2)
                lnw.append((a1, bb1, a2[:, :].rearrange("p (a b) -> p a b", a=2), bb2, a3, bb3, a4, bb4))
            for g in range(S // XG):
                rb_ = [b_xres[g * 8 + c] for c in range(8)]
                load_xT(xres, rb_, g * XG, XG, xT3, b_xTg, xstg)
                for f in range(8):
                    bk, b_bk = bank()
                    for k in range(8):
                        MM(bk[:, 0:XG], wq3[:, k, f * 128:(f + 1) * 128], xT3[:, k, :], [b_wq, b_xTg], [b_bk], start=(k == 0), stop=(k == 7))
                    ACT(qT3[:, f, :], bk[:, 0:XG], AF.Copy, [b_bk], [b_qT], scale=1.0 / 16.0)
                for h in range(4):
                    for mc in range(2):
                        bk, b_bk = bank()
                        for j in range(2):
                            MM(bk[:, 0:XG], kT3[:, 2 * h + j, mc * 128:(mc + 1) * 128], qT3[:, 2 * h + j, :], [b_kT, b_qT], [b_bk],
                               start=(j == 0), stop=(j == 1))
                        ACT(expT[mc][0][:, :], bk[:, 0:XG], AF.Exp, [b_bk], [expT[mc][1]])
                    bd, b_bd = bank()
                    for mc in range(2):
                        MM(bd[:, 0:XG], ones_b[:, :], expT[mc][0][:, :], [b_const, expT[mc][1]], [b_bd], start=(mc == 0), stop=(mc == 1))
                    P.op("dve", lambda e, bd=bd: e.reciprocal(rdn[:, :], bd[:, 0:XG]), [b_bd], [b_rdn])
                    for j in range(2):
                        bk, b_bk = bank()
                        for mc in range(2):
                            MM(bk[:, 0:XG], vv3[:, mc, (2 * h + j) * 128:(2 * h + j + 1) * 128], expT[mc][0][:, :], [b_vv, expT[mc][1]], [b_bk],
                               start=(mc == 0), stop=(mc == 1))
                        TT("dve", oT3[:, 2 * h + j, :], bk[:, 0:XG], rdn[:, :], ALU.mult, [b_bk, b_rdn], [b_oT])
                for t in range(XG // 128):
                    hp_, hb_ = [], []
                    for n in range(2):
                        bk, b_bk = bank()
                        for f in range(8):
                            MM(bk[:, 0:512], oT3[:, f, t * 128:(t + 1) * 128], wo3[:, f, n * 512:(n + 1) * 512], [b_oT, b_wo], [b_bk],
                               start=(f == 0), stop=(f == 7))
                        hp_.append(bk[:, 0:512])
                        hb_.append(b_bk)
                    r0 = g * XG + t * 128
                    rb2 = [b_xres[r0 // 64], b_xres[r0 // 64 + 1]]
                    ln_epilogue(128, r0, hp_, hb_, xres, rb2, G, Bt, b_gb, xres, rb2, lnw[t % 2], False)
            P.barrier()

        def moe(l, dst, dst_bufs_fn):
            AR.reset()
            G, b_gb = AR.f32(1024)
            Bt, _ = AR.f32(1024)
            load_bcast(G, lng[2][l:l + 1, :], 128, [b_gb])
            load_bcast(Bt, lnb[2][l:l + 1, :], 128, [b_gb])
            dest = dest_t
            b_dest = Buf("dest")
            gate, b_gate = AR.f32(32 * 4)
            gate3 = gate.rearrange("p (t k) -> p t k", t=32)
            mark = AR.off
            rw, b_rw = AR.f32(8 * NE)
            rw3 = rw.rearrange("p (k e) -> p k e", k=8)
            DMA("sp", rw3, r_w[l].rearrange("(k p) e -> p k e", p=128), [], [b_rw])
            rb, _ = AR.f32(NE)
            load_bcast(rb, r_b[l:l + 1, :], 128, [b_rw])
            cnt, b_cnt = AR.f32(NE)
            P.op("pool", lambda e: e.memset(cnt[:, :], 0.0), [], [b_cnt])
            xs2 = [AR.f32(1024) for _ in range(2)]
            xT32, b_xT32 = AR.f32(8 * 128)
            xT323 = xT32.rearrange("p (k n) -> p k n", k=8)
            xbf = [AR.bf16(1024) for i in range(2)]
            lg, b_lg = AR.f32(NE)
            mx8, b_mx8 = AR.f32(8)
            sm, b_sm = AR.f32(8)
            ex, b_ex = AR.f32(NE)
            msk, b_msk = AR.f32(NE)
            mskb, b_mskb = AR.bf16(NE)
            pos, b_pos = AR.f32(NE)
            ov, b_ov = AR.f32(NE)
            oh, b_oh = AR.f32(NE)
            tm, b_tm = AR.f32(NE)
            dsel, b_dsel = AR.f32(4)
            dsel16, b_dsel16 = AR.f32(16)
            b_xsall = Buf("xs_all")
            b_ysall = Buf("ys_all")
            for t in range(32):
                xs_, b_xs_ = xs2[t % 2]
                xb_, b_xb_ = xbf[t % 2]
                rbf = [b_xres[2 * t], b_xres[2 * t + 1]]
                DMA("sp", xs_[:, :], xres[t * 128:(t + 1) * 128, :], rbf, [b_xs_])
                for half in range(2):
                    bk, b_bk = bank()
                    for k in range(8):
                        TR(bk[:, k * 64:(k + 1) * 64], xs_[half * 64:(half + 1) * 64, k * 128:(k + 1) * 128],
                           ident_f[half * 64:(half + 1) * 64, half * 64:(half + 1) * 64], [b_xs_, b_const], [b_bk])
                    CP("dve", xT323[:, :, half * 64:(half + 1) * 64], bk[:, 0:512].rearrange("p (k n) -> p k n", k=8), [b_bk], [b_xT32])
                CP("act", xb_[:, :], xs_[:, :], [b_xs_], [b_xb_])
                bl, b_bl = bank()
                for k in range(8):
                    MM(bl[:, 0:NE], xT323[:, k, :], rw3[:, k, :], [b_xT32, b_rw], [b_bl], start=(k == 0), stop=(k == 7))
                TT("dve", lg[:, :], bl[:, 0:NE], rb[:, :], ALU.add, [b_bl, b_rw], [b_lg])
                P.op("dve", lambda e: e.max(mx8[:, :], lg[:, :]), [b_lg], [b_mx8])
                TS("dve", sm[:, 0:1], mx8[:, 0:1], -1.0, ALU.mult, [b_mx8], [b_sm])
                ACT(ex[:, :], lg[:, :], AF.Exp, [b_lg, b_sm], [b_ex], bias=sm[:, 0:1])
                TS("dve", msk[:, :], lg[:, :], mx8[:, 3:4], ALU.is_ge, [b_lg, b_mx8], [b_msk])
                TT("dve", ex[:, :], ex[:, :], msk[:, :], ALU.mult, [b_ex, b_msk], [b_ex])
                P.op("dve", lambda e: e.tensor_reduce(sm[:, 1:2], ex[:, :], AX.X, ALU.add), [b_ex], [b_sm])
                P.op("dve", lambda e: e.reciprocal(sm[:, 2:3], sm[:, 1:2]), [b_sm], [b_sm])
                TS("dve", ex[:, :], ex[:, :], sm[:, 2:3], ALU.mult, [b_ex, b_sm], [b_ex])
                CP("pool", mskb[:, :], msk[:, :], [b_msk], [b_mskb])
                bp, b_bp = bank()
                MM(bp[:, 0:NE], tri_b[:, :], mskb[:, :], [b_const, b_mskb], [b_bp])
                MM(bp[:, NE:2 * NE], ones_b[:, :], mskb[:, :], [b_const, b_mskb], [b_bp])
                TT("dve", pos[:, :], bp[:, 0:NE], cnt[:, :], ALU.add, [b_bp, b_cnt], [b_pos])
                TT("dve", cnt[:, :], cnt[:, :], bp[:, NE:2 * NE], ALU.add, [b_bp, b_cnt], [b_cnt])
                TS("dve", ov[:, :], pos[:, :], float(CAP), ALU.is_ge, [b_pos], [b_ov])
                STT(pos[:, :], ov[:, :], 1.0e7, pos[:, :], ALU.mult, ALU.add, [b_ov, b_pos], [b_pos])
                TT("dve", pos[:, :], pos[:, :], eoff[:, :], ALU.add, [b_pos, b_const], [b_pos])
                TS("dve", ov[:, :], ov[:, :], -1.0, ALU.mult, [b_ov], [b_ov], s2=1.0, op1=ALU.add)
                TT("dve", ex[:, :], ex[:, :], ov[:, :], ALU.mult, [b_ex, b_ov], [b_ex])
                for k in range(4):
                    TS("dve", oh[:, :], lg[:, :], mx8[:, k:k + 1], ALU.is_equal, [b_lg, b_mx8], [b_oh])
                    TT("dve", tm[:, :], oh[:, :], pos[:, :], ALU.mult, [b_oh, b_pos], [b_tm])
                    P.op("dve", lambda e, k=k: e.tensor_reduce(dsel[:, k:k + 1], tm[:, :], AX.X, ALU.add), [b_tm], [b_dsel])
                    TT("dve", tm[:, :], oh[:, :], ex[:, :], ALU.mult, [b_oh, b_ex], [b_tm])
                    P.op("dve", lambda e, k=k, t=t: e.tensor_reduce(gate3[:, t, k:k + 1], tm[:, :], AX.X, ALU.add), [b_tm], [b_gate])
                CP("dve", dest[:, t * 4:t * 4 + 4], dsel[:, 0:4], [b_dsel], [b_dest])
                for k in range(4):
                    q = idx_ctr[0] % 8
                    idx_ctr[0] += 1
                    CP("dve", idx_t[q][:, :], dest[:, t * 4 + k:t * 4 + k + 1], [b_dest], [b_idx[q]])
                    P.dma("pool", lambda e, q=q, xb_=xb_: e.indirect_dma_start(
                        out=xs_d, out_offset=bass.IndirectOffsetOnAxis(ap=idx_t[q][:, :], axis=0),
                        in_=xb_[:, :], in_offset=None, bounds_check=bc_reg(e), oob_is_err=False), [b_xb_, b_idx[q]], [b_xsall])
            P.barrier()
            AR.off = mark
            wgu = [AR.bf16(8 * 2048) for _ in range(2)]
            wdn = [AR.bf16(8 * 1024) for _ in range(2)]
            bdn = [AR.f32(1024) for _ in range(1)]
            bgr, b_bgr = AR.f32(128)
            bguT, b_bguT = AR.f32(NE * 16)
            for q in range(4):
                DMA("sp", bgr[:, :], b_gu[l].rearrange("e (c p) -> (e c) p", p=128)[q * 128:(q + 1) * 128, :], [], [b_bgr])
                bk, b_bk = bank()
                TR(bk[:, 0:128], bgr[:, :], ident_f[:, :], [b_bgr, b_const], [b_bk])
                CP("dve", bguT[:, q * 128:(q + 1) * 128], bk[:, 0:128], [b_bk], [b_bguT])
            bgv = bguT[:, :].rearrange("p (e c) -> p e c", c=16)
            TS("dve", bgv[:, :, 8:16], bgv[:, :, 8:16], 1.0, ALU.add, [b_bguT], [b_bguT])
            xsel, b_xsel = AR.bf16(NCAP * 1024)
            xsel3 = xsel.rearrange("p (j d) -> p j d", j=NCAP)
            xselT, b_xselT = AR.bf16(8 * CAP)
            xselT3 = xselT.rearrange("p (k n) -> p k n", k=8)
            actT, b_actT = AR.bf16(8 * CAP)
            actT3 = actT.rearrange("p (k n) -> p k n", k=8)
            HC = CAP // 2
            gtmp, b_gtmp = AR.f32(HC)
            stmp, b_stmp = AR.f32(HC)
            ltmp, b_ltmp = AR.f32(HC)
            ysb = [AR.f32(1024) for _ in range(1)]
            NST, PD = 5, 4
            wstage = [AR.f32(1024) for _ in range(NST)]
            wctr = [0]

            def wsteps(e2):
                wg_, b_wg_ = wgu[e2 % 2]
                wg3_ = wg_.rearrange("p (k n) -> p k n", k=8)
                wd_, b_wd_ = wdn[e2 % 2]
                wd3_ = wd_.rearrange("p (k n) -> p k n", k=8)
                pieces = []
                for k in range(8):
                    for hf in range(2):
                        pieces.append((w_gu[l, e2, k * 128:(k + 1) * 128, hf * 1024:(hf + 1) * 1024], wg3_[:, k, hf * 1024:(hf + 1) * 1024], b_wg_))
                for k in range(8):
                    pieces.append((w_dn[l, e2, k * 128:(k + 1) * 128, :], wd3_[:, k, :], b_wd_))
                slots = []

                def mk_dma(i):
                    def f():
                        sl = wctr[0] % NST
                        wctr[0] += 1
                        slots.append(sl)
                        DMA("sp", wstage[sl][0][:, :], pieces[i][0], [], [wstage[sl][1]])
                    return f

                def mk_cast(i):
                    def f():
                        sl = slots[i]
                        CP("act", pieces[i][1], wstage[sl][0][:, :], [wstage[sl][1]], [pieces[i][2]])
                    return f
                n = len(pieces)
                steps = []
                for i in range(n + PD):
                    fs = []
                    if i - PD >= 0:
                        fs.append(mk_cast(i - PD))
                    if i < n:
                        fs.append(mk_dma(i))
                    steps.append(lambda fs=fs: [f() for f in fs])
                return steps

            for e_ in range(NE):
                wg, b_wg = wgu[e_ % 2]
                wg3 = wg.rearrange("p (k n) -> p k n", k=8)
                wd, b_wd = wdn[e_ % 2]
                wd3 = wd.rearrange("p (k n) -> p k n", k=8)
                bd_, b_bd_ = bdn[0]
                if e_ == 0:
                    for step in wsteps(0):
                        step()
                pending = wsteps(e_ + 1) if e_ + 1 < NE else []
                DMA("sp", bd_[:, :], b_dn[l, e_:e_ + 1, :].partition_broadcast(128), [], [b_bd_])
                DMA("sp", xsel3, xs_d[e_ * CAP:(e_ + 1) * CAP, :].rearrange("(j p) d -> p j d", p=128), [b_xsall], [b_xsel])
                for j in range(NCAP):
                    for k in range(8):
                        TR(psb[:, k * 128:(k + 1) * 128], xsel3[:, j, k * 128:(k + 1) * 128], ident_b[:, :], [b_xsel, b_const], [b_psb])
                    CP("act", xselT3[:, :, j * 128:(j + 1) * 128], psb[:, :].rearrange("p (k n) -> p k n", k=8), [b_psb], [b_xselT])
                for f in range(8):
                    for hh in range(2):
                        bg, b_bg = bank()
                        for k in range(8):
                            MM(bg[:, 0:HC], wg3[:, k, f * 128:(f + 1) * 128], xselT3[:, k, hh * HC:(hh + 1) * HC], [b_wg, b_xselT], [b_bg],
                               start=(k == 0), stop=(k == 7))
                        bl, b_bl = bank()
                        for k in range(8):
                            MM(bl[:, 0:HC], wg3[:, k, 1024 + f * 128:1024 + (f + 1) * 128], xselT3[:, k, hh * HC:(hh + 1) * HC], [b_wg, b_xselT], [b_bl],
                               start=(k == 0), stop=(k == 7))
                        c0 = e_ * 16 + f
                        TS("dve", gtmp[:, :], bg[:, 0:HC], bguT[:, c0:c0 + 1], ALU.add, [b_bg, b_bguT], [b_gtmp], s2=7.0, op1=ALU.min)
                        ACT(stmp[:, :], gtmp[:, :], AF.Sigmoid, [b_gtmp], [b_stmp], scale=1.702)
                        TS("dve", ltmp[:, :], bl[:, 0:HC], bguT[:, c0 + 8:c0 + 9], ALU.add, [b_bl, b_bguT], [b_ltmp], s2=8.0, op1=ALU.min)
                        TT("pool", gtmp[:, :], gtmp[:, :], stmp[:, :], ALU.mult, [b_gtmp, b_stmp], [b_gtmp])
                        STT(actT3[:, f, hh * HC:(hh + 1) * HC], ltmp[:, :], -6.0, gtmp[:, :], ALU.max, ALU.mult, [b_gtmp, b_ltmp], [b_actT])
                        if pending:
                            pending.pop(0)()
                for j in range(NCAP):
                    ys_, b_ys_ = ysb[0]
                    for n in range(2):
                        bk, b_bk = bank()
                        for f in range(8):
                            MM(bk[:, 0:512], actT3[:, f, j * 128:(j + 1) * 128], wd3[:, f, n * 512:(n + 1) * 512], [b_actT, b_wd], [b_bk],
                               start=(f == 0), stop=(f == 7))
                        TT("dve", ys_[:, n * 512:(n + 1) * 512], bk[:, 0:512], bd_[:, n * 512:(n + 1) * 512], ALU.add, [b_bk, b_bd_], [b_ys_])
                        if pending:
                            pending.pop(0)()
                    DMA("sp", ys_d[e_ * CAP + j * 128:e_ * CAP + (j + 1) * 128, :], ys_[:, :], [b_ys_], [b_ysall])
                while pending:
                    pending.pop(0)()
            P.barrier()
            AR.off = mark
            gth = [AR.f32(1024) for i in range(4)]
            for k in range(4):
                P.op("pool", lambda e, k=k: e.memset(gth[k][0][:, :], 0.0), [], [gth[k][1]])
            accs = [AR.f32(1024) for _ in range(2)]
            lnw = []
            for _ in range(2):
                a1, bb1 = AR.f32(1024)
                a2, bb2 = AR.f32(12)
                a3, bb3 = AR.f32(2)
                a4, bb4 = AR.f32(2)
                lnw.append((a1, bb1, a2[:, :].rearrange("p (a b) -> p a b", a=2), bb2, a3, bb3, a4, bb4))
            for t in range(32):
                for k in range(4):
                    q = idx_ctr[0] % 8
                    idx_ctr[0] += 1
                    CP("dve", idx_t[q][:, :], dest[:, t * 4 + k:t * 4 + k + 1], [b_dest], [b_idx[q]])
                    P.dma("pool", lambda e, k=k, q=q: e.indirect_dma_start(
                        out=gth[k][0][:, :], out_offset=None, in_=ys_d,
                        in_offset=bass.IndirectOffsetOnAxis(ap=idx_t[q][:, :], axis=0),
                        bounds_check=bc_reg(e), oob_is_err=False), [b_ysall, b_idx[q]], [gth[k][1]])
                ac, b_ac = accs[t % 2]
                TS("dve", ac[:, :], gth[0][0][:, :], gate3[:, t, 0:1], ALU.mult, [gth[0][1], b_gate], [b_ac])
                for k in range(1, 4):
                    STT(ac[:, :], gth[k][0][:, :], gate3[:, t, k:k + 1], ac[:, :], ALU.mult, ALU.add, [gth[k][1], b_gate, b_ac], [b_ac])
                rb2 = [b_xres[2 * t], b_xres[2 * t + 1]]
                ln_epilogue(128, t * 128, [ac[:, 0:512], ac[:, 512:1024]], [b_ac, b_ac], xres, rb2, G, Bt, b_gb, dst, dst_bufs_fn(t), lnw[t % 2], False)
            P.barrier()

        for l in range(n_layers):
            if "mixer" in skip:
                AR.reset()
                cpb = [AR.f32(1024) for _ in range(2)]
                for t in range(32):
                    a, bb = cpb[t % 2]
                    DMA("sp", a[:, :], x_in[t * 128:(t + 1) * 128, :], [], [bb])
                    DMA("sp", xres[t * 128:(t + 1) * 128, :], a[:, :], [bb], [b_xres[2 * t], b_xres[2 * t + 1]])
                P.barrier()
            elif l == 0:
                mixer(l, x_in, lambda ci: [], xres, lambda ci: [b_xres[ci]])
            else:
                mixer(l, xres, lambda ci: [b_xres[ci]], xres, lambda ci: [b_xres[ci]])
            if stop_after == (l, "mixer"):
                break
            if "xattn" not in skip:
                xattn(l)
            if stop_after == (l, "xattn"):
                break
            last = (l == n_layers - 1) and stop_after is None
            if last:
                moe(l, out_d, lambda t: [b_out])
            else:
                moe(l, xres, lambda t: [b_xres[2 * t], b_xres[2 * t + 1]])
            if stop_after == (l, "moe"):
                break
        if stop_after is not None:
            AR.reset()
            fin = [AR.f32(1024) for _ in range(2)]
            for t in range(32):
                a, bb = fin[t % 2]
                DMA("sp", a[:, :], xres[t * 128:(t + 1) * 128, :], [b_xres[2 * t], b_xres[2 * t + 1]], [bb])
                DMA("sp", out_d[t * 128:(t + 1) * 128, :], a[:, :], [bb], [b_out])
        P.barrier()
        print("ops:", P.n_ops, {e: len(s) for e, s in P.streams.items()})
        P.emit()
    return nc


def make_consts():
    i = np.arange(64)
    c = {
        "c_ident": np.eye(128, dtype=np.float32),
        "c_t1": (i[:, None] <= i[None, :]).astype(np.float32),
        "c_negc": np.where(i[None, :] >= i[:, None], 0.0, NEG).astype(np.float32),
        "c_negs": np.where(i[None, :] < i[:, None], 0.0, NEG).astype(np.float32),
        "c_tri": (np.arange(128)[:, None] < np.arange(128)[None, :]).astype(np.float32),
        "c_eoff": np.tile((np.arange(NE) * CAP).astype(np.float32)[None, :], (128, 1)),
        "c_ones": np.ones((128, 128), np.float32),
        "c_cp": np.tile(np.arange(4, dtype=np.float32)[None, :], (128, 4)),
    }
    return c


_NAMES = ["w_in", "mlstm_b_i", "mlstm_b_f", "mlstm_norm_w", "gdn_conv_w", "gdn_a_log", "gdn_dt_bias", "gdn_norm_w", "w_out",
          "ln1_g", "ln1_b", "xa_wq", "xa_wk", "xa_wv", "xa_wo", "ln2_g", "ln2_b", "router_w", "router_b",
          "exp_w_gu", "exp_b_gu", "exp_w_down", "exp_b_down", "ln3_g", "ln3_b"]


def run(inputs, n_layers=L, stop_after=None, cores=8, max_groups=None, small=False, skip=(), trace=False):
    nc = build(n_layers, stop_after, max_groups=max_groups, small=small, skip=skip)
    consts = make_consts()
    shared = {k: np.ascontiguousarray(np.asarray(inputs[k], dtype=np.float32)) for k in _NAMES}
    if small:
        names = small if isinstance(small, (set, tuple, list)) else ("xa_wq", "xa_wk", "xa_wv", "xa_wo", "router_w", "exp_w_gu", "exp_b_gu", "exp_w_down", "exp_b_down")
        for k in names:
            shared[k] = np.zeros([1] * shared[k].ndim, np.float32)
    in_maps = []
    for c in range(cores):
        m = dict(shared)
        m.update(consts)
        m["x"] = np.ascontiguousarray(np.asarray(inputs["x"][c], dtype=np.float32))
        m["mem"] = np.ascontiguousarray(np.asarray(inputs["mem"][c], dtype=np.float32))
        in_maps.append(m)
    res = run_bass_kernel_spmd(nc, in_maps, core_ids=list(range(cores)), **({"trace": True} if trace else {}))
    if trace:
        print("EXEC_TIME_NS", res.exec_time_ns)
    return np.stack([np.asarray(r["out"]) for r in res.results], axis=0)


def kernel(**inputs):
    return run(inputs).astype(np.float32)
```

```python
import numpy as np
from contextlib import ExitStack
import concourse.bass as bass
import concourse.mybir as mybir
from concourse.bass_utils import run_bass_kernel_spmd

F32 = mybir.dt.float32
BF16 = mybir.dt.bfloat16
I32 = mybir.dt.int32
AF = mybir.ActivationFunctionType
ALU = mybir.AluOpType
AX = mybir.AxisListType

S = 4096
D = 1024
L = 4
NE = 32
CAP = 1024
NCAP = CAP // 128
ALPHA = (2.0 * L) ** 0.25
NEG = -30000.0
GT = 256
NCG = GT // 64

import os as _os
SAME_ENGINE_SYNC = _os.environ.get("SES", "1") == "1"
EPOCH = 16000
N_DMA_SEMS = 24

C_MLQ, C_MLK, C_MLV, C_MLO, C_MLI, C_MLF = 0, 256, 512, 1024, 1536, 1540
C_GDQ, C_GDK, C_GDV, C_GDZ, C_GDB, C_GDA = 1544, 2056, 2568, 3080, 3592, 3596


class Buf:
    __slots__ = ("name", "w", "r", "excl")

    def __init__(self, name, excl=False):
        self.name = name
        self.w = None
        self.r = []
        self.excl = excl


class Prog:
    ENGS = ("pe", "dve", "act", "pool", "sp")

    def __init__(self, nc, es):
        self.nc = nc
        self.es = es
        self.streams = {e: [] for e in self.ENGS}
        self.count = {e: 0 for e in self.ENGS}
        self.known = {e: {} for e in self.ENGS}
        self.sems = {}
        self.dma_next = {e: 0 for e in self.ENGS}
        self.dma_uses = {}
        self.n_ops = 0

    def sem(self, key):
        s = self.sems.get(key)
        if s is None:
            s = self.es.enter_context(self.nc.semaphore("s_%s_%s" % key))
            self.sems[key] = s
        return s

    def _wait(self, eng, tok):
        key, val = tok
        if key[0] == eng and (eng == "pe" or not SAME_ENGINE_SYNC):
            return
        if self.known[eng].get(key, 0) >= val:
            return
        self.known[eng][key] = val
        self.streams[eng].append(("wait", key, val))

    def _deps(self, eng, reads, writes):
        for b in reads:
            if b.w is not None:
                self._wait(eng, b.w)
        for b in writes:
            if b.w is not None:
                self._wait(eng, b.w)
            for t in b.r:
                self._wait(eng, t)

    def _commit(self, tok, reads, writes):
        for b in reads:
            b.r.append(tok)
        for b in writes:
            b.w = tok
            b.r = []

    def op(self, eng, fn, reads=(), writes=()):
        ex = [b for b in reads if b.excl]
        if ex:
            reads = [b for b in reads if not b.excl]
            writes = list(writes) + ex
        self._deps(eng, reads, writes)
        n = self.count[eng]
        self.count[eng] = n + 1
        key = (eng, n // EPOCH)
        self.sem(key)
        tok = (key, n % EPOCH + 1)
        self.streams[eng].append(("op", fn, key, 1))
        self._commit(tok, reads, writes)
        self.n_ops += 1
        return tok

    def dma(self, eng, fn, reads=(), writes=()):
        self._deps(eng, reads, writes)
        i = self.dma_next[eng]
        self.dma_next[eng] = (i + 1) % N_DMA_SEMS
        key = ("d" + eng, i)
        uses = self.dma_uses.get(key, 0)
        self.sem(key)
        if uses > 0:
            self._wait(eng, (key, 16 * uses))
        self.dma_uses[key] = uses + 1
        tok = (key, 16 * (uses + 1))
        self.streams[eng].append(("op", fn, key, 16))
        self._commit(tok, reads, writes)
        self.n_ops += 1
        return tok

    def barrier(self):
        toks = []
        for e in self.ENGS:
            n = self.count[e]
            if n > 0:
                toks.append(((e, (n - 1) // EPOCH), (n - 1) % EPOCH + 1))
        for k, u in self.dma_uses.items():
            toks.append((k, 16 * u))
        for e in self.ENGS:
            for t in toks:
                if t[0][0] == e:
                    continue
                self._wait(e, t)

    def emit(self):
        nc = self.nc
        engmap = {"pe": "tensor", "dve": "vector", "act": "scalar", "pool": "gpsimd", "sp": "sync"}
        with nc.Block() as block:
            for e in self.ENGS:
                stream = self.streams[e]
                if not stream:
                    continue

                def body(engine, stream=stream):
                    for it in stream:
                        if it[0] == "wait":
                            engine.wait_ge(self.sems[it[1]], it[2])
                        else:
                            it[1](engine).then_inc(self.sems[it[2]], it[3])

                getattr(block, engmap[e])(body)


class Arena:
    def __init__(self, t, ncols):
        self.t = t
        self.n = ncols
        self.off = 0
        self.k = 0

    def reset(self):
        self.off = 0

    def _take(self, c32):
        assert self.off + c32 <= self.n, ("arena overflow", self.off, c32, self.n)
        a = self.t[:, self.off:self.off + c32]
        self.off += c32
        self.k += 1
        return a, Buf("ar%d" % self.k)

    def f32(self, cols):
        return self._take(cols)

    def bf16(self, cols):
        a, b = self._take((cols + 1) // 2)
        return a.bitcast(BF16), b

    def i32(self, cols):
        a, b = self._take(cols)
        return a.bitcast(I32), b


def bc3(ap, shape, axis):
    return ap.unsqueeze(axis).to_broadcast(list(shape))


def build(n_layers=L, stop_after=None, dbg=False, max_groups=None, small=False, skip=()):
    nc = bass.Bass("TRN2", target_bir_lowering=False)
    es = ExitStack()

    BIG = ("xa_wq", "xa_wk", "xa_wv", "xa_wo", "router_w", "exp_w_gu", "exp_b_gu", "exp_w_down", "exp_b_down")

    def din(name, shape, dt=F32):
        if small and name in (small if isinstance(small, (set, tuple, list)) else BIG):
            shape = [1] * len(shape)
        return nc.dram_tensor(name, list(shape), dt, kind="ExternalInput").ap()

    x_in = din("x", [S, D])
    mem_in = din("mem", [256, D])
    w_in = din("w_in", [L, D, 3600])
    b_i = din("mlstm_b_i", [L, 4])
    b_f = din("mlstm_b_f", [L, 4])
    ml_nw = din("mlstm_norm_w", [L, 512])
    conv_w = din("gdn_conv_w", [L, 4, 1536])
    a_log = din("gdn_a_log", [L, 4])
    dt_b = din("gdn_dt_bias", [L, 4])
    gd_nw = din("gdn_norm_w", [L, 128])
    w_out = din("w_out", [L, D, D])
    lng = [din("ln%d_g" % i, [L, D]) for i in (1, 2, 3)]
    lnb = [din("ln%d_b" % i, [L, D]) for i in (1, 2, 3)]
    xa_w = [din("xa_w" + n, [L, D, D]) for n in "qkvo"]
    r_w = din("router_w", [L, D, NE])
    r_b = din("router_b", [L, NE])
    w_gu = din("exp_w_gu", [L, NE, D, 2 * D])
    b_gu = din("exp_b_gu", [L, NE, 2 * D])
    w_dn = din("exp_w_down", [L, NE, D, D])
    b_dn = din("exp_b_down", [L, NE, D])
    c_ident = din("c_ident", [128, 128])
    c_t1 = din("c_t1", [64, 64])
    c_negc = din("c_negc", [64, 64])
    c_negs = din("c_negs", [64, 64])
    c_tri = din("c_tri", [128, 128])
    c_eoff = din("c_eoff", [128, NE])
    c_ones = din("c_ones", [128, 128])
    c_cp = din("c_cp", [128, 16])
    out_d = nc.dram_tensor("out", [S, D], F32, kind="ExternalOutput").ap()
    xres = nc.dram_tensor("xres", [S, D], F32, kind="Internal").ap()
    xs_d = nc.dram_tensor("xs_d", [NE * CAP, D], BF16, kind="Internal").ap()
    ys_d = nc.dram_tensor("ys_d", [NE * CAP, D], F32, kind="Internal").ap()
    b_xres = [Buf("xres%d" % i) for i in range(64)]
    b_out = Buf("out")
    b_xs = [Buf("xs%d" % e) for e in range(NE)]
    b_ys = [Buf("ys%d" % e) for e in range(NE)]

    P = Prog(nc, es)

    def sb(name, shape, dt):
        return es.enter_context(nc.sbuf_tensor(name, list(shape), dt))

    with es:
        ident_f = sb("ident_f", [128, 128], F32)
        ident_b = sb("ident_b", [128, 128], BF16)
        ones_f = sb("ones_f", [128, 128], F32)
        ones_b = sb("ones_b", [128, 128], BF16)
        tri_b = sb("tri_b", [128, 128], BF16)
        t1_f = sb("t1_f", [64, 64], F32)
        negc = sb("negc", [64, 64], F32)
        negs = sb("negs", [64, 64], F32)
        eoff = sb("eoff", [128, NE], F32)
        dest_t = sb("dest_t", [128, 512], I32)
        cpoff = sb("cpoff", [128, 16], F32)
        idx_t = [sb("idx%d" % i, [128, 1], I32) for i in range(8)]
        b_idx = [Buf("idx%d" % i) for i in range(8)]
        idx_ctr = [0]
        b_const = Buf("const")
        ARENA_COLS = 49000
        arena_t = sb("arena", [128, ARENA_COLS], F32)
        AR = Arena(arena_t, ARENA_COLS)
        banks = [es.enter_context(nc.psum_tensor("ps%d" % i, [128, 512], F32)) for i in range(7)]
        b_banks = [Buf("bank%d" % i, True) for i in range(7)]
        psb = es.enter_context(nc.psum_tensor("psb", [128, 1024], BF16))
        b_psb = Buf("psb", True)
        bank_ctr = [0]

        def bank():
            i = bank_ctr[0] % 7
            bank_ctr[0] += 1
            return banks[i], b_banks[i]

        def MM(out, lhsT, rhs, R, W, start=True, stop=True):
            P.op("pe", lambda e: e.matmul(out, lhsT, rhs, start=start, stop=stop), R, W)

        def TR(out, in_, idn, R, W):
            P.op("pe", lambda e: e.transpose(out, in_, idn), R, W)

        def TT(eng, out, a, b, op, R, W):
            P.op(eng, lambda e: e.tensor_tensor(out, a, b, op), R, W)

        def TS(eng, out, a, s1, op0, R, W, s2=None, op1=None):
            if op1 is None:
                P.op(eng, lambda e: e.tensor_scalar(out, a, s1, None, op0), R, W)
            else:
                P.op(eng, lambda e: e.tensor_scalar(out, a, s1, s2, op0, op1), R, W)

        def STT(out, a, s, b, op0, op1, R, W):
            P.op("dve", lambda e: e.scalar_tensor_tensor(out, a, s, b, op0, op1), R, W)

        def ACT(out, in_, func, R, W, bias=0.0, scale=1.0):
            P.op("act", lambda e: e.activation(out, in_, func, bias=bias, scale=scale), R, W)

        def CP(eng, out, in_, R, W):
            if eng == "act":
                P.op("act", lambda e: e.copy(out, in_), R, W)
            else:
                P.op(eng, lambda e: e.tensor_copy(out, in_), R, W)

        def DMA(eng, out, in_, R, W):
            return P.dma(eng, lambda e: e.dma_start(out=out, in_=in_), R, W)

        def rsqrt_(out, in_, scale, eps, R, W, tmp):
            ACT(tmp[0], in_, AF.Sqrt, R, [tmp[1]], bias=eps, scale=scale)
            P.op("dve", lambda e: e.reciprocal(out, tmp[0]), [tmp[1]], W)

        DMA("sp", ident_f[:], c_ident, [], [b_const])
        DMA("sp", ones_f[:], c_ones, [], [b_const])
        DMA("sp", t1_f[:], c_t1, [], [b_const])
        DMA("sp", negc[:], c_negc, [], [b_const])
        DMA("sp", negs[:], c_negs, [], [b_const])
        DMA("sp", eoff[:], c_eoff, [], [b_const])
        DMA("sp", cpoff[:], c_cp, [], [b_const])
        DMA("pool", ident_b[:], c_ident, [], [b_const])
        DMA("pool", ones_b[:], c_ones, [], [b_const])
        DMA("pool", tri_b[:], c_tri, [], [b_const])
        P.barrier()

        bc_state = {}

        def bc_reg(e):
            if "r" not in bc_state:
                bc_state["r"] = e.alloc_register("bc")
                e.reg_mov(bc_state["r"], NE * CAP - 1)
            return bc_state["r"]

        def ln_epilogue(Pn, row0, hparts, hbufs, xsrc, xsrc_bufs, G, Bt, b_gb, dst, dst_bufs, work, want_xT):
            xo, b_xo, stt_, b_st, mv, b_mv, sc, b_sc = work
            DMA("sp", xo[0:Pn, :], xsrc[row0:row0 + Pn, :], xsrc_bufs, [b_xo])
            for n in range(2):
                STT(xo[0:Pn, n * 512:(n + 1) * 512], xo[0:Pn, n * 512:(n + 1) * 512], ALPHA, hparts[n],
                    ALU.mult, ALU.add, [b_xo, hbufs[n]], [b_xo])
            for n in range(2):
                P.op("dve", lambda e, n=n: e.bn_stats(stt_[0:Pn, n, :], xo[0:Pn, n * 512:(n + 1) * 512]), [b_xo], [b_st])
            P.op("dve", lambda e: e.bn_aggr(mv[0:Pn, :], stt_[0:Pn, :, :]), [b_st], [b_mv])
            rsqrt_(sc[0:Pn, 1:2], mv[0:Pn, 1:2], 1.0, 1e-5, [b_mv], [b_sc], (sc[0:Pn, 0:1], b_sc))
            TS("dve", xo[0:Pn, :], xo[0:Pn, :], mv[0:Pn, 0:1], ALU.subtract, [b_xo, b_mv, b_sc], [b_xo],
               s2=sc[0:Pn, 1:2], op1=ALU.mult)
            TT("pool", xo[0:Pn, :], xo[0:Pn, :], G[0:Pn, :], ALU.mult, [b_xo, b_gb], [b_xo])
            TT("pool", xo[0:Pn, :], xo[0:Pn, :], Bt[0:Pn, :], ALU.add, [b_xo, b_gb], [b_xo])
            DMA("sp", dst[row0:row0 + Pn, :], xo[0:Pn, :], [b_xo], dst_bufs)

        def load_bcast(dst, src_row, Pn, W):
            DMA("sp", dst[0:Pn, :], src_row.partition_broadcast(Pn), [], W)

        def load_xT(src, src_bufs, row0, ntok, xTg3, b_xTg, stg):
            for t in range(ntok // 128):
                st_, b_st_ = stg
                DMA("sp", st_[:, :], src[row0 + t * 128:row0 + (t + 1) * 128, :], src_bufs, [b_st_])
                for half in range(2):
                    bk, b_bk = bank()
                    for k in range(8):
                        TR(bk[:, k * 64:(k + 1) * 64], st_[half * 64:(half + 1) * 64, k * 128:(k + 1) * 128],
                           ident_f[half * 64:(half + 1) * 64, half * 64:(half + 1) * 64], [b_st_, b_const], [b_bk])
                    o = t * 128 + half * 64
                    CP("act", xTg3[:, :, o:o + 64], bk[:, 0:512].rearrange("p (k n) -> p k n", k=8), [b_bk], [b_xTg])

        def mixer(l, src, src_bufs_fn, dst, dst_bufs_fn):
            AR.reset()
            win, b_win = AR.bf16(8 * 3600)
            win3 = win.rearrange("p (k n) -> p k n", k=8)
            wout, b_wout = AR.bf16(8 * 1024)
            wout3 = wout.rearrange("p (k n) -> p k n", k=8)
            for k in range(8):
                DMA("pool", win3[:, k, :], w_in[l, k * 128:(k + 1) * 128, :], [], [b_win])
            DMA("pool", wout3, w_out[l].rearrange("(k p) n -> p k n", p=128), [], [b_wout])
            G, b_gb = AR.f32(1024)
            Bt, _ = AR.f32(1024)
            load_bcast(G, lng[0][l:l + 1, :], 64, [b_gb])
            load_bcast(Bt, lnb[0][l:l + 1, :], 64, [b_gb])
            mlnw, b_small = AR.f32(512)
            load_bcast(mlnw, ml_nw[l:l + 1, :], 64, [b_small])
            bif, _ = AR.f32(8)
            load_bcast(bif[:, 0:4], b_i[l:l + 1, :], 64, [b_small])
            load_bcast(bif[:, 4:8], b_f[l:l + 1, :], 64, [b_small])
            negA, _ = AR.f32(4)
            dtb, _ = AR.f32(4)
            load_bcast(negA, a_log[l:l + 1, :], 64, [b_small])
            load_bcast(dtb, dt_b[l:l + 1, :], 64, [b_small])
            ACT(negA[0:64, :], negA[0:64, :], AF.Exp, [b_small], [b_small])
            TS("dve", negA[0:64, :], negA[0:64, :], -1.0, ALU.mult, [b_small], [b_small])
            gdnw, _ = AR.f32(1)
            DMA("sp", gdnw[:, 0:1], gd_nw[l].rearrange("(p o) -> p o", o=1), [], [b_small])
            cwr, b_cwr = AR.f32(128)
            cw, _ = AR.f32(48)
            DMA("sp", cwr[0:48, :], conv_w[l].rearrange("j (c p) -> (j c) p", p=128), [], [b_cwr])
            bk, b_bk = bank()
            TR(bk[:, 0:48], cwr[0:48, :], ident_f[0:48, 0:48], [b_cwr, b_const], [b_bk])
            CP("dve", cw[:, :], bk[:, 0:48], [b_bk], [b_small])
            Cn, b_Cn = AR.f32(4 * 129)
            Cn3 = Cn[0:64, :].rearrange("p (h n) -> p h n", h=4)
            Cnb, b_Cnb = AR.bf16(4 * 130)
            Cnb3 = Cnb[0:64, 0:4 * 130].rearrange("p (h n) -> p h n", h=4)
            Sst, b_S = AR.f32(512)
            Sst3 = Sst.rearrange("p (h n) -> p h n", h=4)
            Sb, b_Sb = AR.bf16(512)
            Sb3 = Sb.rearrange("p (h n) -> p h n", h=4)
            carry, b_carry = AR.f32(36)
            carry3 = carry.rearrange("p (c n) -> p c n", c=12)
            P.op("pool", lambda e: e.memset(Cn[:, :], 0.0), [], [b_Cn])
            P.op("pool", lambda e: e.memset(Cnb[:, :], 0.0), [], [b_Cnb])
            P.op("pool", lambda e: e.memset(Sst[:, :], 0.0), [], [b_S])
            P.op("pool", lambda e: e.memset(Sb[:, :], 0.0), [], [b_Sb])
            P.op("pool", lambda e: e.memset(carry[:, :], 0.0), [], [b_carry])
            mlqT, b_mlqT = AR.bf16(4 * GT)
            mlqT3 = mlqT[0:64, :].rearrange("p (h n) -> p h n", h=4)
            mlkT, b_mlkT = AR.bf16(4 * GT)
            mlkT3 = mlkT[0:64, :].rearrange("p (h n) -> p h n", h=4)
            siluz, b_siluz = AR.f32(4 * GT)
            siluz3 = siluz.rearrange("p (h n) -> p h n", h=4)
            gdq, b_gdq = AR.bf16(4 * GT)
            gdq3 = gdq.rearrange("p (h n) -> p h n", h=4)
            gdk, b_gdk = AR.bf16(4 * GT)
            gdk3 = gdk.rearrange("p (h n) -> p h n", h=4)
            gdkf, b_gdkf = AR.f32(4 * GT)
            gdkf3 = gdkf.rearrange("p (h n) -> p h n", h=4)
            gdvf, b_gdvf = AR.f32(4 * GT)
            gdvf3 = gdvf.rearrange("p (h n) -> p h n", h=4)
            stage, b_stage = AR.f32(GT + 3)
            acc, b_acc = AR.f32(GT)
            post, b_post = AR.f32(GT)
            sq, b_sq = AR.f32(GT)
            rn, b_rn = AR.f32(GT)
            gates, b_gates = AR.f32(NCG * 16)
            gates3 = gates[0:64, :].rearrange("p (c n) -> p c n", c=NCG)
            gt, b_gt = AR.f32(NCG * 16)
            gt3 = gt[0:64, :].rearrange("p (c n) -> p c n", c=NCG)
            LG, b_LG = AR.f32(NCG * 8)
            LG3 = LG[0:64, :].rearrange("p (c n) -> p c n", c=NCG)
            ipre, b_ipre = AR.f32(NCG * 4)
            ipre3 = ipre[0:64, :].rearrange("p (c n) -> p c n", c=NCG)
            beta, b_beta = AR.f32(NCG * 4)
            beta3 = beta[0:64, :].rearrange("p (c n) -> p c n", c=NCG)
            CS, b_CS = AR.f32(NCG * 8)
            CS3 = CS[0:64, :].rearrange("p (c n) -> p c n", c=NCG)
            LA, b_LA = AR.f32(NCG * 8)
            LA3 = LA[0:64, :].rearrange("p (c n) -> p c n", c=NCG)
            EG128, b_EG = AR.f32(NCG * 4)
            EG3 = EG128.rearrange("p (c n) -> p c n", c=NCG)
            der, b_der = AR.f32(NCG * 24)
            der3 = der[0:64, :].rearrange("p (c n) -> p c n", c=NCG)
            mlk_tm, b_mlk_tm = AR.f32(256)
            mlv1, b_mlv1 = AR.bf16(4 * 130)
            mlv13 = mlv1[0:64, 0:4 * 130].rearrange("p (h n) -> p h n", h=4)
            P.op("pool", lambda e: e.memset(mlv1[:, :], 1.0), [], [b_mlv1])
            sigo, b_sigo = AR.f32(512)
            LcolF, b_Lcol = AR.f32(4 * 64)
            LcolF3 = LcolF[0:64, :].rearrange("p (h n) -> p h n", h=4)
            LcolG, _ = AR.f32(4 * 128)
            LcolG3 = LcolG[0:64, :].rearrange("p (h n) -> p h n", h=4)
            t44, b_t44 = AR.f32(256)
            t443 = t44[0:64, :].rearrange("p (h n) -> p h n", h=4)
            DTm, b_DT = AR.f32(256)
            DTm3 = DTm[0:64, :].rearrange("p (h n) -> p h n", h=4)
            Ef, b_Ef = AR.f32(256)
            Ef3 = Ef[0:64, :].rearrange("p (h n) -> p h n", h=4)
            DecS, b_DecS = AR.f32(256)
            DecS3 = DecS[0:64, :].rearrange("p (h n) -> p h n", h=4)
            DecT, b_DecT = AR.f32(256)
            DecT3 = DecT[0:64, :].rearrange("p (h n) -> p h n", h=4)
            E128, b_E128 = AR.f32(256)
            E1283 = E128.rearrange("p (h n) -> p h n", h=4)
            qtil, b_qtil = AR.bf16(256)
            qtil3 = qtil[0:64, :].rearrange("p (h n) -> p h n", h=4)
            gqt, b_gqt = AR.bf16(256)
            gqt3 = gqt.rearrange("p (h n) -> p h n", h=4)
            sint, b_sint = AR.bf16(256)
            sint3 = sint[0:64, :].rearrange("p (h n) -> p h n", h=4)
            rden, b_rden = AR.f32(8)
            hml, b_hml = AR.f32(512)
            hml3 = hml[0:64, :].rearrange("p (h n) -> p h n", h=4)
            hsq, b_hsq = AR.f32(512)
            hsq3 = hsq[0:64, :].rearrange("p (h n) -> p h n", h=4)
            ssum, b_ssum = AR.f32(8)
            kw, b_kw = AR.bf16(256)
            kw3 = kw[0:64, :].rearrange("p (h n) -> p h n", h=4)
            mixT, b_mixT = AR.bf16(8 * 64)
            mixT3 = mixT.rearrange("p (f n) -> p f n", f=8)
            bv, b_bv = AR.bf16(512)
            bv3 = bv[0:64, :].rearrange("p (h n) -> p h n", h=4)
            kbg, b_kbg = AR.bf16(512)
            kbg3 = kbg[0:64, :].rearrange("p (h n) -> p h n", h=4)
            kend, b_kend = AR.bf16(512)
            kend3 = kend[0:64, :].rearrange("p (h n) -> p h n", h=4)
            P0f, b_P0f = AR.f32(256)
            P0f3 = P0f[0:64, :].rearrange("p (h n) -> p h n", h=4)
            Pm = [AR.bf16(256) for _ in range(2)]
            PT = [AR.bf16(256) for _ in range(2)]
            Yb = [AR.bf16(256) for _ in range(2)]
            nwT, b_nwT = AR.bf16(256)
            nwT3 = nwT.rearrange("p (h n) -> p h n", h=4)
            vnew, b_vnew = AR.bf16(512)
            vnew3 = vnew[0:64, :].rearrange("p (h n) -> p h n", h=4)
            qkd, b_qkd = AR.bf16(256)
            qkd3 = qkd[0:64, :].rearrange("p (h n) -> p h n", h=4)
            osq, b_osq = AR.f32(256)
            orstd, b_orstd = AR.f32(256)
            ot1, b_ot1 = AR.f32(256)
            lnw = []
            for _ in range(2):
                a1, bb1 = AR.f32(1024)
                a2, bb2 = AR.f32(12)
                a3, bb3 = AR.f32(2)
                a4, bb4 = AR.f32(2)
                lnw.append((a1, bb1, a2[:, :].rearrange("p (a b) -> p a b", a=2), bb2, a3, bb3, a4, bb4))

            xTg, b_xTg = AR.bf16(8 * GT)
            xT3 = xTg.rearrange("p (k n) -> p k n", k=8)
            xstg = AR.f32(1024)
            for g in range(max_groups or (S // GT)):
                tok0 = 0
                xbufs = [b_xTg]
                load_xT(src, [bb for c in range(NCG) for bb in src_bufs_fn(g * NCG + c)], g * GT, GT, xT3, b_xTg, xstg)
                for qk in range(2):
                    for h in range(4):
                        col0 = (C_MLQ if qk == 0 else C_MLK) + h * 64
                        bk, b_bk = bank()
                        for k in range(8):
                            MM(bk[0:64, 0:GT], win3[:, k, col0:col0 + 64], xT3[:, k, 0:GT],
                               [b_win] + xbufs, [b_bk], start=(k == 0), stop=(k == 7))
                        if qk == 0:
                            ACT(mlqT3[:, h, :], bk[0:64, 0:GT], AF.Copy, [b_bk], [b_mlqT], scale=0.125)
                        else:
                            CP("dve", mlkT3[:, h, :], bk[0:64, 0:GT], [b_bk], [b_mlkT])
                for h in range(4):
                    bk, b_bk = bank()
                    for k in range(8):
                        MM(bk[:, 0:GT], win3[:, k, C_GDZ + h * 128:C_GDZ + (h + 1) * 128], xT3[:, k, 0:GT],
                           [b_win] + xbufs, [b_bk], start=(k == 0), stop=(k == 7))
                    ACT(siluz3[:, h, :], bk[:, 0:GT], AF.Silu, [b_bk], [b_siluz])
                for c in range(12):
                    bk, b_bk = bank()
                    for k in range(8):
                        MM(bk[:, 0:GT], win3[:, k, C_GDQ + c * 128:C_GDQ + (c + 1) * 128], xT3[:, k, 0:GT],
                           [b_win] + xbufs, [b_bk], start=(k == 0), stop=(k == 7))
                    CP("pool", stage[:, 0:3], carry3[:, c, :], [b_carry], [b_stage])
                    CP("act", stage[:, 3:GT + 3], bk[:, 0:GT], [b_bk], [b_stage])
                    CP("pool", carry3[:, c, :], stage[:, GT:GT + 3], [b_stage], [b_carry])
                    TS("dve", acc[:, :], stage[:, 0:GT], cw[:, c:c + 1], ALU.mult, [b_stage, b_small], [b_acc])
                    for j in range(1, 4):
                        STT(acc[:, :], stage[:, j:j + GT], cw[:, j * 12 + c:j * 12 + c + 1], acc[:, :], ALU.mult, ALU.add,
                            [b_stage, b_small, b_acc], [b_acc])
                    if c >= 8:
                        ACT(gdvf3[:, c - 8, :], acc[:, :], AF.Silu, [b_acc], [b_gdvf])
                    else:
                        ACT(post[:, :], acc[:, :], AF.Silu, [b_acc], [b_post])
                        TT("pool", sq[:, :], post[:, :], post[:, :], ALU.mult, [b_post], [b_sq])
                        bk2, b_bk2 = bank()
                        MM(bk2[:, 0:GT], ones_f[:, :], sq[:, :], [b_const, b_sq], [b_bk2])
                        rsqrt_(rn[:, :], bk2[:, 0:GT], 1.0, 1e-6, [b_bk2], [b_rn], (sq[:, :], b_sq))
                        if c < 4:
                            STT(gdq3[:, c, :], post[:, :], 128.0 ** -0.5, rn[:, :], ALU.mult, ALU.mult, [b_post, b_rn], [b_gdq])
                        else:
                            TT("dve", gdkf3[:, c - 4, :], post[:, :], rn[:, :], ALU.mult, [b_post, b_rn], [b_gdkf])
                            CP("pool", gdk3[:, c - 4, :], gdkf3[:, c - 4, :], [b_gdkf], [b_gdk])
                bkg, b_bkg = bank()
                for c in range(NCG):
                    t0 = c * 64
                    for part, col in ((0, C_MLI), (1, C_GDB)):
                        for k in range(8):
                            MM(bkg[0:64, c * 16 + part * 8:c * 16 + part * 8 + 8], xT3[:, k, t0:t0 + 64], win3[:, k, col:col + 8],
                               [b_win, b_xTg], [b_bkg], start=(k == 0), stop=(k == 7))
                CP("dve", gates[0:64, :], bkg[0:64, 0:NCG * 16], [b_bkg], [b_gates])
                TT("dve", gt3[:, :, 0:8], gates3[:, :, 0:8], bc3(bif[0:64, 0:8], [64, NCG, 8], 1), ALU.add, [b_gates, b_small], [b_gt])
                ACT(gt3[:, :, 0:8], gt3[:, :, 0:8], AF.Tanh, [b_gt], [b_gt], scale=1.0 / 15.0)
                TS("dve", ipre3[:, :, :], gt3[:, :, 0:4], 15.0, ALU.mult, [b_gt], [b_ipre])
                ACT(gt3[:, :, 4:8], gt3[:, :, 4:8], AF.Exp, [b_gt], [b_gt], scale=-15.0)
                ACT(gt3[:, :, 4:8], gt3[:, :, 4:8], AF.Ln, [b_gt], [b_gt], bias=1.0)
                TS("dve", LG3[:, :, 0:4], gt3[:, :, 4:8], -1.0, ALU.mult, [b_gt], [b_LG])
                ACT(beta3[:, :, :], gates3[:, :, 8:12], AF.Sigmoid, [b_gates], [b_beta])
                TT("dve", gt3[:, :, 8:12], gates3[:, :, 12:16], bc3(dtb[0:64, 0:4], [64, NCG, 4], 1), ALU.add, [b_gates, b_small], [b_gt])
                ACT(gt3[:, :, 12:16], gt3[:, :, 8:12], AF.Abs, [b_gt], [b_gt])
                ACT(gt3[:, :, 12:16], gt3[:, :, 12:16], AF.Exp, [b_gt], [b_gt], scale=-1.0)
                ACT(gt3[:, :, 12:16], gt3[:, :, 12:16], AF.Ln, [b_gt], [b_gt], bias=1.0)
                TS("dve", gt3[:, :, 8:12], gt3[:, :, 8:12], 0.0, ALU.max, [b_gt], [b_gt])
                TT("dve", gt3[:, :, 8:12], gt3[:, :, 8:12], gt3[:, :, 12:16], ALU.add, [b_gt], [b_gt])
                TT("dve", LG3[:, :, 4:8], gt3[:, :, 8:12], bc3(negA[0:64, 0:4], [64, NCG, 4], 1), ALU.mult, [b_gt, b_small], [b_LG])
                bkc, b_bkc = bank()
                MM(bkc[0:64, 0:NCG * 8], t1_f[:, :], LG[0:64, 0:NCG * 8], [b_const, b_LG], [b_bkc])
                MM(bkc[0:64, 64:64 + NCG * 8], ones_f[0:64, 0:64], LG[0:64, 0:NCG * 8], [b_const, b_LG], [b_bkc])
                MM(bkc[:, 128:128 + NCG * 8], ones_f[0:64, :], LG[0:64, 0:NCG * 8], [b_const, b_LG], [b_bkc])
                CP("dve", CS[0:64, :], bkc[0:64, 0:NCG * 8], [b_bkc], [b_CS])
                CP("dve", LA[0:64, :], bkc[0:64, 64:64 + NCG * 8], [b_bkc], [b_LA])
                ACT(EG3[:, :, :], bkc[:, 128:128 + NCG * 8].rearrange("p (c n) -> p c n", c=NCG)[:, :, 4:8], AF.Exp, [b_bkc], [b_EG])
                TT("dve", der3[:, :, 0:4], ipre3[:, :, :], CS3[:, :, 0:4], ALU.subtract, [b_ipre, b_CS], [b_der])
                TT("dve", der3[:, :, 4:8], der3[:, :, 0:4], LA3[:, :, 0:4], ALU.add, [b_der, b_LA], [b_der])
                ACT(der3[:, :, 4:8], der3[:, :, 4:8], AF.Exp, [b_der], [b_der])
                ACT(der3[:, :, 8:12], LA3[:, :, 0:4], AF.Exp, [b_LA], [b_der])
                ACT(der3[:, :, 12:16], CS3[:, :, 4:8], AF.Exp, [b_CS], [b_der])
                TT("dve", der3[:, :, 12:16], der3[:, :, 12:16], beta3[:, :, :], ALU.mult, [b_der, b_beta], [b_der])
                TT("dve", der3[:, :, 16:20], LA3[:, :, 4:8], CS3[:, :, 4:8], ALU.subtract, [b_LA, b_CS], [b_der])
                ACT(der3[:, :, 16:20], der3[:, :, 16:20], AF.Exp, [b_der], [b_der])
                TS("dve", der3[:, :, 20:24], beta3[:, :, :], -1.0, ALU.mult, [b_beta], [b_der])

                for c in range(NCG):
                    t0 = c * 64
                    lo = c * 64
                    ci = g * NCG + c
                    xb_ = [b_xTg]
                    bk, b_bk = bank()
                    for k in range(8):
                        MM(bk[0:64, 0:256], xT3[:, k, t0:t0 + 64], win3[:, k, C_MLK:C_MLK + 256], [b_win] + xb_, [b_bk],
                           start=(k == 0), stop=(k == 7))
                    CP("act", mlk_tm[0:64, :], bk[0:64, 0:256], [b_bk], [b_mlk_tm])
                    bk, b_bk = bank()
                    for k in range(8):
                        MM(bk[0:64, 0:512], xT3[:, k, t0:t0 + 64], win3[:, k, C_MLV:C_MLV + 512], [b_win] + xb_, [b_bk],
                           start=(k == 0), stop=(k == 7))
                    CP("dve", mlv13[:, :, 0:128], bk[0:64, 0:512].rearrange("p (h n) -> p h n", h=4), [b_bk], [b_mlv1])
                    bk, b_bk = bank()
                    for k in range(8):
                        MM(bk[0:64, 0:512], xT3[:, k, t0:t0 + 64], win3[:, k, C_MLO:C_MLO + 512], [b_win] + xb_, [b_bk],
                           start=(k == 0), stop=(k == 7))
                    ACT(sigo[0:64, :], bk[0:64, 0:512], AF.Sigmoid, [b_bk], [b_sigo])
                    CP("pool", LcolF3[:, :, :], bc3(LG3[:, c, 0:4], [64, 4, 64], 2), [b_LG], [b_Lcol])
                    CP("pool", LcolG3[:, :, :], bc3(LG3[:, c, 4:8], [64, 4, 128], 2), [b_LG], [b_Lcol])
                    bkM, b_bkM = bank()
                    for h in range(4):
                        MM(bkM[0:64, h * 64:(h + 1) * 64], LcolF3[:, h, :], t1_f[:, :], [b_Lcol, b_const], [b_bkM])
                    for h in range(4):
                        MM(bkM[0:64, 256 + h * 64:256 + (h + 1) * 64], LcolG3[:, h, 0:64], t1_f[:, :], [b_Lcol, b_const], [b_bkM])
                    bkE, b_bkE = bank()
                    for h in range(4):
                        MM(bkE[:, h * 64:(h + 1) * 64], LcolG3[:, h, :], t1_f[:, :], [b_Lcol, b_const], [b_bkE])
                    Mf = bkM[0:64, 0:256].rearrange("p (h n) -> p h n", h=4)
                    Mg = bkM[0:64, 256:512].rearrange("p (h n) -> p h n", h=4)
                    TT("dve", t443, Mf, bc3(der3[:, c, 0:4], [64, 4, 64], 2), ALU.add, [b_bkM, b_der], [b_t44])
                    TT("dve", t443, t443, bc3(negc[:, :], [64, 4, 64], 1), ALU.add, [b_t44, b_const], [b_t44])
                    ACT(DTm3, t443, AF.Exp, [b_t44], [b_DT])
                    ACT(Ef3, Mf, AF.Exp, [b_bkM], [b_Ef])
                    TT("dve", qtil3, mlqT3[:, :, lo:lo + 64], Ef3, ALU.mult, [b_mlqT, b_Ef], [b_qtil])
                    STT(t443, Mg, -1.0, bc3(CS3[:, c, 4:8], [64, 4, 64], 2), ALU.mult, ALU.add, [b_bkM, b_CS, b_t44], [b_t44])
                    TT("dve", t443, t443, bc3(negs[:, :], [64, 4, 64], 1), ALU.add, [b_t44, b_const], [b_t44])
                    ACT(DecS3, t443, AF.Exp, [b_t44], [b_DecS])
                    TT("dve", t443, Mg, bc3(CS3[:, c, 4:8], [64, 4, 64], 2), ALU.subtract, [b_bkM, b_CS, b_t44], [b_t44])
                    TT("dve", t443, t443, bc3(negc[:, :], [64, 4, 64], 1), ALU.add, [b_t44, b_const], [b_t44])
                    ACT(DecT3, t443, AF.Exp, [b_t44], [b_DecT])
                    ACT(E128[:, :], bkE[:, 0:256], AF.Exp, [b_bkE], [b_E128])
                    TT("dve", gqt3, gdq3[:, :, lo:lo + 64], E1283, ALU.mult, [b_gdq, b_E128], [b_gqt])

                    bk, b_bk = bank()
                    for h in range(4):
                        MM(bk[0:64, h * 64:(h + 1) * 64], mlkT3[:, h, lo:lo + 64], mlqT3[:, h, lo:lo + 64], [b_mlkT, b_mlqT], [b_bk])
                    TT("dve", sint3, bk[0:64, 0:256].rearrange("p (h n) -> p h n", h=4), DTm3, ALU.mult, [b_bk, b_DT], [b_sint])
                    for hp in range(2):
                        bk, b_bk = bank()
                        for j in range(2):
                            h = hp * 2 + j
                            MM(bk[0:64, j * 129:(j + 1) * 129], qtil3[:, h, :], Cnb3[:, h, 0:129], [b_qtil, b_Cnb], [b_bk], start=True, stop=False)
                            MM(bk[0:64, j * 129:(j + 1) * 129], sint3[:, h, :], mlv13[:, h, 0:129], [b_sint, b_mlv1], [b_bk], start=False, stop=True)
                        bv_ = bk[0:64, 0:258].rearrange("p (j n) -> p j n", j=2)
                        ACT(rden[0:64, hp * 2:hp * 2 + 2], bv_[:, :, 128], AF.Abs, [b_bk], [b_rden])
                        TS("dve", rden[0:64, hp * 2:hp * 2 + 2], rden[0:64, hp * 2:hp * 2 + 2], 1.0, ALU.max, [b_rden], [b_rden])
                        P.op("dve", lambda e, hp=hp: e.reciprocal(rden[0:64, 4 + hp * 2:4 + hp * 2 + 2], rden[0:64, hp * 2:hp * 2 + 2]), [b_rden], [b_rden])
                        TT("dve", hml3[:, hp * 2:hp * 2 + 2, :], bv_[:, :, 0:128], bc3(rden[0:64, 4 + hp * 2:4 + hp * 2 + 2], [64, 2, 128], 2),
                           ALU.mult, [b_bk, b_rden], [b_hml])
                    TT("pool", hsq3, hml3, hml3, ALU.mult, [b_hml], [b_hsq])
                    P.op("dve", lambda e: e.tensor_reduce(ssum[0:64, 0:4], hsq3, AX.X, ALU.add), [b_hsq], [b_ssum])
                    rsqrt_(ssum[0:64, 4:8], ssum[0:64, 0:4], 1.0 / 128.0, 1e-6, [b_ssum], [b_ssum], (ssum[0:64, 0:4], b_ssum))
                    TT("dve", hml3, hml3, bc3(ssum[0:64, 4:8], [64, 4, 128], 2), ALU.mult, [b_hml, b_ssum], [b_hml])
                    TT("pool", hml[0:64, :], hml[0:64, :], mlnw[0:64, :], ALU.mult, [b_hml, b_small], [b_hml])
                    TT("pool", hml[0:64, :], hml[0:64, :], sigo[0:64, :], ALU.mult, [b_hml, b_sigo], [b_hml])
                    bk, b_bk = bank()
                    for h in range(4):
                        TR(bk[:, h * 64:(h + 1) * 64], hml[0:64, h * 128:(h + 1) * 128], ident_f[0:64, 0:64], [b_hml, b_const], [b_bk])
                    CP("act", mixT3[:, 0:4, :], bk[:, 0:256].rearrange("p (h n) -> p h n", h=4), [b_bk], [b_mixT])
                    TT("dve", kw3, mlk_tm[0:64, :].rearrange("p (h n) -> p h n", h=4), bc3(der3[:, c, 4:8], [64, 4, 64], 2), ALU.mult,
                       [b_mlk_tm, b_der], [b_kw])
                    TT("dve", Cn3, Cn3, bc3(der3[:, c, 8:12], [64, 4, 129], 2), ALU.mult, [b_Cn, b_der], [b_Cn])
                    for hp in range(2):
                        bk, b_bk = bank()
                        for j in range(2):
                            h = hp * 2 + j
                            MM(bk[0:64, j * 129:(j + 1) * 129], kw3[:, h, :], mlv13[:, h, 0:129], [b_kw, b_mlv1], [b_bk])
                        TT("dve", Cn3[:, hp * 2:hp * 2 + 2, :], Cn3[:, hp * 2:hp * 2 + 2, :], bk[0:64, 0:258].rearrange("p (j n) -> p j n", j=2),
                           ALU.add, [b_Cn, b_bk], [b_Cn])
                    CP("pool", Cnb3[:, :, 0:129], Cn3, [b_Cn], [b_Cnb])

                    bkk, b_bkk = bank()
                    bkv, b_bkv = bank()
                    for h in range(4):
                        TR(bkk[0:64, h * 128:(h + 1) * 128], gdkf3[:, h, lo:lo + 64], ident_f[:, :], [b_gdkf, b_const], [b_bkk])
                    for h in range(4):
                        TR(bkv[0:64, h * 128:(h + 1) * 128], gdvf3[:, h, lo:lo + 64], ident_f[:, :], [b_gdvf, b_const], [b_bkv])
                    ktm = bkk[0:64, 0:512].rearrange("p (h n) -> p h n", h=4)
                    vtm = bkv[0:64, 0:512].rearrange("p (h n) -> p h n", h=4)
                    TT("dve", bv3, vtm, bc3(beta3[:, c, :], [64, 4, 128], 2), ALU.mult, [b_bkv, b_beta], [b_bv])
                    TT("dve", kbg3, ktm, bc3(der3[:, c, 12:16], [64, 4, 128], 2), ALU.mult, [b_bkk, b_der], [b_kbg])
                    TT("dve", kend3, ktm, bc3(der3[:, c, 16:20], [64, 4, 128], 2), ALU.mult, [b_bkk, b_der], [b_kend])
                    bk, b_bk = bank()
                    for h in range(4):
                        MM(bk[0:64, h * 64:(h + 1) * 64], gdk3[:, h, lo:lo + 64], gdk3[:, h, lo:lo + 64], [b_gdk], [b_bk])
                    TT("dve", t443, bk[0:64, 0:256].rearrange("p (h n) -> p h n", h=4), bc3(der3[:, c, 20:24], [64, 4, 64], 2), ALU.mult,
                       [b_bk, b_der, b_t44], [b_t44])
                    TT("dve", P0f3, t443, DecS3, ALU.mult, [b_t44, b_DecS], [b_P0f])
                    pm, b_pm = Pm[0]
                    pt, b_pt = PT[0]
                    yb, b_yb = Yb[0]
                    pm3 = pm[0:64, :].rearrange("p (h n) -> p h n", h=4)
                    CP("pool", pm[0:64, :], P0f[0:64, :], [b_P0f], [b_pm])
                    bk, b_bk = bank()
                    for h in range(4):
                        TR(bk[0:64, h * 64:(h + 1) * 64], P0f3[:, h, :], ident_f[0:64, 0:64], [b_P0f, b_const], [b_bk])
                    CP("act", pt[0:64, :], bk[0:64, 0:256], [b_bk], [b_pt])
                    TT("dve", yb[0:64, :].rearrange("p (h n) -> p h n", h=4), bk[0:64, 0:256].rearrange("p (h n) -> p h n", h=4),
                       bc3(ident_f[0:64, 0:64], [64, 4, 64], 1), ALU.add, [b_bk, b_const], [b_yb])
                    cur = 0
                    for lev in range(1, 6):
                        pm, b_pm = Pm[cur]
                        pt, b_pt = PT[cur]
                        yb, b_yb = Yb[cur]
                        pmn, b_pmn = Pm[1 - cur]
                        ptn, b_ptn = PT[1 - cur]
                        ybn, b_ybn = Yb[1 - cur]
                        v3 = lambda a: a[0:64, :].rearrange("p (h n) -> p h n", h=4)
                        bkA, b_bkA = bank()
                        for h in range(4):
                            MM(bkA[0:64, h * 64:(h + 1) * 64], v3(pt)[:, h, :], v3(pm)[:, h, :], [b_pt, b_pm], [b_bkA])
                        CP("act", pmn[0:64, :], bkA[0:64, 0:256], [b_bkA], [b_pmn])
                        if lev < 5:
                            bkB, b_bkB = bank()
                            for h in range(4):
                                MM(bkB[0:64, h * 64:(h + 1) * 64], v3(pm)[:, h, :], v3(pt)[:, h, :], [b_pt, b_pm], [b_bkB])
                            CP("dve", ptn[0:64, :], bkB[0:64, 0:256], [b_bkB], [b_ptn])
                        bkC, b_bkC = bank()
                        for h in range(4):
                            MM(bkC[0:64, h * 64:(h + 1) * 64], v3(pmn)[:, h, :], v3(yb)[:, h, :], [b_pmn, b_yb], [b_bkC])
                        TT("dve", ybn[0:64, :], yb[0:64, :], bkC[0:64, 0:256], ALU.add, [b_yb, b_bkC], [b_ybn])
                        cur = 1 - cur
                    RT, b_RT = Yb[cur]
                    RT3 = RT[0:64, :].rearrange("p (h n) -> p h n", h=4)
                    bk, b_bk = bank()
                    for h in range(4):
                        MM(bk[:, h * 64:(h + 1) * 64], kbg3[:, h, :], RT3[:, h, :], [b_kbg, b_RT], [b_bk])
                    ACT(nwT[:, :], bk[:, 0:256], AF.Copy, [b_bk], [b_nwT], scale=-1.0)
                    bk, b_bk = bank()
                    for h in range(4):
                        MM(bk[0:64, h * 128:(h + 1) * 128], RT3[:, h, :], bv3[:, h, :], [b_RT, b_bv], [b_bk], start=True, stop=False)
                        MM(bk[0:64, h * 128:(h + 1) * 128], nwT3[:, h, :], Sb3[:, h, :], [b_nwT, b_Sb], [b_bk], start=False, stop=True)
                    CP("act", vnew[0:64, :], bk[0:64, 0:512], [b_bk], [b_vnew])
                    bk, b_bk = bank()
                    for h in range(4):
                        MM(bk[0:64, h * 64:(h + 1) * 64], gdk3[:, h, lo:lo + 64], gdq3[:, h, lo:lo + 64], [b_gdk, b_gdq], [b_bk])
                    TT("dve", qkd3, bk[0:64, 0:256].rearrange("p (h n) -> p h n", h=4), DecT3, ALU.mult, [b_bk, b_DecT], [b_qkd])
                    bko, b_bko = bank()
                    for h in range(4):
                        MM(bko[:, h * 64:(h + 1) * 64], Sb3[:, h, :], gqt3[:, h, :], [b_Sb, b_gqt], [b_bko], start=True, stop=False)
                        MM(bko[:, h * 64:(h + 1) * 64], vnew3[:, h, :], qkd3[:, h, :], [b_vnew, b_qkd], [b_bko], start=False, stop=True)
                    bk, b_bk = bank()
                    for h in range(4):
                        MM(bk[:, h * 128:(h + 1) * 128], kend3[:, h, :], vnew3[:, h, :], [b_kend, b_vnew], [b_bk])
                    TT("dve", Sst3, Sst3, bc3(EG3[:, c, :], [128, 4, 128], 2), ALU.mult, [b_S, b_EG], [b_S])
                    TT("dve", Sst[:, :], Sst[:, :], bk[:, 0:512], ALU.add, [b_S, b_bk], [b_S])
                    CP("pool", Sb[:, :], Sst[:, :], [b_S], [b_Sb])
                    ACT(osq[:, :], bko[:, 0:256], AF.Square, [b_bko], [b_osq])
                    bk, b_bk = bank()
                    MM(bk[:, 0:256], ones_f[:, :], osq[:, :], [b_const, b_osq], [b_bk])
                    rsqrt_(orstd[:, :], bk[:, 0:256], 1.0 / 128.0, 1e-6, [b_bk], [b_orstd], (osq[:, :], b_osq))
                    TT("dve", ot1[:, :], bko[:, 0:256], orstd[:, :], ALU.mult, [b_bko, b_orstd], [b_ot1])
                    STT(mixT3[:, 4:8, :], ot1[:, :].rearrange("p (h n) -> p h n", h=4), gdnw[:, 0:1], siluz3[:, :, lo:lo + 64],
                        ALU.mult, ALU.mult, [b_ot1, b_small, b_siluz], [b_mixT])
                    hp_ = []
                    hb_ = []
                    for n in range(2):
                        bk, b_bk = bank()
                        for f in range(8):
                            MM(bk[0:64, 0:512], mixT3[:, f, :], wout3[:, f, n * 512:(n + 1) * 512], [b_mixT, b_wout], [b_bk],
                               start=(f == 0), stop=(f == 7))
                        hp_.append(bk[0:64, 0:512])
                        hb_.append(b_bk)
                    ln_epilogue(64, g * GT + c * 64, hp_, hb_, src, src_bufs_fn(ci), G, Bt, b_gb, dst, dst_bufs_fn(ci), lnw[ci % 2], False)
            P.barrier()


        def xattn(l):
            AR.reset()
            W = []
            for i in range(4):
                w, bw = AR.bf16(8 * 1024)
                w3 = w.rearrange("p (k n) -> p k n", k=8)
                DMA("pool", w3, xa_w[i][l].rearrange("(k p) n -> p k n", p=128), [], [bw])
                W.append((w3, bw))
            (wq3, b_wq), (wk3, b_wk), (wv3, b_wv), (wo3, b_wo) = W
            G, b_gb = AR.f32(1024)
            Bt, _ = AR.f32(1024)
            load_bcast(G, lng[1][l:l + 1, :], 128, [b_gb])
            load_bcast(Bt, lnb[1][l:l + 1, :], 128, [b_gb])
            memT, b_memT = AR.bf16(8 * 256)
            memT3 = memT.rearrange("p (k n) -> p k n", k=8)
            xstg = AR.f32(1024)
            load_xT(mem_in, [], 0, 256, memT3, b_memT, xstg)
            kT, b_kT = AR.bf16(8 * 256)
            kT3 = kT.rearrange("p (k n) -> p k n", k=8)
            for f in range(8):
                bk, b_bk = bank()
                for k in range(8):
                    MM(bk[:, 0:256], wk3[:, k, f * 128:(f + 1) * 128], memT3[:, k, :], [b_wk, b_memT], [b_bk], start=(k == 0), stop=(k == 7))
                CP("dve", kT3[:, f, :], bk[:, 0:256], [b_bk], [b_kT])
            vv, b_vv = AR.bf16(2 * 1024)
            vv3 = vv.rearrange("p (m n) -> p m n", m=2)
            for mc in range(2):
                for n in range(2):
                    bk, b_bk = bank()
                    for k in range(8):
                        MM(bk[:, 0:512], memT3[:, k, mc * 128:(mc + 1) * 128], wv3[:, k, n * 512:(n + 1) * 512], [b_wv, b_memT], [b_bk],
                           start=(k == 0), stop=(k == 7))
                    CP("act", vv3[:, mc, n * 512:(n + 1) * 512], bk[:, 0:512], [b_bk], [b_vv])
            XG = 512
            xTg, b_xTg = AR.bf16(8 * XG)
            xT3 = xTg.rearrange("p (k n) -> p k n", k=8)
            qT, b_qT = AR.bf16(8 * XG)
            qT3 = qT.rearrange("p (k n) -> p k n", k=8)
            expT = [AR.bf16(XG) for _ in range(2)]
            rdn, b_rdn = AR.f32(XG)
            oT, b_oT = AR.bf16(8 * XG)
            oT3 = oT.rearrange("p (k n) -> p k n", k=8)
            lnw = []
            for _ in range(2):
                a1, bb1 = AR.f32(1024)
                a2, bb2 = AR.f32(12)
                a3, bb3 = AR.f32(2)
                a4, bb4 = AR.f32(2)
                lnw.append((a1, bb1, a2[:, :].rearrange("p (a b) -> p a b", a=2), bb2, a3, bb3, a4, bb4))
            for g in range(S // XG):
                rb_ = [b_xres[g * 8 + c] for c in range(8)]
                load_xT(xres, rb_, g * XG, XG, xT3, b_xTg, xstg)
                for f in range(8):
                    bk, b_bk = bank()
                    for k in range(8):
                        MM(bk[:, 0:XG], wq3[:, k, f * 128:(f + 1) * 128], xT3[:, k, :], [b_wq, b_xTg], [b_bk], start=(k == 0), stop=(k == 7))
                    ACT(qT3[:, f, :], bk[:, 0:XG], AF.Copy, [b_bk], [b_qT], scale=1.0 / 16.0)
                for h in range(4):
                    for mc in range(2):
                        bk, b_bk = bank()
                        for j in range(2):
                            MM(bk[:, 0:XG], kT3[:, 2 * h + j, mc * 128:(mc + 1) * 128], qT3[:, 2 * h + j, :], [b_kT, b_qT], [b_bk],
                               start=(j == 0), stop=(j == 1))
                        ACT(expT[mc][0][:, :], bk[:, 0:XG], AF.Exp, [b_bk], [expT[mc][1]])
                    bd, b_bd = bank()
                    for mc in range(2):
                        MM(bd[:, 0:XG], ones_b[:, :], expT[mc][0][:, :], [b_const, expT[mc][1]], [b_bd], start=(mc == 0), stop=(mc == 1))
                    P.op("dve", lambda e, bd=bd: e.reciprocal(rdn[:, :], bd[:, 0:XG]), [b_bd], [b_rdn])
                    for j in range(2):
                        bk, b_bk = bank()
                        for mc in range(2):
                            MM(bk[:, 0:XG], vv3[:, mc, (2 * h + j) * 128:(2 * h + j + 1) * 128], expT[mc][0][:, :], [b_vv, expT[mc][1]], [b_bk],
                               start=(mc == 0), stop=(mc == 1))
                        TT("dve", oT3[:, 2 * h + j, :], bk[:, 0:XG], rdn[:, :], ALU.mult, [b_bk, b_rdn], [b_oT])
                for t in range(XG // 128):
                    hp_, hb_ = [], []
                    for n in range(2):
                        bk, b_bk = bank()
                        for f in range(8):
                            MM(bk[:, 0:512], oT3[:, f, t * 128:(t + 1) * 128], wo3[:, f, n * 512:(n + 1) * 512], [b_oT, b_wo], [b_bk],
                               start=(f == 0), stop=(f == 7))
                        hp_.append(bk[:, 0:512])
                        hb_.append(b_bk)
                    r0 = g * XG + t * 128
                    rb2 = [b_xres[r0 // 64], b_xres[r0 // 64 + 1]]
                    ln_epilogue(128, r0, hp_, hb_, xres, rb2, G, Bt, b_gb, xres, rb2, lnw[t % 2], False)
            P.barrier()

        def moe(l, dst, dst_bufs_fn):
            AR.reset()
            G, b_gb = AR.f32(1024)
            Bt, _ = AR.f32(1024)
            load_bcast(G, lng[2][l:l + 1, :], 128, [b_gb])
            load_bcast(Bt, lnb[2][l:l + 1, :], 128, [b_gb])
            dest = dest_t
            b_dest = Buf("dest")
            gate, b_gate = AR.f32(32 * 4)
            gate3 = gate.rearrange("p (t k) -> p t k", t=32)
            mark = AR.off
            rw, b_rw = AR.f32(8 * NE)
            rw3 = rw.rearrange("p (k e) -> p k e", k=8)
            DMA("sp", rw3, r_w[l].rearrange("(k p) e -> p k e", p=128), [], [b_rw])
            rb, _ = AR.f32(NE)
            load_bcast(rb, r_b[l:l + 1, :], 128, [b_rw])
            cnt, b_cnt = AR.f32(NE)
            P.op("pool", lambda e: e.memset(cnt[:, :], 0.0), [], [b_cnt])
            xs2 = [AR.f32(1024) for _ in range(2)]
            xT32, b_xT32 = AR.f32(8 * 128)
            xT323 = xT32.rearrange("p (k n) -> p k n", k=8)
            xbf = [AR.bf16(1024) for i in range(2)]
            lg, b_lg = AR.f32(NE)
            mx8, b_mx8 = AR.f32(8)
            sm, b_sm = AR.f32(8)
            ex, b_ex = AR.f32(NE)
            msk, b_msk = AR.f32(NE)
            mskb, b_mskb = AR.bf16(NE)
            pos, b_pos = AR.f32(NE)
            ov, b_ov = AR.f32(NE)
            oh, b_oh = AR.f32(NE)
            tm, b_tm = AR.f32(NE)
            dsel, b_dsel = AR.f32(4)
            dsel16, b_dsel16 = AR.f32(16)
            b_xsall = Buf("xs_all")
            b_ysall = Buf("ys_all")
            for t in range(32):
                xs_, b_xs_ = xs2[t % 2]
                xb_, b_xb_ = xbf[t % 2]
                rbf = [b_xres[2 * t], b_xres[2 * t + 1]]
                DMA("sp", xs_[:, :], xres[t * 128:(t + 1) * 128, :], rbf, [b_xs_])
                for half in range(2):
                    bk, b_bk = bank()
                    for k in range(8):
                        TR(bk[:, k * 64:(k + 1) * 64], xs_[half * 64:(half + 1) * 64, k * 128:(k + 1) * 128],
                           ident_f[half * 64:(half + 1) * 64, half * 64:(half + 1) * 64], [b_xs_, b_const], [b_bk])
                    CP("dve", xT323[:, :, half * 64:(half + 1) * 64], bk[:, 0:512].rearrange("p (k n) -> p k n", k=8), [b_bk], [b_xT32])
                CP("act", xb_[:, :], xs_[:, :], [b_xs_], [b_xb_])
                bl, b_bl = bank()
                for k in range(8):
                    MM(bl[:, 0:NE], xT323[:, k, :], rw3[:, k, :], [b_xT32, b_rw], [b_bl], start=(k == 0), stop=(k == 7))
                TT("dve", lg[:, :], bl[:, 0:NE], rb[:, :], ALU.add, [b_bl, b_rw], [b_lg])
                P.op("dve", lambda e: e.max(mx8[:, :], lg[:, :]), [b_lg], [b_mx8])
                TS("dve", sm[:, 0:1], mx8[:, 0:1], -1.0, ALU.mult, [b_mx8], [b_sm])
                ACT(ex[:, :], lg[:, :], AF.Exp, [b_lg, b_sm], [b_ex], bias=sm[:, 0:1])
                TS("dve", msk[:, :], lg[:, :], mx8[:, 3:4], ALU.is_ge, [b_lg, b_mx8], [b_msk])
                TT("dve", ex[:, :], ex[:, :], msk[:, :], ALU.mult, [b_ex, b_msk], [b_ex])
                P.op("dve", lambda e: e.tensor_reduce(sm[:, 1:2], ex[:, :], AX.X, ALU.add), [b_ex], [b_sm])
                P.op("dve", lambda e: e.reciprocal(sm[:, 2:3], sm[:, 1:2]), [b_sm], [b_sm])
                TS("dve", ex[:, :], ex[:, :], sm[:, 2:3], ALU.mult, [b_ex, b_sm], [b_ex])
                CP("pool", mskb[:, :], msk[:, :], [b_msk], [b_mskb])
                bp, b_bp = bank()
                MM(bp[:, 0:NE], tri_b[:, :], mskb[:, :], [b_const, b_mskb], [b_bp])
                MM(bp[:, NE:2 * NE], ones_b[:, :], mskb[:, :], [b_const, b_mskb], [b_bp])
                TT("dve", pos[:, :], bp[:, 0:NE], cnt[:, :], ALU.add, [b_bp, b_cnt], [b_pos])
                TT("dve", cnt[:, :], cnt[:, :], bp[:, NE:2 * NE], ALU.add, [b_bp, b_cnt], [b_cnt])
                TS("dve", ov[:, :], pos[:, :], float(CAP), ALU.is_ge, [b_pos], [b_ov])
                STT(pos[:, :], ov[:, :], 1.0e7, pos[:, :], ALU.mult, ALU.add, [b_ov, b_pos], [b_pos])
                TT("dve", pos[:, :], pos[:, :], eoff[:, :], ALU.add, [b_pos, b_const], [b_pos])
                TS("dve", ov[:, :], ov[:, :], -1.0, ALU.mult, [b_ov], [b_ov], s2=1.0, op1=ALU.add)
                TT("dve", ex[:, :], ex[:, :], ov[:, :], ALU.mult, [b_ex, b_ov], [b_ex])
                for k in range(4):
                    TS("dve", oh[:, :], lg[:, :], mx8[:, k:k + 1], ALU.is_equal, [b_lg, b_mx8], [b_oh])
                    TT("dve", tm[:, :], oh[:, :], pos[:, :], ALU.mult, [b_oh, b_pos], [b_tm])
                    P.op("dve", lambda e, k=k: e.tensor_reduce(dsel[:, k:k + 1], tm[:, :], AX.X, ALU.add), [b_tm], [b_dsel])
                    TT("dve", tm[:, :], oh[:, :], ex[:, :], ALU.mult, [b_oh, b_ex], [b_tm])
                    P.op("dve", lambda e, k=k, t=t: e.tensor_reduce(gate3[:, t, k:k + 1], tm[:, :], AX.X, ALU.add), [b_tm], [b_gate])
                CP("dve", dest[:, t * 4:t * 4 + 4], dsel[:, 0:4], [b_dsel], [b_dest])
                for k in range(4):
                    q = idx_ctr[0] % 8
                    idx_ctr[0] += 1
                    CP("dve", idx_t[q][:, :], dest[:, t * 4 + k:t * 4 + k + 1], [b_dest], [b_idx[q]])
                    P.dma("pool", lambda e, q=q, xb_=xb_: e.indirect_dma_start(
                        out=xs_d, out_offset=bass.IndirectOffsetOnAxis(ap=idx_t[q][:, :], axis=0),
                        in_=xb_[:, :], in_offset=None, bounds_check=bc_reg(e), oob_is_err=False), [b_xb_, b_idx[q]], [b_xsall])
            P.barrier()
            AR.off = mark
            wgu = [AR.bf16(8 * 2048) for _ in range(2)]
            wdn = [AR.bf16(8 * 1024) for _ in range(2)]
            bdn = [AR.f32(1024) for _ in range(1)]
            bgr, b_bgr = AR.f32(128)
            bguT, b_bguT = AR.f32(NE * 16)
            for q in range(4):
                DMA("sp", bgr[:, :], b_gu[l].rearrange("e (c p) -> (e c) p", p=128)[q * 128:(q + 1) * 128, :], [], [b_bgr])
                bk, b_bk = bank()
                TR(bk[:, 0:128], bgr[:, :], ident_f[:, :], [b_bgr, b_const], [b_bk])
                CP("dve", bguT[:, q * 128:(q + 1) * 128], bk[:, 0:128], [b_bk], [b_bguT])
            bgv = bguT[:, :].rearrange("p (e c) -> p e c", c=16)
            TS("dve", bgv[:, :, 8:16], bgv[:, :, 8:16], 1.0, ALU.add, [b_bguT], [b_bguT])
            xsel, b_xsel = AR.bf16(NCAP * 1024)
            xsel3 = xsel.rearrange("p (j d) -> p j d", j=NCAP)
            xselT, b_xselT = AR.bf16(8 * CAP)
            xselT3 = xselT.rearrange("p (k n) -> p k n", k=8)
            actT, b_actT = AR.bf16(8 * CAP)
            actT3 = actT.rearrange("p (k n) -> p k n", k=8)
            HC = CAP // 2
            gtmp, b_gtmp = AR.f32(HC)
            stmp, b_stmp = AR.f32(HC)
            ltmp, b_ltmp = AR.f32(HC)
            ysb = [AR.f32(1024) for _ in range(1)]
            NST, PD = 5, 4
            wstage = [AR.f32(1024) for _ in range(NST)]
            wctr = [0]

            def wsteps(e2):
                wg_, b_wg_ = wgu[e2 % 2]
                wg3_ = wg_.rearrange("p (k n) -> p k n", k=8)
                wd_, b_wd_ = wdn[e2 % 2]
                wd3_ = wd_.rearrange("p (k n) -> p k n", k=8)
                pieces = []
                for k in range(8):
                    for hf in range(2):
                        pieces.append((w_gu[l, e2, k * 128:(k + 1) * 128, hf * 1024:(hf + 1) * 1024], wg3_[:, k, hf * 1024:(hf + 1) * 1024], b_wg_))
                for k in range(8):
                    pieces.append((w_dn[l, e2, k * 128:(k + 1) * 128, :], wd3_[:, k, :], b_wd_))
                slots = []

                def mk_dma(i):
                    def f():
                        sl = wctr[0] % NST
                        wctr[0] += 1
                        slots.append(sl)
                        DMA("sp", wstage[sl][0][:, :], pieces[i][0], [], [wstage[sl][1]])
                    return f

                def mk_cast(i):
                    def f():
                        sl = slots[i]
                        CP("act", pieces[i][1], wstage[sl][0][:, :], [wstage[sl][1]], [pieces[i][2]])
                    return f
                n = len(pieces)
                steps = []
                for i in range(n + PD):
                    fs = []
                    if i - PD >= 0:
                        fs.append(mk_cast(i - PD))
                    if i < n:
                        fs.append(mk_dma(i))
                    steps.append(lambda fs=fs: [f() for f in fs])
                return steps

            for e_ in range(NE):
                wg, b_wg = wgu[e_ % 2]
                wg3 = wg.rearrange("p (k n) -> p k n", k=8)
                wd, b_wd = wdn[e_ % 2]
                wd3 = wd.rearrange("p (k n) -> p k n", k=8)
                bd_, b_bd_ = bdn[0]
                if e_ == 0:
                    for step in wsteps(0):
                        step()
                pending = wsteps(e_ + 1) if e_ + 1 < NE else []
                DMA("sp", bd_[:, :], b_dn[l, e_:e_ + 1, :].partition_broadcast(128), [], [b_bd_])
                DMA("sp", xsel3, xs_d[e_ * CAP:(e_ + 1) * CAP, :].rearrange("(j p) d -> p j d", p=128), [b_xsall], [b_xsel])
                for j in range(NCAP):
                    for k in range(8):
                        TR(psb[:, k * 128:(k + 1) * 128], xsel3[:, j, k * 128:(k + 1) * 128], ident_b[:, :], [b_xsel, b_const], [b_psb])
                    CP("act", xselT3[:, :, j * 128:(j + 1) * 128], psb[:, :].rearrange("p (k n) -> p k n", k=8), [b_psb], [b_xselT])
                for f in range(8):
                    for hh in range(2):
                        bg, b_bg = bank()
                        for k in range(8):
                            MM(bg[:, 0:HC], wg3[:, k, f * 128:(f + 1) * 128], xselT3[:, k, hh * HC:(hh + 1) * HC], [b_wg, b_xselT], [b_bg],
                               start=(k == 0), stop=(k == 7))
                        bl, b_bl = bank()
                        for k in range(8):
                            MM(bl[:, 0:HC], wg3[:, k, 1024 + f * 128:1024 + (f + 1) * 128], xselT3[:, k, hh * HC:(hh + 1) * HC], [b_wg, b_xselT], [b_bl],
                               start=(k == 0), stop=(k == 7))
                        c0 = e_ * 16 + f
                        TS("dve", gtmp[:, :], bg[:, 0:HC], bguT[:, c0:c0 + 1], ALU.add, [b_bg, b_bguT], [b_gtmp], s2=7.0, op1=ALU.min)
                        ACT(stmp[:, :], gtmp[:, :], AF.Sigmoid, [b_gtmp], [b_stmp], scale=1.702)
                        TS("dve", ltmp[:, :], bl[:, 0:HC], bguT[:, c0 + 8:c0 + 9], ALU.add, [b_bl, b_bguT], [b_ltmp], s2=8.0, op1=ALU.min)
                        TT("pool", gtmp[:, :], gtmp[:, :], stmp[:, :], ALU.mult, [b_gtmp, b_stmp], [b_gtmp])
                        STT(actT3[:, f, hh * HC:(hh + 1) * HC], ltmp[:, :], -6.0, gtmp[:, :], ALU.max, ALU.mult, [b_gtmp, b_ltmp], [b_actT])
                        if pending:
                            pending.pop(0)()
                for j in range(NCAP):
                    ys_, b_ys_ = ysb[0]
                    for n in range(2):
                        bk, b_bk = bank()
                        for f in range(8):
                            MM(bk[:, 0:512], actT3[:, f, j * 128:(j + 1) * 128], wd3[:, f, n * 512:(n + 1) * 512], [b_actT, b_wd], [b_bk],
                               start=(f == 0), stop=(f == 7))
                        TT("dve", ys_[:, n * 512:(n + 1) * 512], bk[:, 0:512], bd_[:, n * 512:(n + 1) * 512], ALU.add, [b_bk, b_bd_], [b_ys_])
                        if pending:
                            pending.pop(0)()
                    DMA("sp", ys_d[e_ * CAP + j * 128:e_ * CAP + (j + 1) * 128, :], ys_[:, :], [b_ys_], [b_ysall])
                while pending:
                    pending.pop(0)()
            P.barrier()
            AR.off = mark
            gth_all = [AR.f32(1024) for i in range(8)]
            for k in range(8):
                P.op("pool", lambda e, k=k: e.memset(gth_all[k][0][:, :], 0.0), [], [gth_all[k][1]])
            accs = [AR.f32(1024) for _ in range(2)]
            lnw = []
            for _ in range(2):
                a1, bb1 = AR.f32(1024)
                a2, bb2 = AR.f32(12)
                a3, bb3 = AR.f32(2)
                a4, bb4 = AR.f32(2)
                lnw.append((a1, bb1, a2[:, :].rearrange("p (a b) -> p a b", a=2), bb2, a3, bb3, a4, bb4))
            for t in range(32):
                gth = gth_all[(t % 2) * 4:(t % 2) * 4 + 4]
                for k in range(4):
                    q = idx_ctr[0] % 8
                    idx_ctr[0] += 1
                    CP("dve", idx_t[q][:, :], dest[:, t * 4 + k:t * 4 + k + 1], [b_dest], [b_idx[q]])
                    P.dma("pool", lambda e, k=k, q=q, gth=gth: e.indirect_dma_start(
                        out=gth[k][0][:, :], out_offset=None, in_=ys_d,
                        in_offset=bass.IndirectOffsetOnAxis(ap=idx_t[q][:, :], axis=0),
                        bounds_check=bc_reg(e), oob_is_err=False), [b_ysall, b_idx[q]], [gth[k][1]])
                ac, b_ac = accs[t % 2]
                TS("dve", ac[:, :], gth[0][0][:, :], gate3[:, t, 0:1], ALU.mult, [gth[0][1], b_gate], [b_ac])
                for k in range(1, 4):
                    STT(ac[:, :], gth[k][0][:, :], gate3[:, t, k:k + 1], ac[:, :], ALU.mult, ALU.add, [gth[k][1], b_gate, b_ac], [b_ac])
                rb2 = [b_xres[2 * t], b_xres[2 * t + 1]]
                ln_epilogue(128, t * 128, [ac[:, 0:512], ac[:, 512:1024]], [b_ac, b_ac], xres, rb2, G, Bt, b_gb, dst, dst_bufs_fn(t), lnw[t % 2], False)
            P.barrier()

        for l in range(n_layers):
            if "mixer" in skip:
                AR.reset()
                cpb = [AR.f32(1024) for _ in range(2)]
                for t in range(32):
                    a, bb = cpb[t % 2]
                    DMA("sp", a[:, :], x_in[t * 128:(t + 1) * 128, :], [], [bb])
                    DMA("sp", xres[t * 128:(t + 1) * 128, :], a[:, :], [bb], [b_xres[2 * t], b_xres[2 * t + 1]])
                P.barrier()
            elif l == 0:
                mixer(l, x_in, lambda ci: [], xres, lambda ci: [b_xres[ci]])
            else:
                mixer(l, xres, lambda ci: [b_xres[ci]], xres, lambda ci: [b_xres[ci]])
            if stop_after == (l, "mixer"):
                break
            if "xattn" not in skip:
                xattn(l)
            if stop_after == (l, "xattn"):
                break
            last = (l == n_layers - 1) and stop_after is None
            if last:
                moe(l, out_d, lambda t: [b_out])
            else:
                moe(l, xres, lambda t: [b_xres[2 * t], b_xres[2 * t + 1]])
            if stop_after == (l, "moe"):
                break
        if stop_after is not None:
            AR.reset()
            fin = [AR.f32(1024) for _ in range(2)]
            for t in range(32):
                a, bb = fin[t % 2]
                DMA("sp", a[:, :], xres[t * 128:(t + 1) * 128, :], [b_xres[2 * t], b_xres[2 * t + 1]], [bb])
                DMA("sp", out_d[t * 128:(t + 1) * 128, :], a[:, :], [bb], [b_out])
        P.barrier()
        print("ops:", P.n_ops, {e: len(s) for e, s in P.streams.items()})
        P.emit()
    return nc


def make_consts():
    i = np.arange(64)
    c = {
        "c_ident": np.eye(128, dtype=np.float32),
        "c_t1": (i[:, None] <= i[None, :]).astype(np.float32),
        "c_negc": np.where(i[None, :] >= i[:, None], 0.0, NEG).astype(np.float32),
        "c_negs": np.where(i[None, :] < i[:, None], 0.0, NEG).astype(np.float32),
        "c_tri": (np.arange(128)[:, None] < np.arange(128)[None, :]).astype(np.float32),
        "c_eoff": np.tile((np.arange(NE) * CAP).astype(np.float32)[None, :], (128, 1)),
        "c_ones": np.ones((128, 128), np.float32),
        "c_cp": np.tile(np.arange(4, dtype=np.float32)[None, :], (128, 4)),
    }
    return c


_NAMES = ["w_in", "mlstm_b_i", "mlstm_b_f", "mlstm_norm_w", "gdn_conv_w", "gdn_a_log", "gdn_dt_bias", "gdn_norm_w", "w_out",
          "ln1_g", "ln1_b", "xa_wq", "xa_wk", "xa_wv", "xa_wo", "ln2_g", "ln2_b", "router_w", "router_b",
          "exp_w_gu", "exp_b_gu", "exp_w_down", "exp_b_down", "ln3_g", "ln3_b"]


def run(inputs, n_layers=L, stop_after=None, cores=8, max_groups=None, small=False, skip=(), trace=False):
    nc = build(n_layers, stop_after, max_groups=max_groups, small=small, skip=skip)
    consts = make_consts()
    shared = {k: np.ascontiguousarray(np.asarray(inputs[k], dtype=np.float32)) for k in _NAMES}
    if small:
        names = small if isinstance(small, (set, tuple, list)) else ("xa_wq", "xa_wk", "xa_wv", "xa_wo", "router_w", "exp_w_gu", "exp_b_gu", "exp_w_down", "exp_b_down")
        for k in names:
            shared[k] = np.zeros([1] * shared[k].ndim, np.float32)
    in_maps = []
    for c in range(cores):
        m = dict(shared)
        m.update(consts)
        m["x"] = np.ascontiguousarray(np.asarray(inputs["x"][c], dtype=np.float32))
        m["mem"] = np.ascontiguousarray(np.asarray(inputs["mem"][c], dtype=np.float32))
        in_maps.append(m)
    res = run_bass_kernel_spmd(nc, in_maps, core_ids=list(range(cores)), **({"trace": True} if trace else {}))
    if trace:
        print("EXEC_TIME_NS", res.exec_time_ns)
    return np.stack([np.asarray(r["out"]) for r in res.results], axis=0)


def kernel(**inputs):
    return run(inputs).astype(np.float32)
```
